# Optimizing a Trainium2 kernel written in Bass

```python
import jax
import jax.numpy as jnp
from jax import lax
import numpy as np

D_MODEL = 2048
BATCH = 2
SEQ = 4096
DEPTH = 2

POOL_WINDOWS = (2, 4, 8, 16)
POOL_GROUP = 128
POOL_WIDTH = 512
ATT_GROUPS = ((128, 1), (512, 4), (2048, 16))
ATT_HEADS_PER_GROUP = 4
ATT_HEAD_DIM = 64
ATT_HEADS = 12
ATT_WIDTH = 768
ATT_OUT_WIDTH = 256
ATT_BLOCK = 128
ALIBI_SLOPES = tuple(2.0 ** (-8.0 * (h + 1) / ATT_HEADS) for h in range(ATT_HEADS))
GLA_HEADS = 4
GLA_DK = 64
GLA_DV = 128
GLA_KEY_WIDTH = 256
GLA_VAL_WIDTH = 512
GLA_GATE_RANK = 16
GLA_GATE_TEMP = 16.0
GLA_CHUNK = 32
GLA_NORM_EPS = 1e-6
RWKV_HEADS = 8
RWKV_HEAD_DIM = 64
RWKV_WIDTH = 512
RWKV_DECAY_RANK = 32
RWKV_AAA_RANK = 32
RWKV_GATE_RANK = 96
RWKV_SPLITS = (RWKV_WIDTH, RWKV_WIDTH, RWKV_WIDTH, RWKV_DECAY_RANK, RWKV_AAA_RANK, RWKV_GATE_RANK)
RWKV_SHIFT_WIDTH = sum(RWKV_SPLITS)
RWKV_GN_EPS = 64e-5
N_BRANCHES = 4
IN_SPLITS = (POOL_WIDTH, ATT_WIDTH, ATT_WIDTH, ATT_WIDTH, GLA_KEY_WIDTH, GLA_KEY_WIDTH,
             GLA_VAL_WIDTH, GLA_VAL_WIDTH, GLA_GATE_RANK, RWKV_SHIFT_WIDTH, N_BRANCHES * D_MODEL)
D_IN = sum(IN_SPLITS)
N_EXPERTS = 32
TOP_K = 4
EXPERT_FF = 1024
SWIGLU_LIMIT = 7.0
SWIGLU_ALPHA = 1.702
MOE_BLOCK = 128
LN_EPS = 1e-5
DEEPNORM_ALPHA = (2 * DEPTH) ** 0.25
DEEPNORM_BETA = (8 * DEPTH) ** -0.25

kernel_name = 'hybrid_pool_dilated_gla_rwkv7_moe_deepnorm'


def _split(t, sizes):
    return jnp.split(t, np.cumsum(sizes)[:-1].tolist(), axis=-1)


def layer_norm(x, g, b):
    xf = x.astype(jnp.float32)
    mu = jnp.mean(xf, -1, keepdims=True)
    var = jnp.mean(jnp.square(xf - mu), -1, keepdims=True)
    return ((xf - mu) * lax.rsqrt(var + LN_EPS)).astype(x.dtype) * g + b


def pool_mixer(v, pool_w, pool_scale):
    B, S, _ = v.shape
    vg = v.reshape(B, S, len(POOL_WINDOWS), POOL_GROUP)
    cs = jnp.cumsum(vg.astype(jnp.float32), axis=1)
    cs = jnp.concatenate([jnp.zeros_like(cs[:, :1]), cs], axis=1)
    t = jnp.arange(S)
    means = []
    for g, w in enumerate(POOL_WINDOWS):
        csg = cs[:, :, g]
        lo = jnp.maximum(t + 1 - w, 0)
        cnt = (t + 1 - lo).astype(jnp.float32)
        means.append((csg[:, 1:] - csg[:, lo]) / cnt[:, None])
    pooled = jnp.stack(means, axis=2).astype(v.dtype)
    y = jnp.einsum('bsgc,gcd->bsgd', pooled - vg, pool_w)
    return y.reshape(B, S, POOL_WIDTH) * pool_scale


def dilated_group_attention(q, k, v, window, dilation, slopes):
    B, S, H, Dh = q.shape
    L = S // dilation
    nb = -(-L // ATT_BLOCK)
    Lp = nb * ATT_BLOCK
    span = window // dilation

    def to_blocks(t):
        t = t.reshape(B, L, dilation, H, Dh).transpose(0, 2, 1, 3, 4)
        t = jnp.pad(t, ((0, 0), (0, 0), (0, Lp - L), (0, 0), (0, 0)))
        return t.reshape(B, dilation, nb, ATT_BLOCK, H, Dh)

    def with_prev(t):
        prev = jnp.pad(t[:, :, :-1], ((0, 0), (0, 0), (1, 0), (0, 0), (0, 0), (0, 0)))
        return jnp.concatenate([prev, t], axis=3)

    def from_blocks(t):
        t = t.reshape(B, dilation, Lp, *t.shape[4:])[:, :, :L]
        return jnp.swapaxes(t, 1, 2).reshape(B, S, *t.shape[3:])

    f32 = jnp.float32
    qb = to_blocks(q).astype(f32)
    kw = with_prev(to_blocks(k)).astype(f32)
    vw = with_prev(to_blocks(v)).astype(f32)
    scores = jnp.einsum('brnqhc,brnkhc->brnhqk', qb, kw) * (Dh ** -0.5)
    qi = jnp.arange(ATT_BLOCK)[:, None] + ATT_BLOCK
    kj = jnp.arange(2 * ATT_BLOCK)[None, :]
    dist = qi - kj
    blk = jnp.arange(nb)[:, None, None]
    valid = (dist >= 0) & (dist <= span) & ((blk > 0) | (kj >= ATT_BLOCK))
    alibi = -jnp.asarray(slopes, f32)[:, None, None] * (dist * dilation).astype(f32)
    scores = jnp.where(valid[:, None], scores + alibi, -jnp.inf)
    lse = jax.nn.logsumexp(scores, axis=-1)
    p = jnp.exp(scores - lse[..., None])
    out = jnp.einsum('brnhqk,brnkhc->brnqhc', p, vw)
    return from_blocks(out), from_blocks(jnp.swapaxes(lse, 3, 4))


def dilated_attention(q, k, v):
    B, S, _ = q.shape
    shp = (B, S, len(ATT_GROUPS), ATT_HEADS_PER_GROUP, ATT_HEAD_DIM)
    q, k, v = q.reshape(shp), k.reshape(shp), v.reshape(shp)
    outs, lses = [], []
    for g, (window, dilation) in enumerate(ATT_GROUPS):
        slopes = ALIBI_SLOPES[g * ATT_HEADS_PER_GROUP:(g + 1) * ATT_HEADS_PER_GROUP]
        o, s = dilated_group_attention(q[:, :, g], k[:, :, g], v[:, :, g], window, dilation, slopes)
        outs.append(o)
        lses.append(s)
    wts = jax.nn.softmax(jnp.stack(lses, 0), axis=0)
    out = jnp.einsum('gbsh,gbshc->bshc', wts, jnp.stack(outs, 0))
    return out.reshape(B, S, ATT_OUT_WIDTH).astype(q.dtype)


def gla_chunked(q, k, v, log_a):
    B, H, S, dk = q.shape
    dv = v.shape[-1]
    n = S // GLA_CHUNK
    q, k, log_a = (t.reshape(B, H, n, GLA_CHUNK, dk) for t in (q, k, log_a))
    v = v.reshape(B, H, n, GLA_CHUNK, dv)
    b = jnp.cumsum(log_a, axis=3)
    causal = jnp.tril(jnp.ones((GLA_CHUNK, GLA_CHUNK), bool))
    rel = jnp.where(causal[:, :, None], b[..., :, None, :] - b[..., None, :, :], -jnp.inf)
    scores = jnp.einsum('bhnic,bhnjc,bhnijc->bhnij', q, k, jnp.exp(rel))
    o = jnp.einsum('bhnij,bhnjv->bhniv', scores, v)
    b_last = b[..., -1, :]
    chunk_kv = jnp.einsum('bhnjc,bhnjv->bhncv', k * jnp.exp(b_last[..., None, :] - b), v)

    def step(state, inp):
        g_last, kv = inp
        return state * jnp.exp(g_last)[..., None] + kv, state

    _, s_prev = lax.scan(step, jnp.zeros((B, H, dk, dv), jnp.float32),
                         (jnp.moveaxis(b_last, 2, 0), jnp.moveaxis(chunk_kv, 2, 0)))
    o = o + jnp.einsum('bhnic,bhncv->bhniv', q * jnp.exp(b), jnp.moveaxis(s_prev, 0, 2))
    return o.reshape(B, H, S, dv)


def gla_mixer(q, k, v, g, a_lo, w_alpha, b_alpha, norm_g):
    B, S, _ = q.shape
    f32 = jnp.float32
    log_a = jax.nn.log_sigmoid((a_lo @ w_alpha + b_alpha).astype(f32)) / GLA_GATE_TEMP

    def heads(t, d):
        return t.reshape(B, S, GLA_HEADS, d).transpose(0, 2, 1, 3).astype(f32)

    o = gla_chunked(heads(q, GLA_DK) * (GLA_DK ** -0.5), heads(k, GLA_DK),
                    heads(v, GLA_DV), heads(log_a, GLA_DK))
    o = o * lax.rsqrt(jnp.mean(jnp.square(o), -1, keepdims=True) + GLA_NORM_EPS)
    o = o.transpose(0, 2, 1, 3).reshape(B, S, GLA_VAL_WIDTH).astype(q.dtype)
    return o * norm_g * jax.nn.silu(g)


def rwkv7_scan(r, decay, k, v, kk, a):
    B, S, H, N = r.shape

    def step(state, inp):
        r_t, w_t, k_t, v_t, kk_t, a_t = inp
        removed = jnp.einsum('bhvk,bhk->bhv', state, kk_t)
        state = (state * w_t[:, :, None, :]
                 - removed[..., None] * (kk_t * a_t)[:, :, None, :]
                 + v_t[..., None] * k_t[:, :, None, :])
        return state, jnp.einsum('bhvk,bhk->bhv', state, r_t)

    xs = tuple(jnp.moveaxis(t, 1, 0) for t in (r, decay, k, v, kk, a))
    _, y = lax.scan(step, jnp.zeros((B, H, N, N), jnp.float32), xs)
    return jnp.moveaxis(y, 0, 1)


def rwkv7_mixer(p, mu, w0, w2, a0, a2, g2, k_k, k_a, r_k, ln_g, ln_b):
    B, S, _ = p.shape
    f32 = jnp.float32
    p = p + (jnp.pad(p[:, :-1], ((0, 0), (1, 0), (0, 0))) - p) * mu
    r, k, v, w_lo, a_lo, g_lo = _split(p, RWKV_SPLITS)
    log_w = -jax.nn.softplus(-(w0 + jnp.tanh(w_lo) @ w2).astype(f32)) - 0.5
    a = jax.nn.sigmoid((a0 + a_lo @ a2).astype(f32))
    gate = jax.nn.sigmoid(g_lo) @ g2
    hd = (B, S, RWKV_HEADS, RWKV_HEAD_DIM)
    r, k, v = (t.astype(f32).reshape(hd) for t in (r, k, v))
    decay = jnp.exp(-jnp.exp(log_w)).reshape(hd)
    a = a.reshape(hd)
    kk = k * k_k.reshape(RWKV_HEADS, RWKV_HEAD_DIM).astype(f32)
    kk = kk / jnp.maximum(jnp.sqrt(jnp.sum(jnp.square(kk), -1, keepdims=True)), 1e-12)
    k = k * (1.0 + (a - 1.0) * k_a.reshape(RWKV_HEADS, RWKV_HEAD_DIM).astype(f32))
    y = rwkv7_scan(r, decay, k, v, kk, a)
    mean = jnp.mean(y, -1, keepdims=True)
    var = jnp.mean(jnp.square(y - mean), -1, keepdims=True)
    y = ((y - mean) * lax.rsqrt(var + RWKV_GN_EPS)).reshape(B, S, RWKV_WIDTH) * ln_g + ln_b
    bonus = jnp.sum(r * k * r_k.astype(f32), -1, keepdims=True) * v
    return (y + bonus.reshape(B, S, RWKV_WIDTH)).astype(p.dtype) * gate


def hybrid_mixer(h, w_in, pool_w, pool_scale, gla_w_alpha, gla_b_alpha, gla_norm_g,
                 rwkv_mu, rwkv_w0, rwkv_w2, rwkv_a0, rwkv_a2, rwkv_g2, rwkv_k_k, rwkv_k_a,
                 rwkv_r_k, rwkv_ln_g, rwkv_ln_b, w_branch_a, w_branch_b, w_branch_c,
                 w_branch_d, w_out):
    B, S, D = h.shape
    (pool_v, att_q, att_k, att_v, gla_q, gla_k, gla_v, gla_g, gla_a_lo, rwkv_p,
     gate_logits) = _split(h @ w_in, IN_SPLITS)
    y_a = pool_mixer(pool_v, pool_w, pool_scale)
    y_b = dilated_attention(att_q, att_k, att_v)
    y_c = gla_mixer(gla_q, gla_k, gla_v, gla_g, gla_a_lo, gla_w_alpha, gla_b_alpha, gla_norm_g)
    y_d = rwkv7_mixer(rwkv_p, rwkv_mu, rwkv_w0, rwkv_w2, rwkv_a0, rwkv_a2, rwkv_g2,
                      rwkv_k_k, rwkv_k_a, rwkv_r_k, rwkv_ln_g, rwkv_ln_b)
    gates = jax.nn.sigmoid(gate_logits.reshape(B, S, N_BRANCHES, D))
    merged = (gates[:, :, 0] * (y_a @ w_branch_a) + gates[:, :, 1] * (y_b @ w_branch_b)
              + gates[:, :, 2] * (y_c @ w_branch_c) + gates[:, :, 3] * (y_d @ w_branch_d))
    return merged @ w_out


def routed_ffn(h, router_w, router_b, w_gate_up, b_gate_up, w_down, b_down):
    T, D = h.shape
    logits = (h @ router_w + router_b).astype(jnp.float32)
    top_val, top_idx = lax.top_k(logits, TOP_K)
    weights = jax.nn.softmax(top_val, axis=-1).astype(h.dtype)
    n_pairs = T * TOP_K
    flat_e = top_idx.reshape(-1)
    flat_tok = jnp.arange(n_pairs, dtype=jnp.int32) // TOP_K
    flat_w = weights.reshape(-1)
    order = jnp.argsort(flat_e)
    sorted_e = flat_e[order]
    counts = jnp.bincount(flat_e, length=N_EXPERTS)
    start = jnp.cumsum(counts) - counts
    padded = ((counts + MOE_BLOCK - 1) // MOE_BLOCK) * MOE_BLOCK
    pend = jnp.cumsum(padded)
    pstart = pend - padded
    dest = pstart[sorted_e] + (jnp.arange(n_pairs) - start[sorted_e])
    n_blocks = -(-n_pairs // MOE_BLOCK) + N_EXPERTS
    rows = n_blocks * MOE_BLOCK
    slot_tok = jnp.full((rows,), T, jnp.int32).at[dest].set(flat_tok[order])
    slot_w = jnp.zeros((rows,), h.dtype).at[dest].set(flat_w[order])
    block_e = jnp.minimum(jnp.searchsorted(pend, jnp.arange(n_blocks) * MOE_BLOCK, side='right'),
                          N_EXPERTS - 1)
    h_pad = jnp.concatenate([h, jnp.zeros((1, D), h.dtype)], axis=0)
    xb = h_pad[slot_tok].reshape(n_blocks, MOE_BLOCK, D)

    def expert_block(args):
        xblk, e = args
        gu = xblk @ w_gate_up[e] + b_gate_up[e]
        glu = jnp.minimum(gu[:, :EXPERT_FF], SWIGLU_LIMIT)
        lin = jnp.clip(gu[:, EXPERT_FF:], -SWIGLU_LIMIT, SWIGLU_LIMIT)
        act = glu * jax.nn.sigmoid(SWIGLU_ALPHA * glu) * (lin + 1.0)
        return act @ w_down[e] + b_down[e]

    yb = lax.map(expert_block, (xb, block_e))
    y = jnp.zeros((T + 1, D), h.dtype).at[slot_tok].add(yb.reshape(rows, D) * slot_w[:, None])
    return y[:T]


def setup_inputs(seed: int = 0) -> dict:
    key = jax.random.key(seed)
    keys = iter(jax.random.split(key, 64))
    f32 = jnp.float32
    L, D, E, F = DEPTH, D_MODEL, N_EXPERTS, EXPERT_FF

    def normal(shape, scale):
        return jax.random.normal(next(keys), shape, f32) * scale

    def near_one(shape):
        return 1.0 + normal(shape, 0.02)

    return {
        'x': normal((BATCH, SEQ, D), 1.0),
        'c': normal((BATCH, D), 1.0),
        'ada_w': normal((L, D, 6 * D), 0.5 * D ** -0.5),
        'ada_b': normal((L, 6 * D), 0.02),
        'w_in': normal((L, D, D_IN), D ** -0.5),
        'pool_w': normal((L, len(POOL_WINDOWS), POOL_GROUP, POOL_GROUP), POOL_GROUP ** -0.5),
        'pool_scale': near_one((L, POOL_WIDTH)),
        'gla_w_alpha': normal((L, GLA_GATE_RANK, GLA_KEY_WIDTH), GLA_GATE_RANK ** -0.5),
        'gla_b_alpha': normal((L, GLA_KEY_WIDTH), 0.1),
        'gla_norm_g': near_one((L, GLA_VAL_WIDTH)),
        'rwkv_mu': jax.random.uniform(next(keys), (L, RWKV_SHIFT_WIDTH), f32),
        'rwkv_w0': jnp.linspace(-6.0, 1.0, RWKV_WIDTH, dtype=f32) + normal((L, RWKV_WIDTH), 0.1),
        'rwkv_w2': normal((L, RWKV_DECAY_RANK, RWKV_WIDTH), RWKV_DECAY_RANK ** -0.5),
        'rwkv_a0': normal((L, RWKV_WIDTH), 0.1),
        'rwkv_a2': normal((L, RWKV_AAA_RANK, RWKV_WIDTH), RWKV_AAA_RANK ** -0.5),
        'rwkv_g2': normal((L, RWKV_GATE_RANK, RWKV_WIDTH), RWKV_GATE_RANK ** -0.5),
        'rwkv_k_k': 0.85 + normal((L, RWKV_WIDTH), 0.02),
        'rwkv_k_a': near_one((L, RWKV_WIDTH)),
        'rwkv_r_k': normal((L, RWKV_HEADS, RWKV_HEAD_DIM), 0.1),
        'rwkv_ln_g': near_one((L, RWKV_WIDTH)),
        'rwkv_ln_b': normal((L, RWKV_WIDTH), 0.02),
        'w_branch_a': normal((L, POOL_WIDTH, D), POOL_WIDTH ** -0.5),
        'w_branch_b': normal((L, ATT_OUT_WIDTH, D), ATT_OUT_WIDTH ** -0.5),
        'w_branch_c': normal((L, GLA_VAL_WIDTH, D), GLA_VAL_WIDTH ** -0.5),
        'w_branch_d': normal((L, RWKV_WIDTH, D), RWKV_WIDTH ** -0.5),
        'w_out': normal((L, D, D), DEEPNORM_BETA * D ** -0.5),
        'ln1_g': near_one((L, D)),
        'ln1_b': normal((L, D), 0.02),
        'router_w': normal((L, D, E), D ** -0.5),
        'router_b': normal((L, E), 0.01),
        'w_gate_up': normal((L, E, D, 2 * F), D ** -0.5),
        'b_gate_up': normal((L, E, 2 * F), 0.02),
        'w_down': normal((L, E, F, D), DEEPNORM_BETA * F ** -0.5),
        'b_down': normal((L, E, D), 0.02),
        'ln2_g': near_one((L, D)),
        'ln2_b': normal((L, D), 0.02),
    }


def reference(x, c, ada_w, ada_b, w_in, pool_w, pool_scale, gla_w_alpha, gla_b_alpha,
              gla_norm_g, rwkv_mu, rwkv_w0, rwkv_w2, rwkv_a0, rwkv_a2, rwkv_g2, rwkv_k_k,
              rwkv_k_a, rwkv_r_k, rwkv_ln_g, rwkv_ln_b, w_branch_a, w_branch_b, w_branch_c,
              w_branch_d, w_out, ln1_g, ln1_b, router_w, router_b, w_gate_up, b_gate_up,
              w_down, b_down, ln2_g, ln2_b):
    B, S, D = x.shape
    for l in range(DEPTH):
        mod = jax.nn.silu(c) @ ada_w[l] + ada_b[l]
        shift1, scale1, gate1, shift2, scale2, gate2 = jnp.split(mod[:, None, :], 6, axis=-1)
        h = x * (1.0 + scale1) + shift1
        y = hybrid_mixer(h, w_in[l], pool_w[l], pool_scale[l], gla_w_alpha[l], gla_b_alpha[l],
                         gla_norm_g[l], rwkv_mu[l], rwkv_w0[l], rwkv_w2[l], rwkv_a0[l],
                         rwkv_a2[l], rwkv_g2[l], rwkv_k_k[l], rwkv_k_a[l], rwkv_r_k[l],
                         rwkv_ln_g[l], rwkv_ln_b[l], w_branch_a[l], w_branch_b[l],
                         w_branch_c[l], w_branch_d[l], w_out[l])
        x = layer_norm(DEEPNORM_ALPHA * x + gate1 * y, ln1_g[l], ln1_b[l])
        h = x * (1.0 + scale2) + shift2
        y = routed_ffn(h.reshape(B * S, D), router_w[l], router_b[l], w_gate_up[l],
                       b_gate_up[l], w_down[l], b_down[l]).reshape(B, S, D)
        x = layer_norm(DEEPNORM_ALPHA * x + gate2 * y, ln2_g[l], ln2_b[l])
    return x
```

```python
import contextlib
import numpy as np
import concourse.bass as bass
import concourse.mybir as mybir

F32 = mybir.dt.float32
BF16 = mybir.dt.bfloat16
I32 = mybir.dt.int32
U32 = mybir.dt.uint32
ALU = mybir.AluOpType
AF = mybir.ActivationFunctionType
AX = mybir.AxisListType

ENGINES = ("pe", "act", "dve", "pool", "sp")


class Region:
    __slots__ = ("name", "last_w", "readers", "sem", "dma_count")

    def __init__(self, name):
        self.name = name
        self.last_w = None
        self.readers = []
        self.sem = None
        self.dma_count = 0


class Instr:
    __slots__ = ("eng", "fn", "deps", "needed", "token", "is_dma", "home", "seq", "dma_val")

    def __init__(self, eng, fn):
        self.eng = eng
        self.fn = fn
        self.deps = []
        self.needed = False
        self.token = None
        self.is_dma = False
        self.home = None
        self.seq = 0
        self.dma_val = 0


class Prog:
    def __init__(self, nc):
        self.nc = nc
        self.es = contextlib.ExitStack()
        self.q = {e: [] for e in ENGINES}
        self.sems = {}
        self.nreg = 0
        self.out_dmas = []
        self.n_sem = 0

    def sbuf(self, name, shape, dt):
        return self.es.enter_context(self.nc.sbuf_tensor(name, list(shape), dt))

    def psum(self, name, shape, dt=F32):
        return self.es.enter_context(self.nc.psum_tensor(name, list(shape), dt))

    def region(self, name=None):
        self.nreg += 1
        return Region(name or f"r{self.nreg}")

    def regions(self, n, name="r"):
        return [self.region(f"{name}{i}") for i in range(n)]

    def _sem(self, name):
        self.n_sem += 1
        return self.es.enter_context(self.nc.semaphore(name))

    def _track(self, ins, reads, writes):
        deps = []
        for r in reads:
            if r.last_w is not None:
                deps.append(r.last_w)
        for w in writes:
            if w.last_w is not None:
                deps.append(w.last_w)
            deps.extend(w.readers)
        seen = set()
        for d in deps:
            if d is ins or id(d) in seen:
                continue
            seen.add(id(d))
            if d.eng == "pe" and ins.eng == "pe" and not d.is_dma and not ins.is_dma:
                continue
            ins.deps.append(d)
            d.needed = True
        for r in reads:
            r.readers.append(ins)
        for w in writes:
            w.last_w = ins
            w.readers = []

    def op(self, eng, fn, reads=(), writes=()):
        ins = Instr(eng, fn)
        self._track(ins, list(reads), list(writes))
        self.q[eng].append(ins)
        return ins

    def dma(self, eng, out, in_, reads=(), writes=(), home=None, is_output=False, **kw):
        reads = list(reads)
        writes = list(writes)
        if home is None:
            home = writes[0] if writes else reads[0]
        if home.sem is None:
            home.sem = self._sem("d_" + home.name)
        home.dma_count += 1
        val = 16 * home.dma_count

        def fn(e, out=out, in_=in_, kw=kw):
            return e.dma_start(out=out, in_=in_, **kw)

        ins = Instr(eng, fn)
        ins.is_dma = True
        ins.home = home
        ins.dma_val = val
        ins.token = (home.sem, val)
        self._track(ins, reads, writes)
        self.q[eng].append(ins)
        if is_output:
            self.out_dmas.append(ins)
        return ins

    def emit(self):
        nc = self.nc
        for d in self.out_dmas:
            d.needed = True
        fin = Instr("sp", None)
        fin.deps = list(self.out_dmas)
        for e in ENGINES:
            if self.q[e]:
                last = self.q[e][-1]
                if last is not fin and not last.is_dma:
                    last.needed = True
                    fin.deps.append(last)
        self.q["sp"].append(fin)
        for e in ENGINES:
            c = 0
            sem = None
            for ins in self.q[e]:
                if ins.is_dma or ins.fn is None:
                    continue
                if ins.needed:
                    if sem is None:
                        sem = self._sem("e_" + e)
                    c += 1
                    ins.token = (sem, c)
        block = self.es.enter_context(nc.Block())

        def run(e_name, eng):
            known = {}
            for ins in self.q[e_name]:
                need = {}
                for d in ins.deps:
                    s, v = d.token
                    if known.get(s.num, 0) >= v:
                        continue
                    if need.get(s.num, (None, 0))[1] < v:
                        need[s.num] = (s, v)
                for s, v in need.values():
                    eng.wait_ge(s, v)
                    known[s.num] = v
                if ins.fn is None:
                    continue
                r = ins.fn(eng)
                if ins.is_dma:
                    r.then_inc(ins.token[0], 16)
                elif ins.needed:
                    r.then_inc(ins.token[0], 1)

        if self.q["pe"]:
            @block.tensor
            def _(eng):
                run("pe", eng)
        if self.q["act"]:
            @block.scalar
            def _(eng):
                run("act", eng)
        if self.q["dve"]:
            @block.vector
            def _(eng):
                run("dve", eng)
        if self.q["pool"]:
            @block.gpsimd
            def _(eng):
                run("pool", eng)

        @block.sync
        def _(eng):
            run("sp", eng)

        self.es.close()


D = 2048
KC = 16


def build_p0(ncols=3072):
    nc = bass.Bass("TRN2", target_bir_lowering=False)
    cT_d = nc.dram_tensor("cT", [128, KC, 2], F32, kind="ExternalInput").ap()
    w_d = nc.dram_tensor("w", [D, ncols], F32, kind="ExternalInput").ap()
    b_d = nc.dram_tensor("b", [1, ncols], F32, kind="ExternalInput").ap()
    o_d = nc.dram_tensor("o", [2, ncols], F32, kind="ExternalOutput").ap()
    p = Prog(nc)
    cT = p.sbuf("cT_s", [128, KC, 2], F32)
    sc = p.sbuf("sc_s", [128, KC, 2], F32)
    bias = p.sbuf("bias_s", [1, ncols], F32)
    ones = p.sbuf("ones_s", [1, 2], F32)
    osb = p.sbuf("o_s", [2, ncols], F32)
    nt = ncols // 512
    wt = [p.sbuf(f"wt{i}", [128, KC, 512], F32) for i in range(2)]
    ps = [p.psum(f"ps{i}", [128, 512]) for i in range(2)]
    r_c, r_sc, r_b, r_ones, r_o = p.regions(5, "m")
    r_w = p.regions(2, "w")
    r_ps = p.regions(2, "ps")
    p.dma("sp", cT[:], cT_d, writes=[r_c])
    p.dma("sp", bias[:], b_d, writes=[r_b])
    p.op("dve", lambda e: e.memset(ones[:], 1.0), writes=[r_ones])
    p.op("act", lambda e: e.activation(out=sc[:], in_=cT[:], func=AF.Silu), reads=[r_c], writes=[r_sc])
    for t in range(nt):
        i = t % 2
        p.dma("sp", wt[i][:], w_d[:, t * 512:(t + 1) * 512].rearrange("(k p) n -> p k n", p=128), writes=[r_w[i]])
        for kc in range(KC):
            p.op("pe", lambda e, kc=kc, i=i: e.matmul(ps[i][0:2, :], lhsT=sc[:, kc, :], rhs=wt[i][:, kc, :],
                                                        start=(kc == 0), stop=False),
                 reads=[r_sc, r_w[i]], writes=[r_ps[i]])
        p.op("pe", lambda e, i=i, t=t: e.matmul(ps[i][0:2, :], lhsT=ones[:], rhs=bias[:, t * 512:(t + 1) * 512],
                                                  start=False, stop=True),
             reads=[r_ones, r_b], writes=[r_ps[i]])
        p.op("dve", lambda e, i=i, t=t: e.tensor_copy(out=osb[:, t * 512:(t + 1) * 512], in_=ps[i][0:2, :]),
             reads=[r_ps[i]], writes=[r_o])
    p.dma("sp", o_d, osb[:], reads=[r_o], is_output=True)
    p.emit()
    return nc


def build_a(ncols, ntok=1024):
    nc = bass.Bass("TRN2", target_bir_lowering=False)
    xT_d = nc.dram_tensor("xT", [D, ntok], F32, kind="ExternalInput").ap()
    sc_d = nc.dram_tensor("sc", [128, KC], F32, kind="ExternalInput").ap()
    sh_d = nc.dram_tensor("sh", [128, KC], F32, kind="ExternalInput").ap()
    w_d = nc.dram_tensor("w", [D, ncols], F32, kind="ExternalInput").ap()
    o_d = nc.dram_tensor("o", [ncols, ntok], F32, kind="ExternalOutput").ap()
    p = Prog(nc)
    emit_a(p, xT_d, sc_d, sh_d, w_d, o_d, ncols, ntok)
    p.emit()
    return nc


def emit_modulate(p, xT_d, sc_d, sh_d, ntok, name="m"):
    sc = p.sbuf(name + "sc", [128, KC], F32)
    sh = p.sbuf(name + "sh", [128, KC], F32)
    hT = p.sbuf(name + "hT", [128, KC, ntok], BF16)
    xs = [p.sbuf(f"{name}xs{i}", [128, 4, ntok], F32) for i in range(2)]
    r_sc = p.region(name + "sc")
    r_xs = p.regions(2, name + "xs")
    r_h = p.regions(KC, name + "h")
    p.dma("sp", sc[:], sc_d, writes=[r_sc])
    p.dma("sp", sh[:], sh_d, writes=[r_sc])
    p.op("dve", lambda e: e.tensor_scalar_add(out=sc[:], in0=sc[:], scalar1=1.0), reads=[r_sc], writes=[r_sc])
    for g in range(KC // 4):
        i = g % 2
        p.dma("sp", xs[i][:], xT_d[g * 512:(g + 1) * 512, :].rearrange("(k p) t -> p k t", p=128), writes=[r_xs[i]])
        for j in range(4):
            kc = g * 4 + j
            p.op("act", lambda e, i=i, j=j, kc=kc: e.activation(out=hT[:, kc, :], in_=xs[i][:, j, :], func=AF.Identity,
                                                                bias=sh[:, kc:kc + 1], scale=sc[:, kc:kc + 1]),
                 reads=[r_xs[i], r_sc], writes=[r_h[kc]])
    return hT, r_h


def emit_a(p, xT_d, sc_d, sh_d, w_d, o_d, ncols, ntok):
    hT, r_h = emit_modulate(p, xT_d, sc_d, sh_d, ntok)
    ws = [p.sbuf(f"ws{i}", [128, KC, 512], F32) for i in range(2)]
    wb = [p.sbuf(f"wb{i}", [128, KC, 512], BF16) for i in range(2)]
    ob = [p.sbuf(f"ob{i}", [128, 4, 512], F32) for i in range(2)]
    ps = [p.psum(f"ps{i}", [128, 512]) for i in range(4)]
    r_ws = p.regions(2, "ws")
    r_wb = p.regions(2, "wb")
    r_ob = [p.regions(4, f"ob{i}_") for i in range(2)]
    r_ps = p.regions(4, "ps")
    nct = (ncols + 511) // 512
    ntb = ntok // 512
    cnt = 0
    oi = 0
    for ct in range(nct):
        i = ct % 2
        c0 = ct * 512
        cw = min(512, ncols - c0)
        p.dma("sp", ws[i][:, :, 0:cw], w_d[:, c0:c0 + cw].rearrange("(k p) n -> p k n", p=128), writes=[r_ws[i]])
        ceng = "dve" if ct % 2 == 0 else "pool"
        p.op(ceng, lambda e, i=i, cw=cw: e.tensor_copy(out=wb[i][:, :, 0:cw], in_=ws[i][:, :, 0:cw]),
             reads=[r_ws[i]], writes=[r_wb[i]])
        nsub = (cw + 127) // 128
        for tb in range(ntb):
            o = oi % 2
            oi += 1
            for s in range(nsub):
                m = min(128, cw - s * 128)
                b = cnt % 4
                cnt += 1
                for kc in range(KC):
                    p.op("pe", lambda e, b=b, i=i, s=s, m=m, kc=kc, tb=tb: e.matmul(
                        ps[b][0:m, :], lhsT=wb[i][:, kc, s * 128:s * 128 + m], rhs=hT[:, kc, tb * 512:(tb + 1) * 512],
                        start=(kc == 0), stop=(kc == KC - 1)),
                        reads=[r_wb[i], r_h[kc]], writes=[r_ps[b]])
                if cnt % 2 == 0:
                    p.op("act", lambda e, o=o, s=s, m=m, b=b: e.copy(out=ob[o][0:m, s, :], in_=ps[b][0:m, :]),
                         reads=[r_ps[b]], writes=[r_ob[o][s]])
                else:
                    p.op("dve", lambda e, o=o, s=s, m=m, b=b: e.tensor_copy(out=ob[o][0:m, s, :], in_=ps[b][0:m, :]),
                         reads=[r_ps[b]], writes=[r_ob[o][s]])
            nfull = cw // 128
            if nfull:
                p.dma("sp", o_d[c0:c0 + nfull * 128, tb * 512:(tb + 1) * 512].rearrange("(s p) t -> p s t", p=128),
                      ob[o][:, 0:nfull, :], reads=r_ob[o][0:nfull], is_output=True)
            rem = cw - nfull * 128
            if rem:
                p.dma("sp", o_d[c0 + nfull * 128:c0 + cw, tb * 512:(tb + 1) * 512],
                      ob[o][0:rem, nfull, :], reads=[r_ob[o][nfull]], is_output=True)


HALO = 16


def build_pool(ntok=1024):
    nc = bass.Bass("TRN2", target_bir_lowering=False)
    W = ntok + HALO
    v_d = nc.dram_tensor("v", [4, 128, W], F32, kind="ExternalInput").ap()
    inv_d = nc.dram_tensor("inv", [4, 128, ntok], F32, kind="ExternalInput").ap()
    pw_d = nc.dram_tensor("pw", [4, 128, 128], F32, kind="ExternalInput").ap()
    psc_d = nc.dram_tensor("psc", [128, 4], F32, kind="ExternalInput").ap()
    o_d = nc.dram_tensor("o", [4, 128, ntok], F32, kind="ExternalOutput").ap()
    p = Prog(nc)
    v = p.sbuf("v_s", [128, 4, W], F32)
    inv = p.sbuf("inv_s", [128, 4, ntok], F32)
    pw = p.sbuf("pw_s", [128, 4, 128], F32)
    psc = p.sbuf("psc_s", [128, 4], F32)
    ta = p.sbuf("ta", [128, 4, W], F32)
    tb = p.sbuf("tb", [128, 4, W], F32)
    df = p.sbuf("df", [128, 4, ntok], F32)
    ob = p.sbuf("ob", [128, 4, ntok], F32)
    ps = [p.psum(f"ps{i}", [128, 512]) for i in range(4)]
    r_v, r_inv, r_pw = p.regions(3, "in")
    r_ta = p.regions(4, "ta")
    r_tb = p.regions(4, "tb")
    r_df = p.regions(4, "df")
    r_ob = p.regions(4, "ob")
    r_ps = p.regions(4, "ps")
    p.dma("sp", v[:], v_d.rearrange("g p t -> p g t"), writes=[r_v])
    p.dma("sp", inv[:], inv_d.rearrange("g p t -> p g t"), writes=[r_inv])
    p.dma("sp", pw[:], pw_d.rearrange("g p t -> p g t"), writes=[r_pw])
    p.dma("sp", psc[:], psc_d, writes=[r_pw])
    p.op("pool", lambda e: e.memset(ta[:], 0.0), writes=r_ta)
    p.op("pool", lambda e: e.memset(tb[:], 0.0), writes=r_tb)
    cnt = 0
    for g in range(4):
        src, r_src = v, r_v
        bufs = [(ta, r_ta[g]), (tb, r_tb[g])]
        for j in range(g + 1):
            m = 1 << j
            dst, r_dst = bufs[j % 2]
            p.op("dve", lambda e, dst=dst, src=src, g=g, m=m: e.tensor_tensor(
                out=dst[:, g, m:W], in0=src[:, g, m:W], in1=src[:, g, 0:W - m], op=ALU.add),
                reads=[r_src], writes=[r_dst])
            src, r_src = dst, r_dst
        p.op("dve", lambda e, src=src, g=g: e.tensor_tensor(out=df[:, g, :], in0=src[:, g, HALO:W], in1=inv[:, g, :], op=ALU.mult),
             reads=[r_src, r_inv], writes=[r_df[g]])
        p.op("dve", lambda e, g=g: e.tensor_tensor(out=df[:, g, :], in0=df[:, g, :], in1=v[:, g, HALO:W], op=ALU.subtract),
             reads=[r_df[g], r_v], writes=[r_df[g]])
        for tbk in range(ntok // 512):
            b = cnt % 4
            cnt += 1
            p.op("pe", lambda e, b=b, g=g, tbk=tbk: e.matmul(ps[b][:, :], lhsT=pw[:, g, :], rhs=df[:, g, tbk * 512:(tbk + 1) * 512],
                                                             start=True, stop=True),
                 reads=[r_pw, r_df[g]], writes=[r_ps[b]])
            p.op("act", lambda e, b=b, g=g, tbk=tbk: e.activation(out=ob[:, g, tbk * 512:(tbk + 1) * 512], in_=ps[b][:, :],
                                                                  func=AF.Copy, scale=psc[:, g:g + 1]),
                 reads=[r_ps[b], r_pw], writes=[r_ob[g]])
    p.dma("sp", o_d.rearrange("g p t -> p g t"), ob[:], reads=r_ob, is_output=True)
    p.emit()
    return nc


NBLK = 32
ATT_GROUPS = ((128, 1), (512, 4), (2048, 16))


def build_att():
    nc = bass.Bass("TRN2", target_bir_lowering=False)
    S = 4096
    q_d = nc.dram_tensor("qT", [3, 64, S], F32, kind="ExternalInput").ap()
    k_d = nc.dram_tensor("kT", [3, 64, S], F32, kind="ExternalInput").ap()
    v_d = nc.dram_tensor("v", [3, S, 64], F32, kind="ExternalInput").ap()
    bias_d = nc.dram_tensor("bias", [3, 128, 256], F32, kind="ExternalInput").ap()
    o_d = nc.dram_tensor("o", [3, NBLK, 128, 65], F32, kind="ExternalOutput").ap()
    p = Prog(nc)
    qT = p.sbuf("qT_s", [64, 3, S], F32)
    kT = p.sbuf("kT_s", [64, 3, S], F32)
    vx = p.sbuf("vx", [128, 3, NBLK, 65], F32)
    bias = p.sbuf("bias_s", [128, 3, 256], F32)
    oall = p.sbuf("oall", [128, 3, NBLK, 65], F32)
    sc = [p.sbuf(f"sc{i}", [128, 256], F32) for i in range(2)]
    pT = [p.sbuf(f"pT{i}", [128, 256], F32) for i in range(2)]
    ps_s = [p.psum(f"pss{i}", [128, 512]) for i in range(2)]
    ps_o = [p.psum(f"pso{i}", [128, 512]) for i in range(2)]
    r_q = p.regions(3, "q")
    r_k = p.regions(3, "k")
    r_v = p.regions(3, "v")
    r_bias = p.region("bias")
    r_sc = p.regions(2, "sc")
    r_pT = p.regions(2, "pT")
    r_pss = p.regions(2, "pss")
    r_pso = p.regions(2, "pso")
    r_o = [p.regions(NBLK, f"o{g}_") for g in range(3)]
    p.dma("sp", bias[:], bias_d.rearrange("g p k -> p g k"), writes=[r_bias])
    p.op("pool", lambda e: e.memset(vx[:, :, :, 64:65], 1.0), writes=r_v)
    for g in range(3):
        p.dma("sp", qT[:, g, :], q_d[g], writes=[r_q[g]])
        p.dma("sp", kT[:, g, :], k_d[g], writes=[r_k[g]])
        p.dma("sp", vx[:, g, :, 0:64], v_d[g].rearrange("(n p) c -> p n c", p=128), writes=[r_v[g]])
    cnt = 0
    for g, (window, dil) in enumerate(ATT_GROUPS):
        bpr = NBLK // dil
        for blk in range(NBLK):
            i = cnt % 2
            cnt += 1
            has_prev = (blk % bpr) != 0
            k0 = 0 if has_prev else 1
            for kb in range(k0, 2):
                kblk = blk - 1 + kb
                p.op("pe", lambda e, i=i, g=g, kb=kb, kblk=kblk, blk=blk: e.matmul(
                    ps_s[i][:, kb * 128:(kb + 1) * 128], lhsT=kT[:, g, kblk * 128:(kblk + 1) * 128],
                    rhs=qT[:, g, blk * 128:(blk + 1) * 128], start=True, stop=True),
                    reads=[r_k[g], r_q[g]], writes=[r_pss[i]])
            lo = k0 * 128
            p.op("dve", lambda e, i=i, g=g, lo=lo: e.scalar_tensor_tensor(
                out=sc[i][:, lo:256], in0=ps_s[i][:, lo:256], scalar=0.125, in1=bias[:, g, lo:256],
                op0=ALU.mult, op1=ALU.add),
                reads=[r_pss[i], r_bias], writes=[r_sc[i]])
            p.op("act", lambda e, i=i, lo=lo: e.activation(out=pT[i][:, lo:256], in_=sc[i][:, lo:256], func=AF.Exp),
                 reads=[r_sc[i]], writes=[r_pT[i]])
            for kb in range(k0, 2):
                kblk = blk - 1 + kb
                p.op("pe", lambda e, i=i, g=g, kb=kb, kblk=kblk, k0=k0: e.matmul(
                    ps_o[i][:, 0:65], lhsT=pT[i][:, kb * 128:(kb + 1) * 128], rhs=vx[:, g, kblk, :],
                    start=(kb == k0), stop=(kb == 1)),
                    reads=[r_pT[i], r_v[g]], writes=[r_pso[i]])
            p.op("act", lambda e, i=i, g=g, blk=blk: e.copy(out=oall[:, g, blk, :], in_=ps_o[i][:, 0:65]),
                 reads=[r_pso[i]], writes=[r_o[g][blk]])
        p.dma("sp", o_d[g].rearrange("n p c -> p n c"), oall[:, g, :, :], reads=r_o[g], is_output=True)
    p.emit()
    return nc


GC = 128
GN = 32


def build_gla():
    nc = bass.Bass("TRN2", target_bir_lowering=False)
    S = 4096
    q_d = nc.dram_tensor("qT", [64, S], F32, kind="ExternalInput").ap()
    k_d = nc.dram_tensor("kT", [64, S], F32, kind="ExternalInput").ap()
    v_d = nc.dram_tensor("v", [S, 128], F32, kind="ExternalInput").ap()
    g_d = nc.dram_tensor("g", [S, 128], F32, kind="ExternalInput").ap()
    alo_d = nc.dram_tensor("aloT", [16, S], F32, kind="ExternalInput").ap()
    wa_d = nc.dram_tensor("wa", [16, 64], F32, kind="ExternalInput").ap()
    ba_d = nc.dram_tensor("ba", [64, 1], F32, kind="ExternalInput").ap()
    ng_d = nc.dram_tensor("ng", [128, 128], F32, kind="ExternalInput").ap()
    mask_d = nc.dram_tensor("mask", [128, 128], F32, kind="ExternalInput").ap()
    id_d = nc.dram_tensor("ident", [64, 64], F32, kind="ExternalInput").ap()
    o_d = nc.dram_tensor("o", [S, 128], F32, kind="ExternalOutput").ap()
    p = Prog(nc)
    qT = p.sbuf("qT_s", [64, GN, GC], F32)
    kT = p.sbuf("kT_s", [64, GN, GC], F32)
    bA = p.sbuf("bA", [64, GN, GC], F32)
    bB = p.sbuf("bB", [64, GN, GC], F32)
    bC = p.sbuf("bC", [64, GN, GC], F32)
    V = p.sbuf("V", [128, GN, 128], F32)
    G = p.sbuf("G", [128, GN, 128], F32)
    alo = p.sbuf("alo", [16, S], F32)
    wa = p.sbuf("wa_s", [16, 64], F32)
    ba = p.sbuf("ba_s", [64, 1], F32)
    ng = p.sbuf("ng_s", [128, 128], F32)
    mask = p.sbuf("mask_s", [128, 128], F32)
    ident = p.sbuf("ident_s", [64, 64], F32)
    kv = p.sbuf("kv", [64, GN, 128], F32)
    sall = p.sbuf("sall", [64, GN, 128], F32)
    at = [p.sbuf(f"at{i}", [128, 128], F32) for i in range(2)]
    kt = [p.sbuf(f"kt{i}", [128, 64], F32) for i in range(2)]
    ss = [p.sbuf(f"ss{i}", [128, 1], F32) for i in range(2)]
    sq = p.sbuf("sq", [128, 128], F32)
    eps = p.sbuf("eps", [128, 1], F32)
    psa = [p.psum(f"psa{i}", [128, 512]) for i in range(2)]
    psb = [p.psum(f"psb{i}", [128, 512]) for i in range(2)]
    psc = [p.psum(f"psc{i}", [128, 512]) for i in range(2)]
    pso = [p.psum(f"pso{i}", [128, 512]) for i in range(2)]
    R = p.region
    r_q, r_k, r_v, r_g, r_alo, r_c = R("q"), R("k"), R("v"), R("g"), R("alo"), R("c")
    r_bA, r_bB, r_bC = R("bA"), R("bB"), R("bC")
    r_kv = p.regions(GN, "kv")
    r_sall = p.regions(GN, "sall")
    r_at = p.regions(2, "at")
    r_kt = p.regions(2, "kt")
    r_ss = p.regions(2, "ss")
    r_sq = R("sq")
    r_psa, r_psb, r_psc, r_pso = p.regions(2, "psa"), p.regions(2, "psb"), p.regions(2, "psc"), p.regions(2, "pso")
    r_y = p.regions(GN, "y")
    flat = lambda t: t[:].rearrange("p n c -> p (n c)")
    p.dma("sp", flat(qT), q_d, writes=[r_q])
    p.dma("sp", flat(kT), k_d, writes=[r_k])
    p.dma("sp", alo[:], alo_d, writes=[r_alo])
    for t, d_ in ((wa, wa_d), (ba, ba_d), (ng, ng_d), (mask, mask_d), (ident, id_d)):
        p.dma("sp", t[:], d_, writes=[r_c])
    p.dma("sp", V[:], v_d.rearrange("(n p) c -> p n c", p=128), writes=[r_v])
    p.dma("sp", G[:], g_d.rearrange("(n p) c -> p n c", p=128), writes=r_y + [r_g])
    p.op("dve", lambda e: e.tensor_scalar(out=ba[:], in0=ba[:], scalar1=-1.0, scalar2=None, op0=ALU.mult), reads=[r_c], writes=[r_c])
    p.op("pool", lambda e: e.memset(sall[:, 0, :], 0.0), writes=[r_sall[0]])
    p.op("pool", lambda e: e.memset(eps[:], 1e-6), writes=[r_c])
    fA = flat(bA)
    for tb in range(8):
        i = tb % 2
        sl = slice(tb * 512, (tb + 1) * 512)
        p.op("pe", lambda e, i=i, sl=sl: e.matmul(psa[i][0:64, :], lhsT=wa[:], rhs=alo[:, sl], start=True, stop=True),
             reads=[r_c, r_alo], writes=[r_psa[i]])
        p.op("act", lambda e, i=i, sl=sl: e.activation(out=fA[:, sl], in_=psa[i][0:64, :], func=AF.Exp, bias=ba[:], scale=-1.0),
             reads=[r_psa[i], r_c], writes=[r_bA])
    p.op("act", lambda e: e.activation(out=fA, in_=fA, func=AF.Ln, bias=1.0, scale=1.0), reads=[r_bA], writes=[r_bA])
    p.op("dve", lambda e: e.tensor_scalar(out=fA, in0=fA, scalar1=-1.0 / 16.0, scalar2=None, op0=ALU.mult), reads=[r_bA], writes=[r_bA])
    src, r_src, dst, r_dst = bA, r_bA, bB, r_bB
    m = 1
    while m < GC:
        p.op("dve", lambda e, src=src, dst=dst, m=m: e.tensor_tensor(out=dst[:, :, m:GC], in0=src[:, :, m:GC], in1=src[:, :, 0:GC - m], op=ALU.add),
             reads=[r_src], writes=[r_dst])
        p.op("pool", lambda e, src=src, dst=dst, m=m: e.tensor_copy(out=dst[:, :, 0:m], in_=src[:, :, 0:m]),
             reads=[r_src], writes=[r_dst])
        src, r_src, dst, r_dst = dst, r_dst, src, r_src
        m *= 2
    bb, r_bb, oth, r_oth = src, r_src, dst, r_dst
    p.op("act", lambda e: e.activation(out=flat(bC), in_=flat(bb), func=AF.Exp), reads=[r_bb], writes=[r_bC])
    p.op("act", lambda e: e.activation(out=flat(oth), in_=flat(bb), func=AF.Exp, scale=-1.0), reads=[r_bb], writes=[r_oth])
    p.op("dve", lambda e: e.scalar_tensor_tensor(out=flat(qT), in0=flat(qT), scalar=0.125, in1=flat(bC), op0=ALU.mult, op1=ALU.mult),
         reads=[r_q, r_bC], writes=[r_q])
    p.op("pool", lambda e: e.tensor_tensor(out=flat(kT), in0=flat(kT), in1=flat(oth), op=ALU.mult), reads=[r_k, r_oth], writes=[r_k])
    p.op("dve", lambda e: e.tensor_tensor(out=bb[:], in0=kT[:], in1=bC[:, :, GC - 1:GC].to_broadcast([64, GN, GC]), op=ALU.mult),
         reads=[r_k, r_bC, r_bb], writes=[r_bb])
    kp = bb
    r_kp = r_bb
    p.op("act", lambda e: e.activation(out=G[:], in_=G[:], func=AF.Silu), reads=[r_g], writes=[r_g])
    p.op("pool", lambda e: e.tensor_tensor(out=G[:], in0=G[:], in1=ng[:].unsqueeze(1).to_broadcast([128, GN, 128]), op=ALU.mult),
         reads=[r_g, r_c], writes=[r_g])
    for c in range(GN):
        i = c % 2
        p.op("pe", lambda e, i=i, c=c: e.matmul(psa[i][:, 0:128], lhsT=kT[:, c, :], rhs=qT[:, c, :], start=True, stop=True),
             reads=[r_k, r_q], writes=[r_psa[i]])
        p.op("pe", lambda e, i=i, c=c: e.transpose(psb[i][:, 0:64], kp[:, c, :], ident[:]),
             reads=[r_kp, r_c], writes=[r_psb[i]])
        p.op("act", lambda e, i=i: e.copy(out=kt[i][:], in_=psb[i][:, 0:64]), reads=[r_psb[i]], writes=[r_kt[i]])
        p.op("pe", lambda e, i=i, c=c: e.matmul(psc[i][0:64, 0:128], lhsT=kt[i][:], rhs=V[:, c, :], start=True, stop=True),
             reads=[r_kt[i], r_v], writes=[r_psc[i]])
        p.op("act", lambda e, i=i, c=c: e.copy(out=kv[:, c, :], in_=psc[i][0:64, 0:128]), reads=[r_psc[i]], writes=[r_kv[c]])
        if c + 1 < GN:
            p.op("dve", lambda e, c=c: e.scalar_tensor_tensor(out=sall[:, c + 1, :], in0=sall[:, c, :], scalar=bC[:, c, GC - 1:GC],
                                                               in1=kv[:, c, :], op0=ALU.mult, op1=ALU.add),
                 reads=[r_sall[c], r_bC, r_kv[c]], writes=[r_sall[c + 1]])
        p.op("dve", lambda e, i=i: e.tensor_tensor(out=at[i][:], in0=psa[i][:, 0:128], in1=mask[:], op=ALU.mult),
             reads=[r_psa[i], r_c], writes=[r_at[i]])
        p.op("pe", lambda e, i=i, c=c: e.matmul(pso[i][:, 0:128], lhsT=at[i][:], rhs=V[:, c, :], start=True, stop=False),
             reads=[r_at[i], r_v], writes=[r_pso[i]])
        p.op("pe", lambda e, i=i, c=c: e.matmul(pso[i][:, 0:128], lhsT=qT[:, c, :], rhs=sall[:, c, :], start=False, stop=True),
             reads=[r_q, r_sall[c]], writes=[r_pso[i]])
        p.op("act", lambda e, i=i: e.activation(out=sq[:], in_=pso[i][:, 0:128], func=AF.Square, accum_out=ss[i][:]),
             reads=[r_pso[i]], writes=[r_sq, r_ss[i]])
        p.op("act", lambda e, i=i: e.activation(out=ss[i][:], in_=ss[i][:], func=AF.Sqrt, bias=eps[:], scale=1.0 / 128.0),
             reads=[r_ss[i], r_c], writes=[r_ss[i]])
        p.op("dve", lambda e, i=i: e.reciprocal(out=ss[i][:], in_=ss[i][:]), reads=[r_ss[i]], writes=[r_ss[i]])
        p.op("dve", lambda e, i=i, c=c: e.scalar_tensor_tensor(out=G[:, c, :], in0=pso[i][:, 0:128], scalar=ss[i][:], in1=G[:, c, :],
                                                                op0=ALU.mult, op1=ALU.mult),
             reads=[r_pso[i], r_ss[i], r_g], writes=[r_y[c]])
    p.dma("sp", o_d.rearrange("(n p) c -> p n c", p=128), G[:], reads=r_y, is_output=True)
    p.emit()
    return nc


RS = 4096
RC_ = 64
NPAIR = 32
E05 = 0.6065306597126334


def build_rwkv():
    nc = bass.Bass("TRN2", target_bir_lowering=False)
    S = RS
    W = S + 1
    din = {}
    for nm, shp in (("r", [128, W]), ("k", [128, W]), ("v", [128, W]), ("lo1", [64, W]), ("lo2", [96, W]),
                    ("vtok", [W, 128]), ("pv", [128, 16]), ("mulo1", [64, 1]), ("mulo2", [96, 1]), ("muvt", [128, 128]),
                    ("w2a2", [64, 128]), ("g2", [96, 128]), ("bo", [128, 128]), ("msl", [128, 128]), ("msu", [128, 128]),
                    ("miu", [128, 128]), ("ident", [128, 128])):
        din[nm] = nc.dram_tensor(nm, shp, F32, kind="ExternalInput").ap()
    o_d = nc.dram_tensor("o", [128, S], F32, kind="ExternalOutput").ap()
    p = Prog(nc)
    B = [p.sbuf(f"B{i}", [128, W], F32) for i in range(9)]
    rB = p.regions(9, "B")
    VT = p.sbuf("VT", [128, NPAIR, 128], F32)
    r_VT = p.region("VT")
    small = {}
    r_small = p.region("small")
    for nm in ("pv", "mulo1", "mulo2", "muvt", "w2a2", "g2", "bo", "msl", "msu", "miu", "ident"):
        shp = list(din[nm].shape)
        small[nm] = p.sbuf(nm + "_s", shp, F32)
        p.dma("sp", small[nm][:], din[nm], writes=[r_small])
    pv, bo, msl, msu, miu, ident = (small[n] for n in ("pv", "bo", "msl", "msu", "miu", "ident"))
    p.op("dve", lambda e: e.tensor_scalar(out=pv[:, 10:11], in0=pv[:, 6:7], scalar1=-1.0, scalar2=1.0, op0=ALU.mult, op1=ALU.add),
         reads=[r_small], writes=[r_small])
    p.op("pool", lambda e: e.memset(pv[:, 11:12], 64e-5), writes=[r_small])
    p.op("pool", lambda e: e.memset(pv[:, 12:13], 0.0), writes=[r_small])
    egc = p.sbuf("egc", [128, 64], F32)
    egl = p.sbuf("egl", [64, 2, 64], F32)
    r_egc, r_egl = p.region("egc"), p.region("egl")
    hall = p.sbuf("hall", [64, 2, 4, 64], F32)
    r_hall = [[p.region(f"hall{h}_{c}") for c in range(4)] for h in range(2)]
    ps = [p.psum(f"ps{i}", [128, 512]) for i in range(8)]
    r_ps = p.regions(8, "ps")
    pc = [0]

    def bank():
        i = pc[0] % 8
        pc[0] += 1
        return ps[i], r_ps[i]

    R_, K_, V_, T1, L1, L2, A_, G_, GT = range(9)
    p.dma("sp", B[R_][:], din["r"], writes=[rB[R_]])
    p.dma("sp", B[K_][:], din["k"], writes=[rB[K_]])
    p.dma("sp", B[V_][:], din["v"], writes=[rB[V_]])
    p.dma("sp", B[L1][0:64, :], din["lo1"], writes=[rB[L1]])
    p.dma("sp", B[L2][0:96, :], din["lo2"], writes=[rB[L2]])
    p.dma("sp", VT[:], din["vtok"][1:W, :].rearrange("(n p) c -> p n c", p=128), writes=[r_VT])
    g7 = B[G_][:, 0:S].rearrange("p (n c) -> p n c", c=128)
    p.dma("sp", g7, din["vtok"][0:S, :].rearrange("(n p) c -> p n c", p=128), writes=[rB[G_]])
    muv_b = small["muvt"][:].unsqueeze(1).to_broadcast([128, NPAIR, 128])
    p.op("dve", lambda e: e.tensor_tensor(out=g7, in0=g7, in1=VT[:], op=ALU.subtract), reads=[rB[G_], r_VT], writes=[rB[G_]])
    p.op("pool", lambda e: e.tensor_tensor(out=g7, in0=g7, in1=muv_b, op=ALU.mult), reads=[rB[G_], r_small], writes=[rB[G_]])
    p.op("dve", lambda e: e.tensor_tensor(out=VT[:], in0=VT[:], in1=g7, op=ALU.add), reads=[rB[G_], r_VT], writes=[r_VT])

    def shift(bi, np_, mu_ap):
        p.op("dve", lambda e: e.tensor_tensor(out=B[T1][0:np_, 1:W], in0=B[bi][0:np_, 0:S], in1=B[bi][0:np_, 1:W], op=ALU.subtract),
             reads=[rB[bi]], writes=[rB[T1]])
        p.op("dve", lambda e: e.scalar_tensor_tensor(out=B[bi][0:np_, 1:W], in0=B[T1][0:np_, 1:W], scalar=mu_ap, in1=B[bi][0:np_, 1:W],
                                                      op0=ALU.mult, op1=ALU.add),
             reads=[rB[T1], rB[bi], r_small], writes=[rB[bi]])

    shift(R_, 128, pv[:, 0:1])
    shift(K_, 128, pv[:, 1:2])
    shift(V_, 128, pv[:, 2:3])
    shift(L1, 64, small["mulo1"][:])
    shift(L2, 96, small["mulo2"][:])
    v1 = lambda bi: B[bi][:, 1:W]
    p.op("act", lambda e: e.activation(out=B[L1][0:32, 1:W], in_=B[L1][0:32, 1:W], func=AF.Tanh), reads=[rB[L1]], writes=[rB[L1]])
    p.op("act", lambda e: e.activation(out=B[L2][0:96, 1:W], in_=B[L2][0:96, 1:W], func=AF.Sigmoid), reads=[rB[L2]], writes=[rB[L2]])
    w2a2 = small["w2a2"]
    for tb in range(8):
        sl = slice(1 + tb * 512, 1 + (tb + 1) * 512)
        pt, rp = bank()
        p.op("pe", lambda e, pt=pt, sl=sl: e.matmul(pt[:, :], lhsT=w2a2[0:32, :], rhs=B[L1][0:32, sl], start=True, stop=True),
             reads=[r_small, rB[L1]], writes=[rp])
        p.op("act", lambda e, pt=pt, sl=sl: e.activation(out=B[G_][:, sl], in_=pt[:, :], func=AF.Sigmoid, bias=pv[:, 3:4], scale=1.0),
             reads=[rp, r_small], writes=[rB[G_]])
        pt, rp = bank()
        p.op("pe", lambda e, pt=pt, sl=sl: e.matmul(pt[:, :], lhsT=w2a2[32:64, :], rhs=B[L1][32:64, sl], start=True, stop=True),
             reads=[r_small, rB[L1]], writes=[rp])
        p.op("act", lambda e, pt=pt, sl=sl: e.activation(out=B[A_][:, sl], in_=pt[:, :], func=AF.Sigmoid, bias=pv[:, 4:5], scale=1.0),
             reads=[rp, r_small], writes=[rB[A_]])
        pt, rp = bank()
        p.op("pe", lambda e, pt=pt, sl=sl: e.matmul(pt[:, :], lhsT=small["g2"][:], rhs=B[L2][0:96, sl], start=True, stop=True),
             reads=[r_small, rB[L2]], writes=[rp])
        p.op("dve", lambda e, pt=pt, sl=sl: e.tensor_copy(out=B[GT][:, sl], in_=pt[:, :]), reads=[rp], writes=[rB[GT]])
    p.op("dve", lambda e: e.tensor_scalar(out=v1(G_), in0=v1(G_), scalar1=-E05, scalar2=None, op0=ALU.mult), reads=[rB[G_]], writes=[rB[G_]])
    KK = L1
    p.op("dve", lambda e: e.tensor_scalar(out=v1(KK), in0=v1(K_), scalar1=pv[:, 5:6], scalar2=None, op0=ALU.mult),
         reads=[rB[K_], r_small, rB[KK]], writes=[rB[KK]])
    p.op("act", lambda e: e.activation(out=v1(T1), in_=v1(KK), func=AF.Square), reads=[rB[KK], rB[T1]], writes=[rB[T1]])
    for tb in range(8):
        sl = slice(1 + tb * 512, 1 + (tb + 1) * 512)
        pt, rp = bank()
        p.op("pe", lambda e, pt=pt, sl=sl: e.matmul(pt[:, :], lhsT=bo[:], rhs=B[T1][:, sl], start=True, stop=True),
             reads=[r_small, rB[T1]], writes=[rp])
        p.op("act", lambda e, pt=pt, sl=sl: e.activation(out=B[L2][:, sl], in_=pt[:, :], func=AF.Sqrt, bias=pv[:, 12:13], scale=1.0),
             reads=[rp, r_small], writes=[rB[L2]])
    p.op("dve", lambda e: e.tensor_scalar(out=v1(L2), in0=v1(L2), scalar1=1e-12, scalar2=None, op0=ALU.max), reads=[rB[L2]], writes=[rB[L2]])
    p.op("dve", lambda e: e.reciprocal(out=v1(L2), in_=v1(L2)), reads=[rB[L2]], writes=[rB[L2]])
    p.op("dve", lambda e: e.tensor_tensor(out=v1(KK), in0=v1(KK), in1=v1(L2), op=ALU.mult), reads=[rB[KK], rB[L2]], writes=[rB[KK]])
    p.op("dve", lambda e: e.tensor_scalar(out=v1(T1), in0=v1(A_), scalar1=pv[:, 6:7], scalar2=pv[:, 10:11], op0=ALU.mult, op1=ALU.add),
         reads=[rB[A_], r_small, rB[T1]], writes=[rB[T1]])
    p.op("dve", lambda e: e.tensor_tensor(out=v1(K_), in0=v1(K_), in1=v1(T1), op=ALU.mult), reads=[rB[K_], rB[T1]], writes=[rB[K_]])
    p.op("dve", lambda e: e.scalar_tensor_tensor(out=v1(T1), in0=v1(R_), scalar=pv[:, 7:8], in1=v1(K_), op0=ALU.mult, op1=ALU.mult),
         reads=[rB[R_], rB[K_], r_small, rB[T1]], writes=[rB[T1]])
    for tb in range(8):
        sl = slice(1 + tb * 512, 1 + (tb + 1) * 512)
        pt, rp = bank()
        p.op("pe", lambda e, pt=pt, sl=sl: e.matmul(pt[:, :], lhsT=bo[:], rhs=B[T1][:, sl], start=True, stop=True),
             reads=[r_small, rB[T1]], writes=[rp])
        p.op("dve", lambda e, pt=pt, sl=sl: e.tensor_tensor(out=B[V_][:, sl], in0=pt[:, :], in1=B[V_][:, sl], op=ALU.mult),
             reads=[rp, rB[V_]], writes=[rB[V_]])
    c3 = lambda bi: B[bi][:, 1:W].rearrange("p (n c) -> p n c", c=RC_)
    src, dst = G_, T1
    m = 1
    while m < RC_:
        p.op("dve", lambda e, src=src, dst=dst, m=m: e.tensor_tensor(out=c3(dst)[:, :, m:RC_], in0=c3(src)[:, :, m:RC_],
                                                                     in1=c3(src)[:, :, 0:RC_ - m], op=ALU.add),
             reads=[rB[src], rB[dst]], writes=[rB[dst]])
        p.op("pool", lambda e, src=src, dst=dst, m=m: e.tensor_copy(out=c3(dst)[:, :, 0:m], in_=c3(src)[:, :, 0:m]),
             reads=[rB[src], rB[dst]], writes=[rB[dst]])
        src, dst = dst, src
        m *= 2
    assert src == G_
    EG, ENG = L2, T1
    p.op("act", lambda e: e.activation(out=v1(EG), in_=v1(G_), func=AF.Exp), reads=[rB[G_], rB[EG]], writes=[rB[EG]])
    p.op("act", lambda e: e.activation(out=v1(ENG), in_=v1(G_), func=AF.Exp, scale=-1.0), reads=[rB[G_], rB[ENG]], writes=[rB[ENG]])
    p.op("pool", lambda e: e.tensor_copy(out=egc[:], in_=c3(EG)[:, :, RC_ - 1]), reads=[rB[EG]], writes=[r_egc])
    p.dma("sp", egl[:, 0, :], egc[0:64, :], reads=[r_egc], writes=[r_egl])
    p.dma("sp", egl[:, 1, :], egc[64:128, :], reads=[r_egc], writes=[r_egl])
    p.op("dve", lambda e: e.tensor_tensor(out=v1(R_), in0=v1(R_), in1=v1(EG), op=ALU.mult), reads=[rB[R_], rB[EG]], writes=[rB[R_]])
    p.op("pool", lambda e: e.tensor_tensor(out=v1(K_), in0=v1(K_), in1=v1(ENG), op=ALU.mult), reads=[rB[K_], rB[ENG]], writes=[rB[K_]])
    p.op("dve", lambda e: e.tensor_tensor(out=v1(A_), in0=v1(A_), in1=v1(KK), op=ALU.mult), reads=[rB[A_], rB[KK]], writes=[rB[A_]])
    p.op("dve", lambda e: e.scalar_tensor_tensor(out=v1(A_), in0=v1(A_), scalar=-1.0, in1=v1(ENG), op0=ALU.mult, op1=ALU.mult),
         reads=[rB[A_], rB[ENG]], writes=[rB[A_]])
    p.op("dve", lambda e: e.tensor_tensor(out=c3(KK)[:, :, 1:RC_], in0=c3(KK)[:, :, 1:RC_], in1=c3(EG)[:, :, 0:RC_ - 1], op=ALU.mult),
         reads=[rB[KK], rB[EG]], writes=[rB[KK]])
    BT = KK
    YB = [G_, L2]
    for h in range(2):
        p.op("pool", lambda e, h=h: e.memset(hall[:, h, 0, :], 0.0), writes=[r_hall[h][0]])

    def tiles(nm, shp):
        return [p.sbuf(f"{nm}{h}", shp, F32) for h in range(2)], p.regions(2, nm)

    Pa = [[p.sbuf(f"Pa{h}{i}", [128, 128], F32) for i in range(2)] for h in range(2)]
    Pb = [[p.sbuf(f"Pb{h}{i}", [128, 128], F32) for i in range(2)] for h in range(2)]
    X = [[p.sbuf(f"X{h}{i}", [128, 128], F32) for i in range(2)] for h in range(2)]
    rPa = [p.regions(2, f"Pa{h}") for h in range(2)]
    rPb = [p.regions(2, f"Pb{h}") for h in range(2)]
    rX = [p.regions(2, f"X{h}") for h in range(2)]
    AkbT, rAkbT = tiles("AkbT", [128, 128])
    RCt, rRCt = tiles("RCt", [128, 128])
    TBZ, rTBZ = tiles("TBZ", [128, 128])
    ACf, rACf = tiles("ACf", [128, 128])
    KCf, rKCf = tiles("KCf", [128, 128])
    ACt, rACt = tiles("ACt", [128, 64])
    KCt, rKCt = tiles("KCt", [128, 64])
    MT = [[p.sbuf(f"MT{h}{j}", [64, 64], F32) for j in range(2)] for h in range(2)]
    NN = [[p.sbuf(f"NN{h}{j}", [64, 64], F32) for j in range(2)] for h in range(2)]
    rMT = [p.regions(2, f"MT{h}") for h in range(2)]
    rNN = [p.regions(2, f"NN{h}") for h in range(2)]
    AarT, rAarT = tiles("AarT", [128, 128])
    AkrT, rAkrT = tiles("AkrT", [128, 128])
    PT, rPT = tiles("PT", [64, 128])

    def unit(h, pr):
        hs = slice(64 * h, 64 * h + 64)
        tk = slice(1 + pr * 128, 1 + (pr + 1) * 128)
        Bt, At, Kt, Rt = B[BT][hs, tk], B[A_][hs, tk], B[K_][hs, tk], B[R_][hs, tk]
        rBt, rAt, rKt, rRt = rB[BT], rB[A_], rB[K_], rB[R_]
        Vt = VT[:, pr, hs]
        pt, rp = bank()
        p.op("pe", lambda e: e.matmul(pt[:, 0:128], lhsT=Bt, rhs=At, start=True, stop=True), reads=[rBt, rAt], writes=[rp])
        p.op("dve", lambda e: e.tensor_tensor(out=Pa[h][0][:], in0=pt[:, 0:128], in1=msl[:], op=ALU.mult), reads=[rp, r_small], writes=[rPa[h][0]])
        pt2, rp2 = bank()
        p.op("pe", lambda e: e.matmul(pt2[:, 0:128], lhsT=At, rhs=Bt, start=True, stop=True), reads=[rBt, rAt], writes=[rp2])
        p.op("dve", lambda e: e.tensor_tensor(out=Pb[h][0][:], in0=pt2[:, 0:128], in1=msu[:], op=ALU.mult), reads=[rp2, r_small], writes=[rPb[h][0]])
        p.op("pool", lambda e: e.tensor_tensor(out=X[h][0][:], in0=Pb[h][0][:], in1=ident[:], op=ALU.add), reads=[rPb[h][0], r_small], writes=[rX[h][0]])
        yield
        pt3, rp3 = bank()
        p.op("pe", lambda e: e.matmul(pt3[:, 0:128], lhsT=Kt, rhs=Bt, start=True, stop=True), reads=[rKt, rBt], writes=[rp3])
        p.op("dve", lambda e: e.tensor_tensor(out=AkbT[h][:], in0=pt3[:, 0:128], in1=msu[:], op=ALU.mult), reads=[rp3, r_small], writes=[rAkbT[h]])
        pt4, rp4 = bank()
        p.op("pe", lambda e: e.transpose(pt4[:, 0:64], Bt, ident[hs, hs]), reads=[rBt, r_small], writes=[rp4])
        p.op("act", lambda e: e.copy(out=RCt[h][:, 0:64], in_=pt4[:, 0:64]), reads=[rp4], writes=[rRCt[h]])
        pt5, rp5 = bank()
        p.op("pe", lambda e: e.matmul(pt5[:, 0:64], lhsT=AkbT[h][:], rhs=Vt, start=True, stop=True), reads=[rAkbT[h], r_VT], writes=[rp5])
        p.op("act", lambda e: e.copy(out=RCt[h][:, 64:128], in_=pt5[:, 0:64]), reads=[rp5], writes=[rRCt[h]])
        yield
        cur = 0
        for k in range(1, 6):
            nxt = 1 - cur
            pa, rpa = bank()
            p.op("pe", lambda e, pa=pa, cur=cur: e.matmul(pa[:, 0:128], lhsT=Pb[h][cur][:], rhs=Pa[h][cur][:], start=True, stop=True),
                 reads=[rPb[h][cur], rPa[h][cur]], writes=[rpa])
            if k < 5:
                pb, rpb = bank()
                p.op("pe", lambda e, pb=pb, cur=cur: e.matmul(pb[:, 0:128], lhsT=Pa[h][cur][:], rhs=Pb[h][cur][:], start=True, stop=True),
                     reads=[rPb[h][cur], rPa[h][cur]], writes=[rpb])
            p.op("act", lambda e, pa=pa, nxt=nxt: e.copy(out=Pa[h][nxt][:], in_=pa[:, 0:128]), reads=[rpa], writes=[rPa[h][nxt]])
            if k < 5:
                p.op("dve", lambda e, pb=pb, nxt=nxt: e.tensor_copy(out=Pb[h][nxt][:], in_=pb[:, 0:128]), reads=[rpb], writes=[rPb[h][nxt]])
            px, rpx = bank()
            p.op("pe", lambda e, px=px, cur=cur, nxt=nxt: e.matmul(px[:, 0:128], lhsT=Pa[h][nxt][:], rhs=X[h][cur][:], start=True, stop=True),
                 reads=[rPa[h][nxt], rX[h][cur]], writes=[rpx])
            p.op("dve", lambda e, px=px, cur=cur, nxt=nxt: e.tensor_tensor(out=X[h][nxt][:], in0=px[:, 0:128], in1=X[h][cur][:], op=ALU.add),
                 reads=[rpx, rX[h][cur]], writes=[rX[h][nxt]])
            cur = nxt
            yield
        Xf, rXf = X[h][cur], rX[h][cur]
        pt6, rp6 = bank()
        p.op("pe", lambda e: e.matmul(pt6[:, 0:128], lhsT=Xf[:], rhs=RCt[h][:], start=True, stop=True), reads=[rXf, rRCt[h]], writes=[rp6])
        p.op("act", lambda e: e.copy(out=TBZ[h][:], in_=pt6[:, 0:128]), reads=[rp6], writes=[rTBZ[h]])
        egb = egc[hs, 2 * pr:2 * pr + 2].unsqueeze(2).to_broadcast([64, 2, RC_])
        p.op("dve", lambda e: e.tensor_tensor(out=ACf[h][hs, :].rearrange("p (n c) -> p n c", c=RC_), in0=At.rearrange("p (n c) -> p n c", c=RC_),
                                              in1=egb, op=ALU.mult), reads=[rAt, r_egc], writes=[rACf[h]])
        p.op("pool", lambda e: e.tensor_tensor(out=KCf[h][hs, :].rearrange("p (n c) -> p n c", c=RC_), in0=Kt.rearrange("p (n c) -> p n c", c=RC_),
                                               in1=egb, op=ALU.mult), reads=[rKt, r_egc], writes=[rKCf[h]])
        pt7, rp7 = bank()
        p.op("pe", lambda e: e.transpose(pt7[:, 0:64], ACf[h][hs, :], ident[hs, hs]), reads=[rACf[h], r_small], writes=[rp7])
        p.op("act", lambda e: e.copy(out=ACt[h][:], in_=pt7[:, 0:64]), reads=[rp7], writes=[rACt[h]])
        pt8, rp8 = bank()
        p.op("pe", lambda e: e.transpose(pt8[:, 0:64], KCf[h][hs, :], ident[hs, hs]), reads=[rKCf[h], r_small], writes=[rp8])
        p.op("dve", lambda e: e.tensor_copy(out=KCt[h][:], in_=pt8[:, 0:64]), reads=[rp8], writes=[rKCt[h]])
        yield
        pt9, rp9 = bank()
        p.op("pe", lambda e: e.matmul(pt9[:, 0:128], lhsT=At, rhs=Rt, start=True, stop=True), reads=[rAt, rRt], writes=[rp9])
        p.op("dve", lambda e: e.tensor_tensor(out=AarT[h][:], in0=pt9[:, 0:128], in1=miu[:], op=ALU.mult), reads=[rp9, r_small], writes=[rAarT[h]])
        pt10, rp10 = bank()
        p.op("pe", lambda e: e.matmul(pt10[:, 0:128], lhsT=Kt, rhs=Rt, start=True, stop=True), reads=[rKt, rRt], writes=[rp10])
        p.op("dve", lambda e: e.tensor_tensor(out=AkrT[h][:], in0=pt10[:, 0:128], in1=miu[:], op=ALU.mult), reads=[rp10, r_small], writes=[rAkrT[h]])
        pt11, rp11 = bank()
        p.op("pe", lambda e: e.matmul(pt11[0:64, 0:128], lhsT=TBZ[h][:, 0:64], rhs=AarT[h][:], start=True, stop=False),
             reads=[rTBZ[h], rAarT[h]], writes=[rp11])
        p.op("pe", lambda e: e.matmul(pt11[0:64, 0:128], lhsT=ident[hs, hs], rhs=Rt, start=False, stop=True),
             reads=[r_small, rRt], writes=[rp11])
        p.op("act", lambda e: e.copy(out=PT[h][:], in_=pt11[0:64, 0:128]), reads=[rp11], writes=[rPT[h]])
        yield
        for j in range(2):
            c = 2 * pr + j
            rows = slice(64 * j, 64 * j + 64)
            pm, rpm = bank()
            p.op("pe", lambda e, pm=pm, rows=rows: e.matmul(pm[0:64, 0:64], lhsT=TBZ[h][rows, 0:64], rhs=ACt[h][rows, :], start=True, stop=True),
                 reads=[rTBZ[h], rACt[h]], writes=[rpm])
            p.op("pe", lambda e, pm=pm, rows=rows: e.matmul(pm[0:64, 64:128], lhsT=ACt[h][rows, :], rhs=TBZ[h][rows, 64:128], start=True, stop=False),
                 reads=[rTBZ[h], rACt[h]], writes=[rpm])
            p.op("pe", lambda e, pm=pm, rows=rows: e.matmul(pm[0:64, 64:128], lhsT=KCt[h][rows, :], rhs=VT[rows, pr, hs], start=False, stop=True),
                 reads=[rKCt[h], r_VT], writes=[rpm])
            p.op("dve", lambda e, pm=pm, j=j, c=c: e.scalar_tensor_tensor(out=MT[h][j][:], in0=ident[0:64, 0:64], scalar=egl[:, h, c:c + 1],
                                                                           in1=pm[0:64, 0:64], op0=ALU.mult, op1=ALU.add),
                 reads=[rpm, r_small, r_egl], writes=[rMT[h][j]])
            p.op("act", lambda e, pm=pm, j=j: e.copy(out=NN[h][j][:], in_=pm[0:64, 64:128]), reads=[rpm], writes=[rNN[h][j]])
            ph, rph = bank()
            p.op("pe", lambda e, ph=ph, j=j, c=c: e.matmul(ph[0:64, 0:64], lhsT=MT[h][j][:], rhs=hall[:, h, c % 4, :], start=True, stop=True),
                 reads=[rMT[h][j], r_hall[h][c % 4]], writes=[rph])
            p.op("dve", lambda e, ph=ph, j=j, c=c: e.tensor_tensor(out=hall[:, h, (c + 1) % 4, :], in0=ph[0:64, 0:64], in1=NN[h][j][:], op=ALU.add),
                 reads=[rph, rNN[h][j]], writes=[r_hall[h][(c + 1) % 4]])
            yield
        py, rpy = bank()
        p.op("pe", lambda e: e.matmul(py[0:64, 0:128], lhsT=TBZ[h][:, 64:128], rhs=AarT[h][:], start=True, stop=False),
             reads=[rTBZ[h], rAarT[h]], writes=[rpy])
        p.op("pe", lambda e: e.matmul(py[0:64, 0:128], lhsT=Vt, rhs=AkrT[h][:], start=False, stop=False),
             reads=[r_VT, rAkrT[h]], writes=[rpy])
        for j in range(2):
            c = 2 * pr + j
            p.op("pe", lambda e, j=j, c=c: e.matmul(py[0:64, 64 * j:64 * j + 64], lhsT=hall[:, h, c % 4, :], rhs=PT[h][:, 64 * j:64 * j + 64],
                                                    start=False, stop=(j == 1)),
                 reads=[r_hall[h][c % 4], rPT[h]], writes=[rpy])
        p.op("act", lambda e: e.copy(out=B[YB[h]][0:64, tk], in_=py[0:64, 0:128]), reads=[rpy], writes=[rB[YB[h]]])
        yield

    for pr in range(NPAIR):
        gens = [unit(0, pr), unit(1, pr)]
        alive = [True, True]
        while any(alive):
            for gi, g in enumerate(gens):
                if alive[gi]:
                    try:
                        next(g)
                    except StopIteration:
                        alive[gi] = False
    Y = YB[0]
    p.dma("sp", B[Y][64:128, 1:W], B[YB[1]][0:64, 1:W], reads=[rB[YB[1]]], writes=[rB[Y]])
    SQ, OUT = T1, A_
    p.op("act", lambda e: e.activation(out=v1(SQ), in_=v1(Y), func=AF.Square), reads=[rB[Y], rB[SQ]], writes=[rB[SQ]])
    mb = [p.sbuf(f"mb{i}", [128, 512], F32) for i in range(2)]
    vb = [p.sbuf(f"vb{i}", [128, 512], F32) for i in range(2)]
    r_mb, r_vb = p.regions(2, "mb"), p.regions(2, "vb")
    for tb in range(8):
        i = tb % 2
        sl = slice(1 + tb * 512, 1 + (tb + 1) * 512)
        pm, rpm = bank()
        p.op("pe", lambda e, pm=pm, sl=sl: e.matmul(pm[:, :], lhsT=bo[:], rhs=B[Y][:, sl], start=True, stop=True), reads=[r_small, rB[Y]], writes=[rpm])
        pq, rpq = bank()
        p.op("pe", lambda e, pq=pq, sl=sl: e.matmul(pq[:, :], lhsT=bo[:], rhs=B[SQ][:, sl], start=True, stop=True), reads=[r_small, rB[SQ]], writes=[rpq])
        p.op("act", lambda e, pm=pm, i=i: e.activation(out=mb[i][:], in_=pm[:, :], func=AF.Copy, scale=1.0 / 64.0), reads=[rpm], writes=[r_mb[i]])
        p.op("dve", lambda e, i=i, sl=sl: e.tensor_tensor(out=B[OUT][:, sl], in0=B[Y][:, sl], in1=mb[i][:], op=ALU.subtract),
             reads=[rB[Y], r_mb[i], rB[OUT]], writes=[rB[OUT]])
        p.op("act", lambda e, i=i: e.activation(out=mb[i][:], in_=mb[i][:], func=AF.Square), reads=[r_mb[i]], writes=[r_mb[i]])
        p.op("dve", lambda e, pq=pq, i=i: e.scalar_tensor_tensor(out=vb[i][:], in0=pq[:, :], scalar=1.0 / 64.0, in1=mb[i][:], op0=ALU.mult, op1=ALU.subtract),
             reads=[rpq, r_mb[i]], writes=[r_vb[i]])
        p.op("act", lambda e, i=i: e.activation(out=vb[i][:], in_=vb[i][:], func=AF.Sqrt, bias=pv[:, 11:12], scale=1.0), reads=[r_vb[i], r_small], writes=[r_vb[i]])
        p.op("dve", lambda e, i=i: e.reciprocal(out=vb[i][:], in_=vb[i][:]), reads=[r_vb[i]], writes=[r_vb[i]])
        p.op("dve", lambda e, i=i, sl=sl: e.tensor_tensor(out=B[OUT][:, sl], in0=B[OUT][:, sl], in1=vb[i][:], op=ALU.mult),
             reads=[rB[OUT], r_vb[i]], writes=[rB[OUT]])
        p.op("dve", lambda e, sl=sl: e.tensor_scalar(out=B[OUT][:, sl], in0=B[OUT][:, sl], scalar1=pv[:, 8:9], scalar2=pv[:, 9:10], op0=ALU.mult, op1=ALU.add),
             reads=[rB[OUT], r_small], writes=[rB[OUT]])
        p.op("pool", lambda e, sl=sl: e.tensor_tensor(out=B[OUT][:, sl], in0=B[OUT][:, sl], in1=B[V_][:, sl], op=ALU.add),
             reads=[rB[OUT], rB[V_]], writes=[rB[OUT]])
        p.op("pool", lambda e, sl=sl: e.tensor_tensor(out=B[OUT][:, sl], in0=B[OUT][:, sl], in1=B[GT][:, sl], op=ALU.mult),
             reads=[rB[OUT], rB[GT]], writes=[rB[OUT]])
    p.dma("sp", o_d, B[OUT][:, 1:W], reads=[rB[OUT]], is_output=True)
    p.emit()
    return nc


KC = 16
ALPHA = 4 ** 0.25
LN_EPS = 1e-5


def make_stream(p, nwb=2, sk=8):
    stg = [p.sbuf(f"stg{i}", [128, sk * 512], F32) for i in range(2)]
    r_stg = p.regions(2, "stg")
    wb = [p.sbuf(f"wb{i}", [128, 16, 512], BF16) for i in range(nwb)]
    r_wb = p.regions(nwb, "wb")
    st = {"si": 0, "wi": 0, "ci": 0}

    def stage():
        si = st["si"] % 2
        st["si"] += 1
        return stg[si], r_stg[si]

    def ceng():
        st["ci"] += 1
        return "dve" if st["ci"] % 2 == 0 else "pool"

    def load(w_ap, nk, cw=512, into=None):
        if into is None:
            i = st["wi"] % nwb
            st["wi"] += 1
            dst, rdst = wb[i], r_wb[i]
        else:
            dst, rdst = into
        for k0 in range(0, nk, sk):
            kk = min(sk, nk - k0)
            sg, rsg = stage()
            sv = sg[:, 0:kk * 512].rearrange("p (k n) -> p k n", n=512)
            p.dma("sp", sv[:, :, 0:cw], w_ap[k0 * 128:(k0 + kk) * 128, :].rearrange("(k p) n -> p k n", p=128), writes=[rsg])
            p.op(ceng(), lambda e, dst=dst, sv=sv, k0=k0, kk=kk, cw=cw: e.tensor_copy(out=dst[:, k0:k0 + kk, 0:cw], in_=sv[:, :, 0:cw]),
                 reads=[rsg], writes=[rdst])
        return dst, rdst

    def load_act(a_ap, dst, rdst, k0, nk, ntok=1024):
        sg, rsg = stage()
        sv = sg[:, 0:nk * ntok].rearrange("p (k t) -> p k t", t=ntok)
        p.dma("sp", sv, a_ap.rearrange("(k p) t -> p k t", p=128), writes=[rsg])
        p.op(ceng(), lambda e: e.tensor_copy(out=dst[:, k0:k0 + nk, :], in_=sv), reads=[rsg], writes=[rdst])

    return load, load_act, stage, ceng


def build_c1(ntok=1024):
    nc = bass.Bass("TRN2", target_bir_lowering=False)
    D = 2048
    dt_ = lambda n, s: nc.dram_tensor(n, s, F32, kind="ExternalInput").ap()
    xT_d, sc_d, sh_d = dt_("xT", [D, ntok]), dt_("sc", [128, KC]), dt_("sh", [128, KC])
    wg_d, wbr_d = dt_("wg", [D, 4 * D]), dt_("wbr", [1792, D])
    ya_d, yc_d, yd_d = dt_("yaT", [512, ntok]), dt_("ycT", [512, ntok]), dt_("ydT", [512, ntok])
    ybn_d, ybd_d = dt_("ybn", [3, 256, ntok]), dt_("ybd", [3, 256, ntok])
    o_d = nc.dram_tensor("o", [D, ntok], F32, kind="ExternalOutput").ap()
    p = Prog(nc)
    load, load_act, stage, ceng = make_stream(p)
    sc = p.sbuf("sc_s", [128, KC], F32)
    sh = p.sbuf("sh_s", [128, KC], F32)
    hT = p.sbuf("hT", [128, KC, ntok], BF16)
    r_sc = p.region("sc")
    r_h = p.regions(KC, "h")
    p.dma("sp", sc[:], sc_d, writes=[r_sc])
    p.dma("sp", sh[:], sh_d, writes=[r_sc])
    p.op("dve", lambda e: e.tensor_scalar_add(out=sc[:], in0=sc[:], scalar1=1.0), reads=[r_sc], writes=[r_sc])
    for g in range(KC // 4):
        sg, rsg = stage()
        sv = sg[:].rearrange("p (k t) -> p k t", t=ntok)
        p.dma("sp", sv, xT_d[g * 512:(g + 1) * 512, :].rearrange("(k p) t -> p k t", p=128), writes=[rsg])
        for j in range(4):
            kc = g * 4 + j
            p.op("act", lambda e, sv=sv, j=j, kc=kc: e.activation(out=hT[:, kc, :], in_=sv[:, j, :], func=AF.Identity,
                                                                  bias=sh[:, kc:kc + 1], scale=sc[:, kc:kc + 1]),
                 reads=[rsg, r_sc], writes=[r_h[kc]])
    yT = p.sbuf("yT", [128, 14, ntok], BF16)
    r_y = p.regions(4, "y")
    load_act(ya_d, yT, r_y[0], 0, 4, ntok)
    load_act(yc_d, yT, r_y[2], 6, 4, ntok)
    load_act(yd_d, yT, r_y[3], 10, 4, ntok)
    for j in range(2):
        sn, rsn = stage()
        sd, rsd = stage()
        nv = sn[:, 0:3 * ntok].rearrange("p (g t) -> p g t", t=ntok)
        dv = sd[:, 0:3 * ntok].rearrange("p (g t) -> p g t", t=ntok)
        p.dma("sp", nv, ybn_d[:, j * 128:(j + 1) * 128, :].rearrange("g p t -> p g t"), writes=[rsn])
        p.dma("sp", dv, ybd_d[:, j * 128:(j + 1) * 128, :].rearrange("g p t -> p g t"), writes=[rsd])
        p.op("dve", lambda e, nv=nv: e.tensor_tensor(out=nv[:, 0, :], in0=nv[:, 0, :], in1=nv[:, 1, :], op=ALU.add), reads=[rsn], writes=[rsn])
        p.op("dve", lambda e, nv=nv: e.tensor_tensor(out=nv[:, 0, :], in0=nv[:, 0, :], in1=nv[:, 2, :], op=ALU.add), reads=[rsn], writes=[rsn])
        p.op("pool", lambda e, dv=dv: e.tensor_tensor(out=dv[:, 0, :], in0=dv[:, 0, :], in1=dv[:, 1, :], op=ALU.add), reads=[rsd], writes=[rsd])
        p.op("pool", lambda e, dv=dv: e.tensor_tensor(out=dv[:, 0, :], in0=dv[:, 0, :], in1=dv[:, 2, :], op=ALU.add), reads=[rsd], writes=[rsd])
        p.op("dve", lambda e, dv=dv: e.reciprocal(out=dv[:, 0, :], in_=dv[:, 0, :]), reads=[rsd], writes=[rsd])
        p.op("dve", lambda e, nv=nv, dv=dv, j=j: e.tensor_tensor(out=yT[:, 4 + j, :], in0=nv[:, 0, :], in1=dv[:, 0, :], op=ALU.mult),
             reads=[rsn, rsd], writes=[r_y[1]])
    brk = ((0, 4), (4, 6), (6, 10), (10, 14))
    wbrb = p.sbuf("wbrb", [128, 16, 512], BF16)
    r_wbrb = p.region("wbrb")
    acc = p.sbuf("acc", [128, 2, 4, 2, 512], F32)
    r_acc = [[[p.region(f"acc{a}{b}{c}") for c in range(2)] for b in range(4)] for a in range(2)]
    sig = [p.sbuf(f"sig{i}", [128, 512], F32) for i in range(3)]
    r_sig = p.regions(3, "sig")
    pg = [p.psum(f"pg{i}", [128, 512]) for i in range(3)]
    pb = [p.psum(f"pb{i}", [128, 512]) for i in range(3)]
    r_pg, r_pb = p.regions(3, "pg"), p.regions(3, "pb")
    cnt = 0
    ntb = ntok // 512
    for dg in range(4):
        par = dg % 2
        load(wbr_d[:, dg * 512:(dg + 1) * 512], 14, into=(wbrb, r_wbrb))
        for br in range(4):
            wt, rwt = load(wg_d[:, br * D + dg * 512: br * D + (dg + 1) * 512], 16)
            k0, k1 = brk[br]
            for dci in range(4):
                cs = slice(dci * 128, (dci + 1) * 128)
                for tb in range(ntb):
                    ts_ = slice(tb * 512, (tb + 1) * 512)
                    i = cnt % 3
                    cnt += 1
                    for kc in range(KC):
                        p.op("pe", lambda e, i=i, wt=wt, kc=kc, cs=cs, ts_=ts_: e.matmul(pg[i][:, :], lhsT=wt[:, kc, cs], rhs=hT[:, kc, ts_],
                                                                                        start=(kc == 0), stop=(kc == KC - 1)),
                             reads=[rwt, r_h[kc]], writes=[r_pg[i]])
                    for kc in range(k0, k1):
                        p.op("pe", lambda e, i=i, kc=kc, cs=cs, ts_=ts_, k0=k0, k1=k1: e.matmul(pb[i][:, :], lhsT=wbrb[:, kc, cs], rhs=yT[:, kc, ts_],
                                                                                                start=(kc == k0), stop=(kc == k1 - 1)),
                             reads=[r_wbrb, r_y[br]], writes=[r_pb[i]])
                    p.op("act", lambda e, i=i: e.activation(out=sig[i][:], in_=pg[i][:, :], func=AF.Sigmoid), reads=[r_pg[i]], writes=[r_sig[i]])
                    a_ap = acc[:, par, dci, tb, :]
                    ra = r_acc[par][dci][tb]
                    if br == 0:
                        p.op("dve", lambda e, i=i, a_ap=a_ap: e.tensor_tensor(out=a_ap, in0=pb[i][:, :], in1=sig[i][:], op=ALU.mult),
                             reads=[r_pb[i], r_sig[i]], writes=[ra])
                    else:
                        p.op("dve", lambda e, i=i: e.tensor_tensor(out=sig[i][:], in0=pb[i][:, :], in1=sig[i][:], op=ALU.mult),
                             reads=[r_pb[i], r_sig[i]], writes=[r_sig[i]])
                        p.op("pool", lambda e, i=i, a_ap=a_ap: e.tensor_tensor(out=a_ap, in0=a_ap, in1=sig[i][:], op=ALU.add),
                             reads=[ra, r_sig[i]], writes=[ra])
        p.dma("sp", o_d[dg * 512:(dg + 1) * 512, :].rearrange("(c p) (b t) -> p c b t", p=128, t=512), acc[:, par],
              reads=[r for b_ in r_acc[par] for r in b_], is_output=True)
    p.emit()
    return nc


def emit_ln(p, zT, r_z, ntok, gcol, bcol, onesD, r_const, eps_ap, bank, name, tmps=None):
    if tmps is None:
        sq = [p.sbuf(f"{name}sq{i}", [128, 512], F32) for i in range(2)]
        r_sq = p.regions(2, name + "sq")
        mean = p.sbuf(name + "mean", [128, 512], F32)
        rstd = p.sbuf(name + "rstd", [128, 512], F32)
        r_mean, r_rstd = p.region(name + "mean"), p.region(name + "rstd")
    else:
        (sq0, rs0), (sq1, rs1), (mean, r_mean), (rstd, r_rstd) = tmps
        sq, r_sq = [sq0, sq1], [rs0, rs1]
    def one_tb(tb):
        ts_ = slice(tb * 512, (tb + 1) * 512)
        pm, rpm = bank()
        pq, rpq = bank()
        for dc in range(KC):
            i = dc % 2
            p.op("pe", lambda e, dc=dc: e.matmul(pm[:, :], lhsT=onesD[:], rhs=zT[:, dc, ts_], start=(dc == 0), stop=(dc == KC - 1)),
                 reads=[r_const, r_z[dc]], writes=[rpm])
            p.op("act", lambda e, dc=dc, i=i: e.activation(out=sq[i][:], in_=zT[:, dc, ts_], func=AF.Square), reads=[r_z[dc]], writes=[r_sq[i]])
            p.op("pe", lambda e, dc=dc, i=i: e.matmul(pq[:, :], lhsT=onesD[:], rhs=sq[i][:], start=(dc == 0), stop=(dc == KC - 1)),
                 reads=[r_const, r_sq[i]], writes=[rpq])
        p.op("act", lambda e: e.copy(out=mean[:], in_=pm[:, :]), reads=[rpm], writes=[r_mean])
        p.op("act", lambda e: e.activation(out=rstd[:], in_=pm[:, :], func=AF.Square), reads=[rpm], writes=[r_rstd])
        p.op("dve", lambda e: e.tensor_tensor(out=rstd[:], in0=pq[:, :], in1=rstd[:], op=ALU.subtract), reads=[rpq, r_rstd], writes=[r_rstd])
        p.op("act", lambda e: e.activation(out=rstd[:], in_=rstd[:], func=AF.Sqrt, bias=eps_ap, scale=1.0), reads=[r_rstd, r_const], writes=[r_rstd])
        p.op("dve", lambda e: e.reciprocal(out=rstd[:], in_=rstd[:]), reads=[r_rstd], writes=[r_rstd])
        for dc in range(KC):
            p.op("dve", lambda e, dc=dc: e.tensor_tensor(out=zT[:, dc, ts_], in0=zT[:, dc, ts_], in1=mean[:], op=ALU.subtract),
                 reads=[r_z[dc], r_mean], writes=[r_z[dc]])
            p.op("pool", lambda e, dc=dc: e.tensor_tensor(out=zT[:, dc, ts_], in0=zT[:, dc, ts_], in1=rstd[:], op=ALU.mult),
                 reads=[r_z[dc], r_rstd], writes=[r_z[dc]])
            p.op("act", lambda e, dc=dc: e.activation(out=zT[:, dc, ts_], in_=zT[:, dc, ts_], func=AF.Identity, bias=bcol(dc), scale=gcol(dc)),
                 reads=[r_z[dc], r_const], writes=[r_z[dc]])

    for tb in range(ntok // 512):
        one_tb(tb)


def build_c2(ntok=1024):
    nc = bass.Bass("TRN2", target_bir_lowering=False)
    D = 2048
    dt_ = lambda n, s: nc.dram_tensor(n, s, F32, kind="ExternalInput").ap()
    m_d, xT_d, wo_d = dt_("mT", [D, ntok]), dt_("xT", [D, ntok]), dt_("wout", [D, D])
    pv_d = dt_("pv", [128, 8, KC])
    rw_d, rb_d = dt_("rw", [D, 32]), dt_("rb", [32, 1])
    id_d, on_d = dt_("ident", [128, 128]), dt_("onesD", [128, 128])
    x1_d = nc.dram_tensor("x1T", [D, ntok], F32, kind="ExternalOutput").ap()
    wt_d = nc.dram_tensor("wtT", [32, ntok], F32, kind="ExternalOutput").ap()
    p = Prog(nc)
    load, load_act, stage, ceng = make_stream(p)
    pv = p.sbuf("pv_s", [128, 8, KC], F32)
    rw = p.sbuf("rw_s", [128, KC, 32], F32)
    rb = p.sbuf("rb_s", [32, 1], F32)
    ident = p.sbuf("ident_s", [128, 128], F32)
    onesD = p.sbuf("onesD_s", [128, 128], F32)
    eps = p.sbuf("eps_s", [128, 1], F32)
    r_const = p.region("const")
    p.dma("sp", pv[:], pv_d, writes=[r_const])
    p.dma("sp", rw[:], rw_d.rearrange("(k p) e -> p k e", p=128), writes=[r_const])
    p.dma("sp", rb[:], rb_d, writes=[r_const])
    p.dma("sp", ident[:], id_d, writes=[r_const])
    p.dma("sp", onesD[:], on_d, writes=[r_const])
    p.op("pool", lambda e: e.memset(eps[:], LN_EPS), writes=[r_const])
    p.op("dve", lambda e: e.tensor_scalar_add(out=pv[:, 3, :], in0=pv[:, 3, :], scalar1=1.0), reads=[r_const], writes=[r_const])
    ps = [p.psum(f"ps{i}", [128, 512]) for i in range(8)]
    r_ps = p.regions(8, "ps")
    pc = [0]

    def bank():
        i = pc[0] % 8
        pc[0] += 1
        return ps[i], r_ps[i]

    mTb = p.sbuf("mTb", [128, KC, ntok], BF16)
    r_m = p.regions(4, "m")
    for g in range(4):
        load_act(m_d[g * 512:(g + 1) * 512, :], mTb, r_m[g], g * 4, 4, ntok)
    zT = p.sbuf("zT", [128, KC, ntok], F32)
    r_z = p.regions(KC, "z")
    xa = [p.sbuf(f"xa{i}", [128, ntok], F32) for i in range(2)]
    r_xa = p.regions(2, "xa")
    ntb = ntok // 512
    for dg in range(4):
        wt, rwt = load(wo_d[:, dg * 512:(dg + 1) * 512], 16)
        for dci in range(4):
            dc = dg * 4 + dci
            i = dc % 2
            p.dma("sp", xa[i][:], xT_d[dc * 128:(dc + 1) * 128, :], writes=[r_xa[i]])
            p.op("act", lambda e, i=i: e.mul(out=xa[i][:], in_=xa[i][:], mul=ALPHA), reads=[r_xa[i]], writes=[r_xa[i]])
            for tb in range(ntb):
                ts_ = slice(tb * 512, (tb + 1) * 512)
                pt, rp = bank()
                for kc in range(KC):
                    p.op("pe", lambda e, pt=pt, wt=wt, kc=kc, dci=dci, ts_=ts_: e.matmul(pt[:, :], lhsT=wt[:, kc, dci * 128:(dci + 1) * 128],
                                                                                        rhs=mTb[:, kc, ts_], start=(kc == 0), stop=(kc == KC - 1)),
                         reads=[rwt, r_m[kc // 4]], writes=[rp])
                p.op("dve", lambda e, pt=pt, dc=dc, i=i, ts_=ts_: e.scalar_tensor_tensor(out=zT[:, dc, ts_], in0=pt[:, :], scalar=pv[:, 0, dc:dc + 1],
                                                                                        in1=xa[i][:, ts_], op0=ALU.mult, op1=ALU.add),
                     reads=[rp, r_const, r_xa[i]], writes=[r_z[dc]])
    emit_ln(p, zT, r_z, ntok, lambda dc: pv[:, 1, dc:dc + 1], lambda dc: pv[:, 2, dc:dc + 1], onesD, r_const, eps[:], bank, "ln1")
    p.dma("sp", x1_d.rearrange("(k p) t -> p k t", p=128), zT[:], reads=r_z, is_output=True)
    h2 = [p.sbuf(f"h2{i}", [128, ntok], F32) for i in range(2)]
    r_h2 = p.regions(2, "h2")
    pl = [bank() for _ in range(ntb)]
    for dc in range(KC):
        i = dc % 2
        p.op("act", lambda e, dc=dc, i=i: e.activation(out=h2[i][:], in_=zT[:, dc, :], func=AF.Identity, bias=pv[:, 4, dc:dc + 1], scale=pv[:, 3, dc:dc + 1]),
             reads=[r_z[dc], r_const], writes=[r_h2[i]])
        for tb in range(ntb):
            p.op("pe", lambda e, dc=dc, i=i, tb=tb: e.matmul(pl[tb][0][0:32, :], lhsT=rw[:, dc, :], rhs=h2[i][:, tb * 512:(tb + 1) * 512],
                                                             start=(dc == 0), stop=(dc == KC - 1)),
                 reads=[r_const, r_h2[i]], writes=[pl[tb][1]])
    LT = p.sbuf("LT", [32, ntok], F32)
    r_LT = p.region("LT")
    for tb in range(ntb):
        p.op("act", lambda e, tb=tb: e.activation(out=LT[:, tb * 512:(tb + 1) * 512], in_=pl[tb][0][0:32, :], func=AF.Identity, bias=rb[:], scale=1.0),
             reads=[pl[tb][1], r_const], writes=[r_LT])
    wtT = p.sbuf("wtT_s", [32, ntok], F32)
    r_wtT = p.regions(ntok // 128, "wtT")
    L = [p.sbuf(f"L{i}", [128, 32], F32) for i in range(2)]
    E = [p.sbuf(f"E{i}", [128, 32], F32) for i in range(2)]
    M8 = [p.sbuf(f"M8{i}", [128, 8], F32) for i in range(2)]
    S1 = [p.sbuf(f"S1{i}", [128, 2], F32) for i in range(2)]
    r_L, r_E, r_M8, r_S1 = p.regions(2, "L"), p.regions(2, "E"), p.regions(2, "M8"), p.regions(2, "S1")
    for tt in range(ntok // 128):
        i = tt % 2
        pt, rp = bank()
        p.op("pe", lambda e, pt=pt, tt=tt: e.transpose(pt[:, 0:32], LT[:, tt * 128:(tt + 1) * 128], ident[0:32, 0:32]), reads=[r_LT, r_const], writes=[rp])
        p.op("act", lambda e, pt=pt, i=i: e.copy(out=L[i][:], in_=pt[:, 0:32]), reads=[rp], writes=[r_L[i]])
        p.op("dve", lambda e, i=i: e.max(out=M8[i][:], in_=L[i][:]), reads=[r_L[i]], writes=[r_M8[i]])
        p.op("dve", lambda e, i=i: e.tensor_scalar(out=S1[i][:, 0:1], in0=M8[i][:, 0:1], scalar1=-1.0, scalar2=None, op0=ALU.mult),
             reads=[r_M8[i]], writes=[r_S1[i]])
        p.op("act", lambda e, i=i: e.activation(out=E[i][:], in_=L[i][:], func=AF.Exp, bias=S1[i][:, 0:1], scale=1.0),
             reads=[r_L[i], r_S1[i]], writes=[r_E[i]])
        p.op("dve", lambda e, i=i: e.tensor_scalar(out=L[i][:], in0=L[i][:], scalar1=M8[i][:, 3:4], scalar2=None, op0=ALU.is_ge),
             reads=[r_L[i], r_M8[i], r_E[i]], writes=[r_L[i]])
        p.op("dve", lambda e, i=i: e.tensor_tensor(out=E[i][:], in0=E[i][:], in1=L[i][:], op=ALU.mult), reads=[r_E[i], r_L[i]], writes=[r_E[i]])
        p.op("dve", lambda e, i=i: e.reduce_sum(out=S1[i][:, 1:2], in_=E[i][:], axis=AX.X), reads=[r_E[i], r_S1[i]], writes=[r_S1[i]])
        p.op("dve", lambda e, i=i: e.reciprocal(out=S1[i][:, 1:2], in_=S1[i][:, 1:2]), reads=[r_S1[i]], writes=[r_S1[i]])
        p.op("dve", lambda e, i=i: e.tensor_scalar(out=E[i][:], in0=E[i][:], scalar1=S1[i][:, 1:2], scalar2=None, op0=ALU.mult),
             reads=[r_E[i], r_S1[i]], writes=[r_E[i]])
        pt2, rp2 = bank()
        p.op("pe", lambda e, pt2=pt2, i=i: e.transpose(pt2[0:32, 0:128], E[i][:], ident[:]), reads=[r_E[i], r_const], writes=[rp2])
        p.op("act", lambda e, pt2=pt2, tt=tt: e.copy(out=wtT[:, tt * 128:(tt + 1) * 128], in_=pt2[0:32, 0:128]), reads=[rp2], writes=[r_wtT[tt]])
    p.dma("sp", wt_d, wtT[:], reads=r_wtT, is_output=True)
    p.emit()
    return nc


def build_c3(ntok=1024, nexp=32):
    nc = bass.Bass("TRN2", target_bir_lowering=False)
    D, F = 2048, 1024
    dt_ = lambda n, s: nc.dram_tensor(n, s, F32, kind="ExternalInput").ap()
    x1_d, wt_d = dt_("x1T", [D, ntok]), dt_("wtT", [32, ntok])
    pv_d = dt_("pv", [128, 8, KC])
    wgu_d, bgu_d = dt_("wgu", [nexp, D, 2 * F]), dt_("bgu", [128, nexp, 16])
    wd_d, bd_d = dt_("wd", [nexp, F, D]), dt_("bd", [32, D])
    on_d = dt_("onesD", [128, 128])
    o_d = nc.dram_tensor("o", [D, ntok], F32, kind="ExternalOutput").ap()
    p = Prog(nc)
    load, load_act, stage, ceng = make_stream(p, nwb=3, sk=4)
    pv = p.sbuf("pv_s", [128, 8, KC], F32)
    onesD = p.sbuf("onesD_s", [128, 128], F32)
    cst = p.sbuf("cst", [128, 4], F32)
    bgu = p.sbuf("bgu_s", [128, nexp, 16], F32)
    bd = p.sbuf("bd_s", [32, D], F32)
    wts = p.sbuf("wts", [32, ntok], F32)
    r_const = p.region("const")
    p.dma("sp", pv[:], pv_d, writes=[r_const])
    p.dma("sp", onesD[:], on_d, writes=[r_const])
    p.dma("sp", bgu[:], bgu_d, writes=[r_const])
    p.dma("sp", bd[:], bd_d, writes=[r_const])
    p.dma("sp", wts[:], wt_d, writes=[r_const])
    for j, val in enumerate((LN_EPS, 7.0, -7.0, 1.0)):
        p.op("pool", lambda e, j=j, val=val: e.memset(cst[:, j:j + 1], val), writes=[r_const])
    p.op("dve", lambda e: e.tensor_scalar_add(out=pv[:, 3, :], in0=pv[:, 3, :], scalar1=1.0), reads=[r_const], writes=[r_const])
    ps = [p.psum(f"ps{i}", [128, 512]) for i in range(8)]
    r_ps = p.regions(8, "ps")
    pc = [0]

    def bank():
        i = pc[0] % 8
        pc[0] += 1
        return ps[i], r_ps[i]

    ntb = ntok // 512
    h2 = p.sbuf("h2", [128, KC, ntok], BF16)
    r_h = p.regions(KC, "h")
    for dc in range(KC):
        sg, rsg = stage()
        p.dma("sp", sg[:, 0:ntok], x1_d[dc * 128:(dc + 1) * 128, :], writes=[rsg])
        p.op("act", lambda e, dc=dc, sg=sg: e.activation(out=h2[:, dc, :], in_=sg[:, 0:ntok], func=AF.Identity, bias=pv[:, 4, dc:dc + 1], scale=pv[:, 3, dc:dc + 1]),
             reads=[rsg, r_const], writes=[r_h[dc]])
    accT = p.sbuf("accT", [128, KC, ntok], F32)
    r_acc = [[p.region(f"acc{dc}_{tb}") for tb in range(ntb)] for dc in range(KC)]
    actT = p.sbuf("actT", [128, 8, ntok], BF16)
    r_act = [[p.region(f"act{fc}_{tb}") for tb in range(ntb)] for fc in range(8)]
    wtb = [p.sbuf("wtb0", [128, ntok], F32)] * 2
    r_wtb = [p.region("wtb")] * 2
    tg = [p.sbuf(f"tg{i}", [128, 512], F32) for i in range(2)]
    tsg = [p.sbuf(f"tsg{i}", [128, 512], F32) for i in range(2)]
    tl = [p.sbuf(f"tl{i}", [128, 512], F32) for i in range(2)]
    r_tg, r_tsg, r_tl = p.regions(2, "tg"), p.regions(2, "tsg"), p.regions(2, "tl")
    ec = 0
    for ex in range(nexp):
        xi = ex % 2
        p.dma("sp", wtb[xi][:], wt_d[ex:ex + 1, :].partition_broadcast(128), writes=[r_wtb[xi]])
        for half in range(2):
            wg_t, rwg = load(wgu_d[ex][:, half * 512:(half + 1) * 512], 16)
            wl_t, rwl = load(wgu_d[ex][:, F + half * 512:F + (half + 1) * 512], 16)
            for fi in range(4):
                fc = half * 4 + fi
                cs = slice(fi * 128, (fi + 1) * 128)
                for tb in range(ntb):
                    ts_ = slice(tb * 512, (tb + 1) * 512)
                    i = ec % 2
                    ec += 1
                    pgb, rpg = bank()
                    plb, rpl = bank()
                    for kc in range(KC):
                        p.op("pe", lambda e, pgb=pgb, wg_t=wg_t, kc=kc, cs=cs, ts_=ts_: e.matmul(pgb[:, :], lhsT=wg_t[:, kc, cs], rhs=h2[:, kc, ts_],
                                                                                                  start=(kc == 0), stop=(kc == KC - 1)),
                             reads=[rwg, r_h[kc]], writes=[rpg])
                    for kc in range(KC):
                        p.op("pe", lambda e, plb=plb, wl_t=wl_t, kc=kc, cs=cs, ts_=ts_: e.matmul(plb[:, :], lhsT=wl_t[:, kc, cs], rhs=h2[:, kc, ts_],
                                                                                                  start=(kc == 0), stop=(kc == KC - 1)),
                             reads=[rwl, r_h[kc]], writes=[rpl])
                    p.op("dve", lambda e, pgb=pgb, i=i, ex=ex, fc=fc: e.tensor_scalar(out=tg[i][:], in0=pgb[:, :], scalar1=bgu[:, ex, fc:fc + 1], scalar2=cst[:, 1:2],
                                                                                       op0=ALU.add, op1=ALU.min),
                         reads=[rpg, r_const], writes=[r_tg[i]])
                    p.op("act", lambda e, i=i: e.activation(out=tsg[i][:], in_=tg[i][:], func=AF.Sigmoid, scale=1.702), reads=[r_tg[i]], writes=[r_tsg[i]])
                    p.op("dve", lambda e, plb=plb, i=i, ex=ex, fc=fc: e.tensor_scalar(out=tl[i][:], in0=plb[:, :], scalar1=bgu[:, ex, 8 + fc:9 + fc], scalar2=cst[:, 1:2],
                                                                                       op0=ALU.add, op1=ALU.min),
                         reads=[rpl, r_const], writes=[r_tl[i]])
                    p.op("pool", lambda e, i=i: e.tensor_scalar(out=tl[i][:], in0=tl[i][:], scalar1=cst[:, 2:3], scalar2=cst[:, 3:4], op0=ALU.max, op1=ALU.add),
                         reads=[r_tl[i], r_const], writes=[r_tl[i]])
                    p.op("pool", lambda e, i=i, xi=xi, ts_=ts_: e.tensor_tensor(out=tl[i][:], in0=tl[i][:], in1=wtb[xi][:, ts_], op=ALU.mult),
                         reads=[r_tl[i], r_wtb[xi]], writes=[r_tl[i]])
                    p.op("dve", lambda e, i=i: e.tensor_tensor(out=tg[i][:], in0=tg[i][:], in1=tsg[i][:], op=ALU.mult), reads=[r_tg[i], r_tsg[i]], writes=[r_tg[i]])
                    p.op("dve", lambda e, i=i, fc=fc, ts_=ts_: e.tensor_tensor(out=actT[:, fc, ts_], in0=tg[i][:], in1=tl[i][:], op=ALU.mult),
                         reads=[r_tg[i], r_tl[i]], writes=[r_act[fc][tb]])
        for dg in range(4):
            wd_t, rwd = load(wd_d[ex][:, dg * 512:(dg + 1) * 512], 8)
            for dci in range(4):
                dc = dg * 4 + dci
                cs = slice(dci * 128, (dci + 1) * 128)
                for tb in range(ntb):
                    ts_ = slice(tb * 512, (tb + 1) * 512)
                    pdb, rpd = bank()
                    for fc in range(8):
                        p.op("pe", lambda e, pdb=pdb, wd_t=wd_t, fc=fc, cs=cs, ts_=ts_, ex=ex: e.matmul(pdb[:, :], lhsT=wd_t[:, fc, cs], rhs=actT[:, fc, ts_],
                                                                                                         start=(fc == 0), stop=(fc == 7 and ex > 0)),
                             reads=[rwd, r_act[fc][tb]], writes=[rpd])
                    a_ap = accT[:, dc, ts_]
                    if ex == 0:
                        p.op("pe", lambda e, pdb=pdb, dc=dc, ts_=ts_: e.matmul(pdb[:, :], lhsT=bd[:, dc * 128:(dc + 1) * 128], rhs=wts[:, ts_], start=False, stop=True),
                             reads=[r_const], writes=[rpd])
                        p.op("act", lambda e, pdb=pdb, a_ap=a_ap: e.copy(out=a_ap, in_=pdb[:, :]), reads=[rpd], writes=[r_acc[dc][tb]])
                    else:
                        p.op("dve", lambda e, pdb=pdb, a_ap=a_ap: e.tensor_tensor(out=a_ap, in0=pdb[:, :], in1=a_ap, op=ALU.add),
                             reads=[rpd, r_acc[dc][tb]], writes=[r_acc[dc][tb]])
    r_z = p.regions(KC, "z")
    for dc in range(KC):
        sg, rsg = stage()
        p.dma("sp", sg[:, 0:ntok], x1_d[dc * 128:(dc + 1) * 128, :], writes=[rsg])
        p.op("act", lambda e, sg=sg: e.mul(out=sg[:, 0:ntok], in_=sg[:, 0:ntok], mul=ALPHA), reads=[rsg], writes=[rsg])
        p.op("dve", lambda e, dc=dc, sg=sg: e.scalar_tensor_tensor(out=accT[:, dc, :], in0=accT[:, dc, :], scalar=pv[:, 0, dc:dc + 1], in1=sg[:, 0:ntok],
                                                                    op0=ALU.mult, op1=ALU.add),
             reads=[rsg, r_const] + r_acc[dc], writes=[r_z[dc]])
    emit_ln(p, accT, r_z, ntok, lambda dc: pv[:, 1, dc:dc + 1], lambda dc: pv[:, 2, dc:dc + 1], onesD, r_const, cst[:, 0:1], bank, "ln2",
            tmps=[(tg[0], r_tg[0]), (tg[1], r_tg[1]), (tsg[0], r_tsg[0]), (tsg[1], r_tsg[1])])
    p.dma("sp", o_d.rearrange("(k p) t -> p k t", p=128), accT[:], reads=r_z, is_output=True)
    p.emit()
    return nc

import numpy as np
from concourse.bass_utils import run_bass_kernel_spmd

NCORES = 8
_cache = {}


def run_prog(key, builder, in_maps):
    if key not in _cache:
        _cache[key] = builder()
    res = run_bass_kernel_spmd(_cache[key], in_maps, core_ids=list(range(NCORES)))
    return res.results


def fm16(v):
    return np.ascontiguousarray(v.reshape(16, 128).T)


def host_pool(pv, pool_w, pool_scale):
    B, S, _ = pv.shape
    H = HALO
    pvT = np.zeros((B, 4, 128, S + H), np.float32)
    pvT[:, :, :, H:] = pv.reshape(B, S, 4, 128).transpose(0, 2, 3, 1)
    t = np.arange(S)
    inv = np.stack([1.0 / np.minimum(t + 1, w) for w in (2, 4, 8, 16)]).astype(np.float32)
    psc = np.ascontiguousarray(pool_scale.reshape(4, 128).T)
    in_maps = []
    for core in range(NCORES):
        b, q = core // 4, core % 4
        in_maps.append({"v": np.ascontiguousarray(pvT[b, :, :, q * 1024:(q + 1) * 1024 + H]),
                        "inv": np.ascontiguousarray(np.broadcast_to(inv[:, None, q * 1024:(q + 1) * 1024], (4, 128, 1024))),
                        "pw": np.ascontiguousarray(pool_w), "psc": psc})
    res = run_prog("pool", lambda: build_pool(1024), in_maps)
    ya = np.zeros((B, S, 512), np.float32)
    for core in range(NCORES):
        b, q = core // 4, core % 4
        ya[b, q * 1024:(q + 1) * 1024] = res[core]["o"].transpose(2, 0, 1).reshape(1024, 512)
    return ya


ATT_GROUPS = ((128, 1), (512, 4), (2048, 16))
ALIBI_SLOPES = tuple(2.0 ** (-8.0 * (h + 1) / 12) for h in range(12))


def att_bias_tables(hs):
    kk = np.arange(128)[:, None]
    qi = np.arange(128)[None, :]
    tabs = []
    for g, (window, dil) in enumerate(ATT_GROUPS):
        slope = ALIBI_SLOPES[g * 4 + hs]
        span = window // dil
        halves = []
        for kb in range(2):
            dist = qi + 128 - (kk + kb * 128)
            valid = (dist >= 0) & (dist <= span)
            halves.append(np.where(valid, -slope * dist * dil, -30000.0))
        tabs.append(np.concatenate(halves, axis=1))
    return np.stack(tabs).astype(np.float32)


def host_att(aq, ak, av):
    B, S, _ = aq.shape
    perms = []
    for window, dil in ATT_GROUPS:
        L = S // dil
        perms.append((np.arange(S).reshape(L, dil).T).reshape(-1))
    in_maps = []
    for core in range(NCORES):
        b, hs = core // 4, core % 4
        qT = np.zeros((3, 64, S), np.float32); kT = np.zeros((3, 64, S), np.float32); v = np.zeros((3, S, 64), np.float32)
        for g in range(3):
            c0 = g * 256 + hs * 64
            qT[g] = aq[b, perms[g], c0:c0 + 64].T
            kT[g] = ak[b, perms[g], c0:c0 + 64].T
            v[g] = av[b, perms[g], c0:c0 + 64]
        in_maps.append({"qT": qT, "kT": kT, "v": v, "bias": att_bias_tables(hs)})
    res = run_prog("att", build_att, in_maps)
    num = np.zeros((B, 3, S, 256), np.float32)
    den = np.zeros((B, 3, S, 4), np.float32)
    for core in range(NCORES):
        b, hs = core // 4, core % 4
        o = res[core]["o"].reshape(3, S, 65)
        for g in range(3):
            num[b, g, perms[g], hs * 64:(hs + 1) * 64] = o[g, :, 0:64]
            den[b, g, perms[g], hs] = o[g, :, 64]
    return num, den


def host_gla(gq, gk, gv, gg, galo, w_alpha, b_alpha, norm_g):
    B, S, _ = gq.shape
    mask = (np.arange(128)[:, None] <= np.arange(128)[None, :]).astype(np.float32)
    ident = np.eye(64, dtype=np.float32)
    in_maps = []
    for core in range(NCORES):
        b, h = core // 4, core % 4
        in_maps.append({
            "qT": np.ascontiguousarray(gq[b, :, h * 64:(h + 1) * 64].T),
            "kT": np.ascontiguousarray(gk[b, :, h * 64:(h + 1) * 64].T),
            "v": np.ascontiguousarray(gv[b, :, h * 128:(h + 1) * 128]),
            "g": np.ascontiguousarray(gg[b, :, h * 128:(h + 1) * 128]),
            "aloT": np.ascontiguousarray(galo[b].T),
            "wa": np.ascontiguousarray(w_alpha[:, h * 64:(h + 1) * 64]),
            "ba": np.ascontiguousarray(b_alpha[h * 64:(h + 1) * 64, None]),
            "ng": np.ascontiguousarray(np.broadcast_to(norm_g[None, h * 128:(h + 1) * 128], (128, 128))),
            "mask": mask, "ident": ident})
    res = run_prog("gla", build_gla, in_maps)
    yc = np.zeros((B, S, 512), np.float32)
    for core in range(NCORES):
        b, h = core // 4, core % 4
        yc[b, :, h * 128:(h + 1) * 128] = res[core]["o"]
    return yc


def host_rwkv(rp, mu, w0, w2, a0, a2, g2, k_k, k_a, r_k, ln_g, ln_b):
    B, S, _ = rp.shape
    W = S + 1
    blk = np.arange(128) // 64
    same = blk[:, None] == blk[None, :]
    ii = np.arange(128)
    bo = same.astype(np.float32)
    msl = (same & (ii[None, :] < ii[:, None])).astype(np.float32)
    msu = np.ascontiguousarray(msl.T)
    miu = (same & (ii[:, None] <= ii[None, :])).astype(np.float32)
    ident = np.eye(128, dtype=np.float32)
    rkf = r_k.reshape(512)

    def padT(a):
        o = np.zeros((a.shape[1], W), np.float32)
        o[:, 1:] = a.T
        return o

    in_maps = []
    for core in range(NCORES):
        b, hq = core // 4, core % 4
        cs = slice(128 * hq, 128 * hq + 128)
        x = rp[b]
        pv = np.zeros((128, 16), np.float32)
        for j, vec in enumerate((mu[0:512][cs], mu[512:1024][cs], mu[1024:1536][cs], w0[cs], a0[cs], k_k[cs], k_a[cs], rkf[cs], ln_g[cs], ln_b[cs])):
            pv[:, j] = vec
        vtok = np.zeros((W, 128), np.float32)
        vtok[1:] = x[:, 1024:1536][:, cs]
        in_maps.append({
            "r": padT(x[:, 0:512][:, cs]), "k": padT(x[:, 512:1024][:, cs]), "v": padT(x[:, 1024:1536][:, cs]),
            "lo1": padT(x[:, 1536:1600]), "lo2": padT(x[:, 1600:1696]), "vtok": vtok, "pv": pv,
            "mulo1": np.ascontiguousarray(mu[1536:1600, None]), "mulo2": np.ascontiguousarray(mu[1600:1696, None]),
            "muvt": np.ascontiguousarray(np.broadcast_to(mu[1024:1536][cs][None, :], (128, 128))),
            "w2a2": np.ascontiguousarray(np.concatenate([w2[:, cs], a2[:, cs]], 0)), "g2": np.ascontiguousarray(g2[:, cs]),
            "bo": bo, "msl": msl, "msu": msu, "miu": miu, "ident": ident})
    res = run_prog("rwkv", build_rwkv, in_maps)
    ydT = np.zeros((B, 512, S), np.float32)
    for core in range(NCORES):
        b, hq = core // 4, core % 4
        ydT[b, 128 * hq:128 * hq + 128] = res[core]["o"]
    return ydT


def tok_slices():
    return [(core // 4, slice((core % 4) * 1024, (core % 4 + 1) * 1024)) for core in range(NCORES)]


def host_c1(x, mod_l, w_in_l, wbr, ya, num, den, yc, ydT):
    wg = np.ascontiguousarray(w_in_l[:, 6064:])
    in_maps = []
    for b, ts in tok_slices():
        in_maps.append({
            "xT": np.ascontiguousarray(x[b, ts, :].T), "sc": fm16(mod_l[b, 2048:4096]), "sh": fm16(mod_l[b, 0:2048]),
            "wg": wg, "wbr": wbr,
            "yaT": np.ascontiguousarray(ya[b, ts, :].T), "ycT": np.ascontiguousarray(yc[b, ts, :].T),
            "ydT": np.ascontiguousarray(ydT[b, :, ts]),
            "ybn": np.ascontiguousarray(num[b, :, ts, :].transpose(0, 2, 1)),
            "ybd": np.ascontiguousarray(np.repeat(den[b, :, ts, :], 64, axis=2).transpose(0, 2, 1))})
    res = run_prog("c1", build_c1, in_maps)
    return [r["o"] for r in res]


def _pv(vecs):
    pv = np.zeros((128, 8, 16), np.float32)
    for j, v in enumerate(vecs):
        pv[:, j, :] = fm16(v)
    return pv


def host_c2(mT, x, mod_l, wout, ln_g, ln_b, router_w, router_b):
    ident = np.eye(128, dtype=np.float32)
    onesD = np.full((128, 128), 1.0 / 2048, np.float32)
    in_maps = []
    for core, (b, ts) in enumerate(tok_slices()):
        in_maps.append({
            "mT": mT[core], "xT": np.ascontiguousarray(x[b, ts, :].T), "wout": wout,
            "pv": _pv([mod_l[b, 4096:6144], ln_g, ln_b, mod_l[b, 8192:10240], mod_l[b, 6144:8192]]),
            "rw": router_w, "rb": np.ascontiguousarray(router_b[:, None]), "ident": ident, "onesD": onesD})
    res = run_prog("c2", build_c2, in_maps)
    return [r["x1T"] for r in res], [r["wtT"] for r in res]


def host_c3(x1T, wtT, mod_l, wgu, bgu, wd, bd, ln_g, ln_b):
    onesD = np.full((128, 128), 1.0 / 2048, np.float32)
    bgu_l = np.ascontiguousarray(bgu.reshape(32, 16, 128).transpose(2, 0, 1))
    in_maps = []
    for core, (b, ts) in enumerate(tok_slices()):
        in_maps.append({
            "x1T": x1T[core], "wtT": wtT[core],
            "pv": _pv([mod_l[b, 10240:12288], ln_g, ln_b, mod_l[b, 8192:10240], mod_l[b, 6144:8192]]),
            "wgu": wgu, "bgu": bgu_l, "wd": wd, "bd": bd, "onesD": onesD})
    res = run_prog("c3", build_c3, in_maps)
    return [r["o"] for r in res]


def host_p0(c, ada_w, ada_b):
    cT = np.ascontiguousarray(c.T.reshape(16, 128, 2).transpose(1, 0, 2))
    in_maps = []
    for core in range(NCORES):
        l, q = core // 4, core % 4
        in_maps.append({"cT": cT, "w": np.ascontiguousarray(ada_w[l][:, q * 3072:(q + 1) * 3072]),
                        "b": np.ascontiguousarray(ada_b[l][None, q * 3072:(q + 1) * 3072])})
    res = run_prog("p0", lambda: build_p0(3072), in_maps)
    mod = np.zeros((2, 2, 12288), np.float32)
    for core in range(NCORES):
        l, q = core // 4, core % 4
        mod[l][:, q * 3072:(q + 1) * 3072] = res[core]["o"]
    return mod


def host_a(x, mod_l, w_in_l):
    NC_ = 6064
    wA = np.ascontiguousarray(w_in_l[:, :NC_])
    in_maps = []
    for b, ts in tok_slices():
        in_maps.append({"xT": np.ascontiguousarray(x[b, ts, :].T), "sc": fm16(mod_l[b, 2048:4096]), "sh": fm16(mod_l[b, 0:2048]), "w": wA})
    res = run_prog("a", lambda: build_a(NC_, 1024), in_maps)
    P = np.zeros((x.shape[0], x.shape[1], NC_), np.float32)
    for core, (b, ts) in enumerate(tok_slices()):
        P[b, ts] = res[core]["o"].T
    return P


def kernel(**inputs):
    g = {k: np.asarray(v) for k, v in inputs.items()}
    x = g["x"].astype(np.float32, copy=False)
    B, S, D_ = x.shape
    mod = host_p0(g["c"], g["ada_w"], g["ada_b"])
    for l in range(2):
        L = lambda n: g[n][l]
        P = host_a(x, mod[l], L("w_in"))
        ya = host_pool(P[..., 0:512], L("pool_w"), L("pool_scale"))
        num, den = host_att(P[..., 512:1280], P[..., 1280:2048], P[..., 2048:2816])
        o = 2816
        yc = host_gla(P[..., o:o + 256], P[..., o + 256:o + 512], P[..., o + 512:o + 1024], P[..., o + 1024:o + 1536],
                      P[..., o + 1536:o + 1552], L("gla_w_alpha"), L("gla_b_alpha"), L("gla_norm_g"))
        ydT = host_rwkv(P[..., 4368:6064], L("rwkv_mu"), L("rwkv_w0"), L("rwkv_w2"), L("rwkv_a0"), L("rwkv_a2"), L("rwkv_g2"),
                        L("rwkv_k_k"), L("rwkv_k_a"), L("rwkv_r_k"), L("rwkv_ln_g"), L("rwkv_ln_b"))
        wbr = np.ascontiguousarray(np.concatenate([L("w_branch_a"), L("w_branch_b"), L("w_branch_c"), L("w_branch_d")], 0))
        mT = host_c1(x, mod[l], L("w_in"), wbr, ya, num, den, yc, ydT)
        x1T, wtT = host_c2(mT, x, mod[l], L("w_out"), L("ln1_g"), L("ln1_b"), L("router_w"), L("router_b"))
        x2T = host_c3(x1T, wtT, mod[l], L("w_gate_up"), L("b_gate_up"), L("w_down"), L("b_down"), L("ln2_g"), L("ln2_b"))
        xn = np.zeros((B, S, D_), np.float32)
        for core, (b, ts) in enumerate(tok_slices()):
            xn[b, ts] = x2T[core].T
        x = xn
    return x
```

```python
import contextlib
import numpy as np
import concourse.bass as bass
import concourse.mybir as mybir

F32 = mybir.dt.float32
BF16 = mybir.dt.bfloat16
I32 = mybir.dt.int32
U32 = mybir.dt.uint32
ALU = mybir.AluOpType
AF = mybir.ActivationFunctionType
AX = mybir.AxisListType

ENGINES = ("pe", "act", "dve", "pool", "sp")


class Region:
    __slots__ = ("name", "last_w", "readers", "sem", "dma_count")

    def __init__(self, name):
        self.name = name
        self.last_w = None
        self.readers = []
        self.sem = None
        self.dma_count = 0


class Instr:
    __slots__ = ("eng", "fn", "deps", "needed", "token", "is_dma", "home", "seq", "dma_val")

    def __init__(self, eng, fn):
        self.eng = eng
        self.fn = fn
        self.deps = []
        self.needed = False
        self.token = None
        self.is_dma = False
        self.home = None
        self.seq = 0
        self.dma_val = 0


class Prog:
    def __init__(self, nc):
        self.nc = nc
        self.es = contextlib.ExitStack()
        self.q = {e: [] for e in ENGINES}
        self.sems = {}
        self.nreg = 0
        self.out_dmas = []
        self.n_sem = 0

    def sbuf(self, name, shape, dt):
        return self.es.enter_context(self.nc.sbuf_tensor(name, list(shape), dt))

    def psum(self, name, shape, dt=F32):
        return self.es.enter_context(self.nc.psum_tensor(name, list(shape), dt))

    def region(self, name=None):
        self.nreg += 1
        return Region(name or f"r{self.nreg}")

    def regions(self, n, name="r"):
        return [self.region(f"{name}{i}") for i in range(n)]

    def _sem(self, name):
        self.n_sem += 1
        return self.es.enter_context(self.nc.semaphore(name))

    def _track(self, ins, reads, writes):
        deps = []
        for r in reads:
            if r.last_w is not None:
                deps.append(r.last_w)
        for w in writes:
            if w.last_w is not None:
                deps.append(w.last_w)
            deps.extend(w.readers)
        seen = set()
        for d in deps:
            if d is ins or id(d) in seen:
                continue
            seen.add(id(d))
            if d.eng == "pe" and ins.eng == "pe" and not d.is_dma and not ins.is_dma:
                continue
            ins.deps.append(d)
            d.needed = True
        for r in reads:
            r.readers.append(ins)
        for w in writes:
            w.last_w = ins
            w.readers = []

    def op(self, eng, fn, reads=(), writes=()):
        ins = Instr(eng, fn)
        self._track(ins, list(reads), list(writes))
        self.q[eng].append(ins)
        return ins

    def dma(self, eng, out, in_, reads=(), writes=(), home=None, is_output=False, **kw):
        reads = list(reads)
        writes = list(writes)
        if home is None:
            home = writes[0] if writes else reads[0]
        if home.sem is None:
            home.sem = self._sem("d_" + home.name)
        home.dma_count += 1
        val = 16 * home.dma_count

        def fn(e, out=out, in_=in_, kw=kw):
            return e.dma_start(out=out, in_=in_, **kw)

        ins = Instr(eng, fn)
        ins.is_dma = True
        ins.home = home
        ins.dma_val = val
        ins.token = (home.sem, val)
        self._track(ins, reads, writes)
        self.q[eng].append(ins)
        if is_output:
            self.out_dmas.append(ins)
        return ins

    def emit(self):
        nc = self.nc
        for d in self.out_dmas:
            d.needed = True
        fin = Instr("sp", None)
        fin.deps = list(self.out_dmas)
        for e in ENGINES:
            if self.q[e]:
                last = self.q[e][-1]
                if last is not fin and not last.is_dma:
                    last.needed = True
                    fin.deps.append(last)
        self.q["sp"].append(fin)
        for e in ENGINES:
            c = 0
            sem = None
            for ins in self.q[e]:
                if ins.is_dma or ins.fn is None:
                    continue
                if ins.needed:
                    if sem is None:
                        sem = self._sem("e_" + e)
                    c += 1
                    ins.token = (sem, c)
        block = self.es.enter_context(nc.Block())

        def run(e_name, eng):
            known = {}
            for ins in self.q[e_name]:
                need = {}
                for d in ins.deps:
                    s, v = d.token
                    if known.get(s.num, 0) >= v:
                        continue
                    if need.get(s.num, (None, 0))[1] < v:
                        need[s.num] = (s, v)
                for s, v in need.values():
                    eng.wait_ge(s, v)
                    known[s.num] = v
                if ins.fn is None:
                    continue
                r = ins.fn(eng)
                if ins.is_dma:
                    r.then_inc(ins.token[0], 16)
                elif ins.needed:
                    r.then_inc(ins.token[0], 1)

        if self.q["pe"]:
            @block.tensor
            def _(eng):
                run("pe", eng)
        if self.q["act"]:
            @block.scalar
            def _(eng):
                run("act", eng)
        if self.q["dve"]:
            @block.vector
            def _(eng):
                run("dve", eng)
        if self.q["pool"]:
            @block.gpsimd
            def _(eng):
                run("pool", eng)

        @block.sync
        def _(eng):
            run("sp", eng)

        self.es.close()


D = 2048
KC = 16


def build_p0(ncols=3072):
    nc = bass.Bass("TRN2", target_bir_lowering=False)
    cT_d = nc.dram_tensor("cT", [128, KC, 2], F32, kind="ExternalInput").ap()
    w_d = nc.dram_tensor("w", [D, ncols], F32, kind="ExternalInput").ap()
    b_d = nc.dram_tensor("b", [1, ncols], F32, kind="ExternalInput").ap()
    o_d = nc.dram_tensor("o", [2, ncols], F32, kind="ExternalOutput").ap()
    p = Prog(nc)
    cT = p.sbuf("cT_s", [128, KC, 2], F32)
    sc = p.sbuf("sc_s", [128, KC, 2], F32)
    bias = p.sbuf("bias_s", [1, ncols], F32)
    ones = p.sbuf("ones_s", [1, 2], F32)
    osb = p.sbuf("o_s", [2, ncols], F32)
    nt = ncols // 512
    wt = [p.sbuf(f"wt{i}", [128, KC, 512], F32) for i in range(2)]
    ps = [p.psum(f"ps{i}", [128, 512]) for i in range(2)]
    r_c, r_sc, r_b, r_ones, r_o = p.regions(5, "m")
    r_w = p.regions(2, "w")
    r_ps = p.regions(2, "ps")
    p.dma("sp", cT[:], cT_d, writes=[r_c])
    p.dma("sp", bias[:], b_d, writes=[r_b])
    p.op("dve", lambda e: e.memset(ones[:], 1.0), writes=[r_ones])
    p.op("act", lambda e: e.activation(out=sc[:], in_=cT[:], func=AF.Silu), reads=[r_c], writes=[r_sc])
    for t in range(nt):
        i = t % 2
        p.dma("sp", wt[i][:], w_d[:, t * 512:(t + 1) * 512].rearrange("(k p) n -> p k n", p=128), writes=[r_w[i]])
        for kc in range(KC):
            p.op("pe", lambda e, kc=kc, i=i: e.matmul(ps[i][0:2, :], lhsT=sc[:, kc, :], rhs=wt[i][:, kc, :],
                                                        start=(kc == 0), stop=False),
                 reads=[r_sc, r_w[i]], writes=[r_ps[i]])
        p.op("pe", lambda e, i=i, t=t: e.matmul(ps[i][0:2, :], lhsT=ones[:], rhs=bias[:, t * 512:(t + 1) * 512],
                                                  start=False, stop=True),
             reads=[r_ones, r_b], writes=[r_ps[i]])
        p.op("dve", lambda e, i=i, t=t: e.tensor_copy(out=osb[:, t * 512:(t + 1) * 512], in_=ps[i][0:2, :]),
             reads=[r_ps[i]], writes=[r_o])
    p.dma("sp", o_d, osb[:], reads=[r_o], is_output=True)
    p.emit()
    return nc


def build_a(ncols, ntok=1024):
    nc = bass.Bass("TRN2", target_bir_lowering=False)
    xT_d = nc.dram_tensor("xT", [D, ntok], F32, kind="ExternalInput").ap()
    sc_d = nc.dram_tensor("sc", [128, KC], F32, kind="ExternalInput").ap()
    sh_d = nc.dram_tensor("sh", [128, KC], F32, kind="ExternalInput").ap()
    w_d = nc.dram_tensor("w", [D, ncols], F32, kind="ExternalInput").ap()
    o_d = nc.dram_tensor("o", [ncols, ntok], F32, kind="ExternalOutput").ap()
    p = Prog(nc)
    emit_a(p, xT_d, sc_d, sh_d, w_d, o_d, ncols, ntok)
    p.emit()
    return nc


def emit_modulate(p, xT_d, sc_d, sh_d, ntok, name="m"):
    sc = p.sbuf(name + "sc", [128, KC], F32)
    sh = p.sbuf(name + "sh", [128, KC], F32)
    hT = p.sbuf(name + "hT", [128, KC, ntok], BF16)
    xs = [p.sbuf(f"{name}xs{i}", [128, 4, ntok], F32) for i in range(2)]
    r_sc = p.region(name + "sc")
    r_xs = p.regions(2, name + "xs")
    r_h = p.regions(KC, name + "h")
    p.dma("sp", sc[:], sc_d, writes=[r_sc])
    p.dma("sp", sh[:], sh_d, writes=[r_sc])
    p.op("dve", lambda e: e.tensor_scalar_add(out=sc[:], in0=sc[:], scalar1=1.0), reads=[r_sc], writes=[r_sc])
    for g in range(KC // 4):
        i = g % 2
        p.dma("sp", xs[i][:], xT_d[g * 512:(g + 1) * 512, :].rearrange("(k p) t -> p k t", p=128), writes=[r_xs[i]])
        for j in range(4):
            kc = g * 4 + j
            p.op("act", lambda e, i=i, j=j, kc=kc: e.activation(out=hT[:, kc, :], in_=xs[i][:, j, :], func=AF.Identity,
                                                                bias=sh[:, kc:kc + 1], scale=sc[:, kc:kc + 1]),
                 reads=[r_xs[i], r_sc], writes=[r_h[kc]])
    return hT, r_h


def emit_a(p, xT_d, sc_d, sh_d, w_d, o_d, ncols, ntok):
    hT, r_h = emit_modulate(p, xT_d, sc_d, sh_d, ntok)
    ws = [p.sbuf(f"ws{i}", [128, KC, 512], F32) for i in range(2)]
    wb = [p.sbuf(f"wb{i}", [128, KC, 512], BF16) for i in range(2)]
    ob = [p.sbuf(f"ob{i}", [128, 4, 512], F32) for i in range(2)]
    ps = [p.psum(f"ps{i}", [128, 512]) for i in range(4)]
    r_ws = p.regions(2, "ws")
    r_wb = p.regions(2, "wb")
    r_ob = [p.regions(4, f"ob{i}_") for i in range(2)]
    r_ps = p.regions(4, "ps")
    nct = (ncols + 511) // 512
    ntb = ntok // 512
    cnt = 0
    oi = 0
    for ct in range(nct):
        i = ct % 2
        c0 = ct * 512
        cw = min(512, ncols - c0)
        p.dma("sp", ws[i][:, :, 0:cw], w_d[:, c0:c0 + cw].rearrange("(k p) n -> p k n", p=128), writes=[r_ws[i]])
        p.op("dve", lambda e, i=i, cw=cw: e.tensor_copy(out=wb[i][:, 0:8, 0:cw], in_=ws[i][:, 0:8, 0:cw]),
             reads=[r_ws[i]], writes=[r_wb[i]])
        p.op("act", lambda e, i=i, cw=cw: e.copy(out=wb[i][:, 8:16, 0:cw], in_=ws[i][:, 8:16, 0:cw]),
             reads=[r_ws[i]], writes=[r_wb[i]])
        nsub = (cw + 127) // 128
        for tb in range(ntb):
            o = oi % 2
            oi += 1
            for s in range(nsub):
                m = min(128, cw - s * 128)
                b = cnt % 4
                cnt += 1
                for kc in range(KC):
                    p.op("pe", lambda e, b=b, i=i, s=s, m=m, kc=kc, tb=tb: e.matmul(
                        ps[b][0:m, :], lhsT=wb[i][:, kc, s * 128:s * 128 + m], rhs=hT[:, kc, tb * 512:(tb + 1) * 512],
                        start=(kc == 0), stop=(kc == KC - 1)),
                        reads=[r_wb[i], r_h[kc]], writes=[r_ps[b]])
                if cnt % 2 == 0:
                    p.op("act", lambda e, o=o, s=s, m=m, b=b: e.copy(out=ob[o][0:m, s, :], in_=ps[b][0:m, :]),
                         reads=[r_ps[b]], writes=[r_ob[o][s]])
                else:
                    p.op("dve", lambda e, o=o, s=s, m=m, b=b: e.tensor_copy(out=ob[o][0:m, s, :], in_=ps[b][0:m, :]),
                         reads=[r_ps[b]], writes=[r_ob[o][s]])
            nfull = cw // 128
            if nfull:
                p.dma("sp", o_d[c0:c0 + nfull * 128, tb * 512:(tb + 1) * 512].rearrange("(s p) t -> p s t", p=128),
                      ob[o][:, 0:nfull, :], reads=r_ob[o][0:nfull], is_output=True)
            rem = cw - nfull * 128
            if rem:
                p.dma("sp", o_d[c0 + nfull * 128:c0 + cw, tb * 512:(tb + 1) * 512],
                      ob[o][0:rem, nfull, :], reads=[r_ob[o][nfull]], is_output=True)


HALO = 16


def build_pool(ntok=1024):
    nc = bass.Bass("TRN2", target_bir_lowering=False)
    W = ntok + HALO
    v_d = nc.dram_tensor("v", [4, 128, W], F32, kind="ExternalInput").ap()
    inv_d = nc.dram_tensor("inv", [4, 128, ntok], F32, kind="ExternalInput").ap()
    pw_d = nc.dram_tensor("pw", [4, 128, 128], F32, kind="ExternalInput").ap()
    psc_d = nc.dram_tensor("psc", [128, 4], F32, kind="ExternalInput").ap()
    o_d = nc.dram_tensor("o", [4, 128, ntok], F32, kind="ExternalOutput").ap()
    p = Prog(nc)
    v = p.sbuf("v_s", [128, 4, W], F32)
    inv = p.sbuf("inv_s", [128, 4, ntok], F32)
    pw = p.sbuf("pw_s", [128, 4, 128], F32)
    psc = p.sbuf("psc_s", [128, 4], F32)
    ta = p.sbuf("ta", [128, 4, W], F32)
    tb = p.sbuf("tb", [128, 4, W], F32)
    df = p.sbuf("df", [128, 4, ntok], F32)
    ob = p.sbuf("ob", [128, 4, ntok], F32)
    ps = [p.psum(f"ps{i}", [128, 512]) for i in range(4)]
    r_v, r_inv, r_pw = p.regions(3, "in")
    r_ta = p.regions(4, "ta")
    r_tb = p.regions(4, "tb")
    r_df = p.regions(4, "df")
    r_ob = p.regions(4, "ob")
    r_ps = p.regions(4, "ps")
    p.dma("sp", v[:], v_d.rearrange("g p t -> p g t"), writes=[r_v])
    p.dma("sp", inv[:], inv_d.rearrange("g p t -> p g t"), writes=[r_inv])
    p.dma("sp", pw[:], pw_d.rearrange("g p t -> p g t"), writes=[r_pw])
    p.dma("sp", psc[:], psc_d, writes=[r_pw])
    p.op("pool", lambda e: e.memset(ta[:], 0.0), writes=r_ta)
    p.op("pool", lambda e: e.memset(tb[:], 0.0), writes=r_tb)
    cnt = 0
    for g in range(4):
        src, r_src = v, r_v
        bufs = [(ta, r_ta[g]), (tb, r_tb[g])]
        for j in range(g + 1):
            m = 1 << j
            dst, r_dst = bufs[j % 2]
            p.op("dve", lambda e, dst=dst, src=src, g=g, m=m: e.tensor_tensor(
                out=dst[:, g, m:W], in0=src[:, g, m:W], in1=src[:, g, 0:W - m], op=ALU.add),
                reads=[r_src], writes=[r_dst])
            src, r_src = dst, r_dst
        p.op("dve", lambda e, src=src, g=g: e.tensor_tensor(out=df[:, g, :], in0=src[:, g, HALO:W], in1=inv[:, g, :], op=ALU.mult),
             reads=[r_src, r_inv], writes=[r_df[g]])
        p.op("dve", lambda e, g=g: e.tensor_tensor(out=df[:, g, :], in0=df[:, g, :], in1=v[:, g, HALO:W], op=ALU.subtract),
             reads=[r_df[g], r_v], writes=[r_df[g]])
        for tbk in range(ntok // 512):
            b = cnt % 4
            cnt += 1
            p.op("pe", lambda e, b=b, g=g, tbk=tbk: e.matmul(ps[b][:, :], lhsT=pw[:, g, :], rhs=df[:, g, tbk * 512:(tbk + 1) * 512],
                                                             start=True, stop=True),
                 reads=[r_pw, r_df[g]], writes=[r_ps[b]])
            p.op("act", lambda e, b=b, g=g, tbk=tbk: e.activation(out=ob[:, g, tbk * 512:(tbk + 1) * 512], in_=ps[b][:, :],
                                                                  func=AF.Copy, scale=psc[:, g:g + 1]),
                 reads=[r_ps[b], r_pw], writes=[r_ob[g]])
    p.dma("sp", o_d.rearrange("g p t -> p g t"), ob[:], reads=r_ob, is_output=True)
    p.emit()
    return nc


NBLK = 32
ATT_GROUPS = ((128, 1), (512, 4), (2048, 16))


def build_att():
    nc = bass.Bass("TRN2", target_bir_lowering=False)
    S = 4096
    q_d = nc.dram_tensor("qT", [3, 64, S], F32, kind="ExternalInput").ap()
    k_d = nc.dram_tensor("kT", [3, 64, S], F32, kind="ExternalInput").ap()
    v_d = nc.dram_tensor("v", [3, S, 64], F32, kind="ExternalInput").ap()
    bias_d = nc.dram_tensor("bias", [3, 128, 256], F32, kind="ExternalInput").ap()
    o_d = nc.dram_tensor("o", [3, NBLK, 128, 65], F32, kind="ExternalOutput").ap()
    p = Prog(nc)
    qT = p.sbuf("qT_s", [64, 3, S], F32)
    kT = p.sbuf("kT_s", [64, 3, S], F32)
    vx = p.sbuf("vx", [128, 3, NBLK, 65], F32)
    bias = p.sbuf("bias_s", [128, 3, 256], F32)
    oall = p.sbuf("oall", [128, 3, NBLK, 65], F32)
    sc = [p.sbuf(f"sc{i}", [128, 256], F32) for i in range(2)]
    pT = [p.sbuf(f"pT{i}", [128, 256], F32) for i in range(2)]
    ps_s = [p.psum(f"pss{i}", [128, 512]) for i in range(2)]
    ps_o = [p.psum(f"pso{i}", [128, 512]) for i in range(2)]
    r_q = p.regions(3, "q")
    r_k = p.regions(3, "k")
    r_v = p.regions(3, "v")
    r_bias = p.region("bias")
    r_sc = p.regions(2, "sc")
    r_pT = p.regions(2, "pT")
    r_pss = p.regions(2, "pss")
    r_pso = p.regions(2, "pso")
    r_o = [p.regions(NBLK, f"o{g}_") for g in range(3)]
    p.dma("sp", bias[:], bias_d.rearrange("g p k -> p g k"), writes=[r_bias])
    p.op("pool", lambda e: e.memset(vx[:, :, :, 64:65], 1.0), writes=r_v)
    for g in range(3):
        p.dma("sp", qT[:, g, :], q_d[g], writes=[r_q[g]])
        p.dma("sp", kT[:, g, :], k_d[g], writes=[r_k[g]])
        p.dma("sp", vx[:, g, :, 0:64], v_d[g].rearrange("(n p) c -> p n c", p=128), writes=[r_v[g]])
    cnt = 0
    for g, (window, dil) in enumerate(ATT_GROUPS):
        bpr = NBLK // dil
        for blk in range(NBLK):
            i = cnt % 2
            cnt += 1
            has_prev = (blk % bpr) != 0
            k0 = 0 if has_prev else 1
            for kb in range(k0, 2):
                kblk = blk - 1 + kb
                p.op("pe", lambda e, i=i, g=g, kb=kb, kblk=kblk, blk=blk: e.matmul(
                    ps_s[i][:, kb * 128:(kb + 1) * 128], lhsT=kT[:, g, kblk * 128:(kblk + 1) * 128],
                    rhs=qT[:, g, blk * 128:(blk + 1) * 128], start=True, stop=True),
                    reads=[r_k[g], r_q[g]], writes=[r_pss[i]])
            lo = k0 * 128
            p.op("dve", lambda e, i=i, g=g, lo=lo: e.scalar_tensor_tensor(
                out=sc[i][:, lo:256], in0=ps_s[i][:, lo:256], scalar=0.125, in1=bias[:, g, lo:256],
                op0=ALU.mult, op1=ALU.add),
                reads=[r_pss[i], r_bias], writes=[r_sc[i]])
            p.op("act", lambda e, i=i, lo=lo: e.activation(out=pT[i][:, lo:256], in_=sc[i][:, lo:256], func=AF.Exp),
                 reads=[r_sc[i]], writes=[r_pT[i]])
            for kb in range(k0, 2):
                kblk = blk - 1 + kb
                p.op("pe", lambda e, i=i, g=g, kb=kb, kblk=kblk, k0=k0: e.matmul(
                    ps_o[i][:, 0:65], lhsT=pT[i][:, kb * 128:(kb + 1) * 128], rhs=vx[:, g, kblk, :],
                    start=(kb == k0), stop=(kb == 1)),
                    reads=[r_pT[i], r_v[g]], writes=[r_pso[i]])
            p.op("act", lambda e, i=i, g=g, blk=blk: e.copy(out=oall[:, g, blk, :], in_=ps_o[i][:, 0:65]),
                 reads=[r_pso[i]], writes=[r_o[g][blk]])
        p.dma("sp", o_d[g].rearrange("n p c -> p n c"), oall[:, g, :, :], reads=r_o[g], is_output=True)
    p.emit()
    return nc


GC = 128
GN = 32


def build_gla():
    nc = bass.Bass("TRN2", target_bir_lowering=False)
    S = 4096
    q_d = nc.dram_tensor("qT", [64, S], F32, kind="ExternalInput").ap()
    k_d = nc.dram_tensor("kT", [64, S], F32, kind="ExternalInput").ap()
    v_d = nc.dram_tensor("v", [S, 128], F32, kind="ExternalInput").ap()
    g_d = nc.dram_tensor("g", [S, 128], F32, kind="ExternalInput").ap()
    alo_d = nc.dram_tensor("aloT", [16, S], F32, kind="ExternalInput").ap()
    wa_d = nc.dram_tensor("wa", [16, 64], F32, kind="ExternalInput").ap()
    ba_d = nc.dram_tensor("ba", [64, 1], F32, kind="ExternalInput").ap()
    ng_d = nc.dram_tensor("ng", [128, 128], F32, kind="ExternalInput").ap()
    mask_d = nc.dram_tensor("mask", [128, 128], F32, kind="ExternalInput").ap()
    id_d = nc.dram_tensor("ident", [64, 64], F32, kind="ExternalInput").ap()
    o_d = nc.dram_tensor("o", [S, 128], F32, kind="ExternalOutput").ap()
    p = Prog(nc)
    qT = p.sbuf("qT_s", [64, GN, GC], F32)
    kT = p.sbuf("kT_s", [64, GN, GC], F32)
    bA = p.sbuf("bA", [64, GN, GC], F32)
    bB = p.sbuf("bB", [64, GN, GC], F32)
    bC = p.sbuf("bC", [64, GN, GC], F32)
    V = p.sbuf("V", [128, GN, 128], F32)
    G = p.sbuf("G", [128, GN, 128], F32)
    alo = p.sbuf("alo", [16, S], F32)
    wa = p.sbuf("wa_s", [16, 64], F32)
    ba = p.sbuf("ba_s", [64, 1], F32)
    ng = p.sbuf("ng_s", [128, 128], F32)
    mask = p.sbuf("mask_s", [128, 128], F32)
    ident = p.sbuf("ident_s", [64, 64], F32)
    kv = p.sbuf("kv", [64, GN, 128], F32)
    sall = p.sbuf("sall", [64, GN, 128], F32)
    at = [p.sbuf(f"at{i}", [128, 128], F32) for i in range(2)]
    kt = [p.sbuf(f"kt{i}", [128, 64], F32) for i in range(2)]
    ss = [p.sbuf(f"ss{i}", [128, 1], F32) for i in range(2)]
    sq = p.sbuf("sq", [128, 128], F32)
    eps = p.sbuf("eps", [128, 1], F32)
    psa = [p.psum(f"psa{i}", [128, 512]) for i in range(2)]
    psb = [p.psum(f"psb{i}", [128, 512]) for i in range(2)]
    psc = [p.psum(f"psc{i}", [128, 512]) for i in range(2)]
    pso = [p.psum(f"pso{i}", [128, 512]) for i in range(2)]
    R = p.region
    r_q, r_k, r_v, r_g, r_alo, r_c = R("q"), R("k"), R("v"), R("g"), R("alo"), R("c")
    r_bA, r_bB, r_bC = R("bA"), R("bB"), R("bC")
    r_kv = p.regions(GN, "kv")
    r_sall = p.regions(GN, "sall")
    r_at = p.regions(2, "at")
    r_kt = p.regions(2, "kt")
    r_ss = p.regions(2, "ss")
    r_sq = R("sq")
    r_psa, r_psb, r_psc, r_pso = p.regions(2, "psa"), p.regions(2, "psb"), p.regions(2, "psc"), p.regions(2, "pso")
    r_y = p.regions(GN, "y")
    flat = lambda t: t[:].rearrange("p n c -> p (n c)")
    p.dma("sp", flat(qT), q_d, writes=[r_q])
    p.dma("sp", flat(kT), k_d, writes=[r_k])
    p.dma("sp", alo[:], alo_d, writes=[r_alo])
    for t, d_ in ((wa, wa_d), (ba, ba_d), (ng, ng_d), (mask, mask_d), (ident, id_d)):
        p.dma("sp", t[:], d_, writes=[r_c])
    p.dma("sp", V[:], v_d.rearrange("(n p) c -> p n c", p=128), writes=[r_v])
    p.dma("sp", G[:], g_d.rearrange("(n p) c -> p n c", p=128), writes=r_y + [r_g])
    p.op("dve", lambda e: e.tensor_scalar(out=ba[:], in0=ba[:], scalar1=-1.0, scalar2=None, op0=ALU.mult), reads=[r_c], writes=[r_c])
    p.op("pool", lambda e: e.memset(sall[:, 0, :], 0.0), writes=[r_sall[0]])
    p.op("pool", lambda e: e.memset(eps[:], 1e-6), writes=[r_c])
    fA = flat(bA)
    for tb in range(8):
        i = tb % 2
        sl = slice(tb * 512, (tb + 1) * 512)
        p.op("pe", lambda e, i=i, sl=sl: e.matmul(psa[i][0:64, :], lhsT=wa[:], rhs=alo[:, sl], start=True, stop=True),
             reads=[r_c, r_alo], writes=[r_psa[i]])
        p.op("act", lambda e, i=i, sl=sl: e.activation(out=fA[:, sl], in_=psa[i][0:64, :], func=AF.Exp, bias=ba[:], scale=-1.0),
             reads=[r_psa[i], r_c], writes=[r_bA])
    p.op("act", lambda e: e.activation(out=fA, in_=fA, func=AF.Ln, bias=1.0, scale=1.0), reads=[r_bA], writes=[r_bA])
    p.op("dve", lambda e: e.tensor_scalar(out=fA, in0=fA, scalar1=-1.0 / 16.0, scalar2=None, op0=ALU.mult), reads=[r_bA], writes=[r_bA])
    src, r_src, dst, r_dst = bA, r_bA, bB, r_bB
    m = 1
    while m < GC:
        p.op("dve", lambda e, src=src, dst=dst, m=m: e.tensor_tensor(out=dst[:, :, m:GC], in0=src[:, :, m:GC], in1=src[:, :, 0:GC - m], op=ALU.add),
             reads=[r_src], writes=[r_dst])
        p.op("pool", lambda e, src=src, dst=dst, m=m: e.tensor_copy(out=dst[:, :, 0:m], in_=src[:, :, 0:m]),
             reads=[r_src], writes=[r_dst])
        src, r_src, dst, r_dst = dst, r_dst, src, r_src
        m *= 2
    bb, r_bb, oth, r_oth = src, r_src, dst, r_dst
    p.op("act", lambda e: e.activation(out=flat(bC), in_=flat(bb), func=AF.Exp), reads=[r_bb], writes=[r_bC])
    p.op("act", lambda e: e.activation(out=flat(oth), in_=flat(bb), func=AF.Exp, scale=-1.0), reads=[r_bb], writes=[r_oth])
    p.op("dve", lambda e: e.scalar_tensor_tensor(out=flat(qT), in0=flat(qT), scalar=0.125, in1=flat(bC), op0=ALU.mult, op1=ALU.mult),
         reads=[r_q, r_bC], writes=[r_q])
    p.op("pool", lambda e: e.tensor_tensor(out=flat(kT), in0=flat(kT), in1=flat(oth), op=ALU.mult), reads=[r_k, r_oth], writes=[r_k])
    p.op("dve", lambda e: e.tensor_tensor(out=bb[:], in0=kT[:], in1=bC[:, :, GC - 1:GC].to_broadcast([64, GN, GC]), op=ALU.mult),
         reads=[r_k, r_bC, r_bb], writes=[r_bb])
    kp = bb
    r_kp = r_bb
    p.op("act", lambda e: e.activation(out=G[:], in_=G[:], func=AF.Silu), reads=[r_g], writes=[r_g])
    p.op("pool", lambda e: e.tensor_tensor(out=G[:], in0=G[:], in1=ng[:].unsqueeze(1).to_broadcast([128, GN, 128]), op=ALU.mult),
         reads=[r_g, r_c], writes=[r_g])
    for c in range(GN):
        i = c % 2
        p.op("pe", lambda e, i=i, c=c: e.matmul(psa[i][:, 0:128], lhsT=kT[:, c, :], rhs=qT[:, c, :], start=True, stop=True),
             reads=[r_k, r_q], writes=[r_psa[i]])
        p.op("pe", lambda e, i=i, c=c: e.transpose(psb[i][:, 0:64], kp[:, c, :], ident[:]),
             reads=[r_kp, r_c], writes=[r_psb[i]])
        p.op("act", lambda e, i=i: e.copy(out=kt[i][:], in_=psb[i][:, 0:64]), reads=[r_psb[i]], writes=[r_kt[i]])
        p.op("pe", lambda e, i=i, c=c: e.matmul(psc[i][0:64, 0:128], lhsT=kt[i][:], rhs=V[:, c, :], start=True, stop=True),
             reads=[r_kt[i], r_v], writes=[r_psc[i]])
        p.op("act", lambda e, i=i, c=c: e.copy(out=kv[:, c, :], in_=psc[i][0:64, 0:128]), reads=[r_psc[i]], writes=[r_kv[c]])
        if c + 1 < GN:
            p.op("dve", lambda e, c=c: e.scalar_tensor_tensor(out=sall[:, c + 1, :], in0=sall[:, c, :], scalar=bC[:, c, GC - 1:GC],
                                                               in1=kv[:, c, :], op0=ALU.mult, op1=ALU.add),
                 reads=[r_sall[c], r_bC, r_kv[c]], writes=[r_sall[c + 1]])
        p.op("dve", lambda e, i=i: e.tensor_tensor(out=at[i][:], in0=psa[i][:, 0:128], in1=mask[:], op=ALU.mult),
             reads=[r_psa[i], r_c], writes=[r_at[i]])
        p.op("pe", lambda e, i=i, c=c: e.matmul(pso[i][:, 0:128], lhsT=at[i][:], rhs=V[:, c, :], start=True, stop=False),
             reads=[r_at[i], r_v], writes=[r_pso[i]])
        p.op("pe", lambda e, i=i, c=c: e.matmul(pso[i][:, 0:128], lhsT=qT[:, c, :], rhs=sall[:, c, :], start=False, stop=True),
             reads=[r_q, r_sall[c]], writes=[r_pso[i]])
        p.op("act", lambda e, i=i: e.activation(out=sq[:], in_=pso[i][:, 0:128], func=AF.Square, accum_out=ss[i][:]),
             reads=[r_pso[i]], writes=[r_sq, r_ss[i]])
        p.op("act", lambda e, i=i: e.activation(out=ss[i][:], in_=ss[i][:], func=AF.Sqrt, bias=eps[:], scale=1.0 / 128.0),
             reads=[r_ss[i], r_c], writes=[r_ss[i]])
        p.op("dve", lambda e, i=i: e.reciprocal(out=ss[i][:], in_=ss[i][:]), reads=[r_ss[i]], writes=[r_ss[i]])
        p.op("dve", lambda e, i=i, c=c: e.scalar_tensor_tensor(out=G[:, c, :], in0=pso[i][:, 0:128], scalar=ss[i][:], in1=G[:, c, :],
                                                                op0=ALU.mult, op1=ALU.mult),
             reads=[r_pso[i], r_ss[i], r_g], writes=[r_y[c]])
    p.dma("sp", o_d.rearrange("(n p) c -> p n c", p=128), G[:], reads=r_y, is_output=True)
    p.emit()
    return nc


RS = 4096
RC_ = 64
NPAIR = 32
E05 = 0.6065306597126334


def build_rwkv():
    nc = bass.Bass("TRN2", target_bir_lowering=False)
    S = RS
    W = S + 1
    din = {}
    for nm, shp in (("r", [128, W]), ("k", [128, W]), ("v", [128, W]), ("lo1", [64, W]), ("lo2", [96, W]),
                    ("vtok", [W, 128]), ("pv", [128, 16]), ("mulo1", [64, 1]), ("mulo2", [96, 1]), ("muvt", [128, 128]),
                    ("w2a2", [64, 128]), ("g2", [96, 128]), ("bo", [128, 128]), ("msl", [128, 128]), ("msu", [128, 128]),
                    ("miu", [128, 128]), ("ident", [128, 128])):
        din[nm] = nc.dram_tensor(nm, shp, F32, kind="ExternalInput").ap()
    o_d = nc.dram_tensor("o", [128, S], F32, kind="ExternalOutput").ap()
    p = Prog(nc)
    B = [p.sbuf(f"B{i}", [128, W], F32) for i in range(9)]
    rB = p.regions(9, "B")
    VT = p.sbuf("VT", [128, NPAIR, 128], F32)
    r_VT = p.region("VT")
    small = {}
    r_small = p.region("small")
    for nm in ("pv", "mulo1", "mulo2", "muvt", "w2a2", "g2", "bo", "msl", "msu", "miu", "ident"):
        shp = list(din[nm].shape)
        small[nm] = p.sbuf(nm + "_s", shp, F32)
        p.dma("sp", small[nm][:], din[nm], writes=[r_small])
    pv, bo, msl, msu, miu, ident = (small[n] for n in ("pv", "bo", "msl", "msu", "miu", "ident"))
    p.op("dve", lambda e: e.tensor_scalar(out=pv[:, 10:11], in0=pv[:, 6:7], scalar1=-1.0, scalar2=1.0, op0=ALU.mult, op1=ALU.add),
         reads=[r_small], writes=[r_small])
    p.op("pool", lambda e: e.memset(pv[:, 11:12], 64e-5), writes=[r_small])
    p.op("pool", lambda e: e.memset(pv[:, 12:13], 0.0), writes=[r_small])
    egc = p.sbuf("egc", [128, 64], F32)
    egl = p.sbuf("egl", [64, 2, 64], F32)
    r_egc, r_egl = p.region("egc"), p.region("egl")
    hall = p.sbuf("hall", [64, 2, 4, 64], F32)
    r_hall = [[p.region(f"hall{h}_{c}") for c in range(4)] for h in range(2)]
    ps = [p.psum(f"ps{i}", [128, 512]) for i in range(8)]
    r_ps = p.regions(8, "ps")
    pc = [0]

    def bank():
        i = pc[0] % 8
        pc[0] += 1
        return ps[i], r_ps[i]

    R_, K_, V_, T1, L1, L2, A_, G_, GT = range(9)
    p.dma("sp", B[R_][:], din["r"], writes=[rB[R_]])
    p.dma("sp", B[K_][:], din["k"], writes=[rB[K_]])
    p.dma("sp", B[V_][:], din["v"], writes=[rB[V_]])
    p.dma("sp", B[L1][0:64, :], din["lo1"], writes=[rB[L1]])
    p.dma("sp", B[L2][0:96, :], din["lo2"], writes=[rB[L2]])
    p.dma("sp", VT[:], din["vtok"][1:W, :].rearrange("(n p) c -> p n c", p=128), writes=[r_VT])
    g7 = B[G_][:, 0:S].rearrange("p (n c) -> p n c", c=128)
    p.dma("sp", g7, din["vtok"][0:S, :].rearrange("(n p) c -> p n c", p=128), writes=[rB[G_]])
    muv_b = small["muvt"][:].unsqueeze(1).to_broadcast([128, NPAIR, 128])
    p.op("dve", lambda e: e.tensor_tensor(out=g7, in0=g7, in1=VT[:], op=ALU.subtract), reads=[rB[G_], r_VT], writes=[rB[G_]])
    p.op("pool", lambda e: e.tensor_tensor(out=g7, in0=g7, in1=muv_b, op=ALU.mult), reads=[rB[G_], r_small], writes=[rB[G_]])
    p.op("dve", lambda e: e.tensor_tensor(out=VT[:], in0=VT[:], in1=g7, op=ALU.add), reads=[rB[G_], r_VT], writes=[r_VT])

    def shift(bi, np_, mu_ap):
        p.op("dve", lambda e: e.tensor_tensor(out=B[T1][0:np_, 1:W], in0=B[bi][0:np_, 0:S], in1=B[bi][0:np_, 1:W], op=ALU.subtract),
             reads=[rB[bi]], writes=[rB[T1]])
        p.op("dve", lambda e: e.scalar_tensor_tensor(out=B[bi][0:np_, 1:W], in0=B[T1][0:np_, 1:W], scalar=mu_ap, in1=B[bi][0:np_, 1:W],
                                                      op0=ALU.mult, op1=ALU.add),
             reads=[rB[T1], rB[bi], r_small], writes=[rB[bi]])

    shift(R_, 128, pv[:, 0:1])
    shift(K_, 128, pv[:, 1:2])
    shift(V_, 128, pv[:, 2:3])
    shift(L1, 64, small["mulo1"][:])
    shift(L2, 96, small["mulo2"][:])
    v1 = lambda bi: B[bi][:, 1:W]
    p.op("act", lambda e: e.activation(out=B[L1][0:32, 1:W], in_=B[L1][0:32, 1:W], func=AF.Tanh), reads=[rB[L1]], writes=[rB[L1]])
    p.op("act", lambda e: e.activation(out=B[L2][0:96, 1:W], in_=B[L2][0:96, 1:W], func=AF.Sigmoid), reads=[rB[L2]], writes=[rB[L2]])
    w2a2 = small["w2a2"]
    for tb in range(8):
        sl = slice(1 + tb * 512, 1 + (tb + 1) * 512)
        pt, rp = bank()
        p.op("pe", lambda e, pt=pt, sl=sl: e.matmul(pt[:, :], lhsT=w2a2[0:32, :], rhs=B[L1][0:32, sl], start=True, stop=True),
             reads=[r_small, rB[L1]], writes=[rp])
        p.op("act", lambda e, pt=pt, sl=sl: e.activation(out=B[G_][:, sl], in_=pt[:, :], func=AF.Sigmoid, bias=pv[:, 3:4], scale=1.0),
             reads=[rp, r_small], writes=[rB[G_]])
        pt, rp = bank()
        p.op("pe", lambda e, pt=pt, sl=sl: e.matmul(pt[:, :], lhsT=w2a2[32:64, :], rhs=B[L1][32:64, sl], start=True, stop=True),
             reads=[r_small, rB[L1]], writes=[rp])
        p.op("act", lambda e, pt=pt, sl=sl: e.activation(out=B[A_][:, sl], in_=pt[:, :], func=AF.Sigmoid, bias=pv[:, 4:5], scale=1.0),
             reads=[rp, r_small], writes=[rB[A_]])
        pt, rp = bank()
        p.op("pe", lambda e, pt=pt, sl=sl: e.matmul(pt[:, :], lhsT=small["g2"][:], rhs=B[L2][0:96, sl], start=True, stop=True),
             reads=[r_small, rB[L2]], writes=[rp])
        p.op("dve", lambda e, pt=pt, sl=sl: e.tensor_copy(out=B[GT][:, sl], in_=pt[:, :]), reads=[rp], writes=[rB[GT]])
    p.op("dve", lambda e: e.tensor_scalar(out=v1(G_), in0=v1(G_), scalar1=-E05, scalar2=None, op0=ALU.mult), reads=[rB[G_]], writes=[rB[G_]])
    KK = L1
    p.op("dve", lambda e: e.tensor_scalar(out=v1(KK), in0=v1(K_), scalar1=pv[:, 5:6], scalar2=None, op0=ALU.mult),
         reads=[rB[K_], r_small, rB[KK]], writes=[rB[KK]])
    p.op("act", lambda e: e.activation(out=v1(T1), in_=v1(KK), func=AF.Square), reads=[rB[KK], rB[T1]], writes=[rB[T1]])
    for tb in range(8):
        sl = slice(1 + tb * 512, 1 + (tb + 1) * 512)
        pt, rp = bank()
        p.op("pe", lambda e, pt=pt, sl=sl: e.matmul(pt[:, :], lhsT=bo[:], rhs=B[T1][:, sl], start=True, stop=True),
             reads=[r_small, rB[T1]], writes=[rp])
        p.op("act", lambda e, pt=pt, sl=sl: e.activation(out=B[L2][:, sl], in_=pt[:, :], func=AF.Sqrt, bias=pv[:, 12:13], scale=1.0),
             reads=[rp, r_small], writes=[rB[L2]])
    p.op("dve", lambda e: e.tensor_scalar(out=v1(L2), in0=v1(L2), scalar1=1e-12, scalar2=None, op0=ALU.max), reads=[rB[L2]], writes=[rB[L2]])
    p.op("dve", lambda e: e.reciprocal(out=v1(L2), in_=v1(L2)), reads=[rB[L2]], writes=[rB[L2]])
    p.op("dve", lambda e: e.tensor_tensor(out=v1(KK), in0=v1(KK), in1=v1(L2), op=ALU.mult), reads=[rB[KK], rB[L2]], writes=[rB[KK]])
    p.op("dve", lambda e: e.tensor_scalar(out=v1(T1), in0=v1(A_), scalar1=pv[:, 6:7], scalar2=pv[:, 10:11], op0=ALU.mult, op1=ALU.add),
         reads=[rB[A_], r_small, rB[T1]], writes=[rB[T1]])
    p.op("dve", lambda e: e.tensor_tensor(out=v1(K_), in0=v1(K_), in1=v1(T1), op=ALU.mult), reads=[rB[K_], rB[T1]], writes=[rB[K_]])
    p.op("dve", lambda e: e.scalar_tensor_tensor(out=v1(T1), in0=v1(R_), scalar=pv[:, 7:8], in1=v1(K_), op0=ALU.mult, op1=ALU.mult),
         reads=[rB[R_], rB[K_], r_small, rB[T1]], writes=[rB[T1]])
    for tb in range(8):
        sl = slice(1 + tb * 512, 1 + (tb + 1) * 512)
        pt, rp = bank()
        p.op("pe", lambda e, pt=pt, sl=sl: e.matmul(pt[:, :], lhsT=bo[:], rhs=B[T1][:, sl], start=True, stop=True),
             reads=[r_small, rB[T1]], writes=[rp])
        p.op("dve", lambda e, pt=pt, sl=sl: e.tensor_tensor(out=B[V_][:, sl], in0=pt[:, :], in1=B[V_][:, sl], op=ALU.mult),
             reads=[rp, rB[V_]], writes=[rB[V_]])
    c3 = lambda bi: B[bi][:, 1:W].rearrange("p (n c) -> p n c", c=RC_)
    src, dst = G_, T1
    m = 1
    while m < RC_:
        p.op("dve", lambda e, src=src, dst=dst, m=m: e.tensor_tensor(out=c3(dst)[:, :, m:RC_], in0=c3(src)[:, :, m:RC_],
                                                                     in1=c3(src)[:, :, 0:RC_ - m], op=ALU.add),
             reads=[rB[src], rB[dst]], writes=[rB[dst]])
        p.op("pool", lambda e, src=src, dst=dst, m=m: e.tensor_copy(out=c3(dst)[:, :, 0:m], in_=c3(src)[:, :, 0:m]),
             reads=[rB[src], rB[dst]], writes=[rB[dst]])
        src, dst = dst, src
        m *= 2
    assert src == G_
    EG, ENG = L2, T1
    p.op("act", lambda e: e.activation(out=v1(EG), in_=v1(G_), func=AF.Exp), reads=[rB[G_], rB[EG]], writes=[rB[EG]])
    p.op("act", lambda e: e.activation(out=v1(ENG), in_=v1(G_), func=AF.Exp, scale=-1.0), reads=[rB[G_], rB[ENG]], writes=[rB[ENG]])
    p.op("pool", lambda e: e.tensor_copy(out=egc[:], in_=c3(EG)[:, :, RC_ - 1]), reads=[rB[EG]], writes=[r_egc])
    p.dma("sp", egl[:, 0, :], egc[0:64, :], reads=[r_egc], writes=[r_egl])
    p.dma("sp", egl[:, 1, :], egc[64:128, :], reads=[r_egc], writes=[r_egl])
    p.op("dve", lambda e: e.tensor_tensor(out=v1(R_), in0=v1(R_), in1=v1(EG), op=ALU.mult), reads=[rB[R_], rB[EG]], writes=[rB[R_]])
    p.op("pool", lambda e: e.tensor_tensor(out=v1(K_), in0=v1(K_), in1=v1(ENG), op=ALU.mult), reads=[rB[K_], rB[ENG]], writes=[rB[K_]])
    p.op("dve", lambda e: e.tensor_tensor(out=v1(A_), in0=v1(A_), in1=v1(KK), op=ALU.mult), reads=[rB[A_], rB[KK]], writes=[rB[A_]])
    p.op("dve", lambda e: e.scalar_tensor_tensor(out=v1(A_), in0=v1(A_), scalar=-1.0, in1=v1(ENG), op0=ALU.mult, op1=ALU.mult),
         reads=[rB[A_], rB[ENG]], writes=[rB[A_]])
    p.op("dve", lambda e: e.tensor_tensor(out=c3(KK)[:, :, 1:RC_], in0=c3(KK)[:, :, 1:RC_], in1=c3(EG)[:, :, 0:RC_ - 1], op=ALU.mult),
         reads=[rB[KK], rB[EG]], writes=[rB[KK]])
    BT = KK
    YB = [G_, L2]
    for h in range(2):
        p.op("pool", lambda e, h=h: e.memset(hall[:, h, 0, :], 0.0), writes=[r_hall[h][0]])

    def tiles(nm, shp):
        return [p.sbuf(f"{nm}{h}", shp, F32) for h in range(2)], p.regions(2, nm)

    Pa = [[p.sbuf(f"Pa{h}{i}", [128, 128], F32) for i in range(2)] for h in range(2)]
    Pb = [[p.sbuf(f"Pb{h}{i}", [128, 128], F32) for i in range(2)] for h in range(2)]
    X = [[p.sbuf(f"X{h}{i}", [128, 128], F32) for i in range(2)] for h in range(2)]
    rPa = [p.regions(2, f"Pa{h}") for h in range(2)]
    rPb = [p.regions(2, f"Pb{h}") for h in range(2)]
    rX = [p.regions(2, f"X{h}") for h in range(2)]
    AkbT, rAkbT = tiles("AkbT", [128, 128])
    RCt, rRCt = tiles("RCt", [128, 128])
    TBZ, rTBZ = tiles("TBZ", [128, 128])
    ACf, rACf = tiles("ACf", [128, 128])
    KCf, rKCf = tiles("KCf", [128, 128])
    ACt, rACt = tiles("ACt", [128, 64])
    KCt, rKCt = tiles("KCt", [128, 64])
    MT = [[p.sbuf(f"MT{h}{j}", [64, 64], F32) for j in range(2)] for h in range(2)]
    NN = [[p.sbuf(f"NN{h}{j}", [64, 64], F32) for j in range(2)] for h in range(2)]
    rMT = [p.regions(2, f"MT{h}") for h in range(2)]
    rNN = [p.regions(2, f"NN{h}") for h in range(2)]
    AarT, rAarT = tiles("AarT", [128, 128])
    AkrT, rAkrT = tiles("AkrT", [128, 128])
    PT, rPT = tiles("PT", [64, 128])

    def unit(h, pr):
        hs = slice(64 * h, 64 * h + 64)
        tk = slice(1 + pr * 128, 1 + (pr + 1) * 128)
        Bt, At, Kt, Rt = B[BT][hs, tk], B[A_][hs, tk], B[K_][hs, tk], B[R_][hs, tk]
        rBt, rAt, rKt, rRt = rB[BT], rB[A_], rB[K_], rB[R_]
        Vt = VT[:, pr, hs]
        pt, rp = bank()
        p.op("pe", lambda e: e.matmul(pt[:, 0:128], lhsT=Bt, rhs=At, start=True, stop=True), reads=[rBt, rAt], writes=[rp])
        p.op("dve", lambda e: e.tensor_tensor(out=Pa[h][0][:], in0=pt[:, 0:128], in1=msl[:], op=ALU.mult), reads=[rp, r_small], writes=[rPa[h][0]])
        pt2, rp2 = bank()
        p.op("pe", lambda e: e.matmul(pt2[:, 0:128], lhsT=At, rhs=Bt, start=True, stop=True), reads=[rBt, rAt], writes=[rp2])
        p.op("dve", lambda e: e.tensor_tensor(out=Pb[h][0][:], in0=pt2[:, 0:128], in1=msu[:], op=ALU.mult), reads=[rp2, r_small], writes=[rPb[h][0]])
        p.op("pool", lambda e: e.tensor_tensor(out=X[h][0][:], in0=Pb[h][0][:], in1=ident[:], op=ALU.add), reads=[rPb[h][0], r_small], writes=[rX[h][0]])
        yield
        pt3, rp3 = bank()
        p.op("pe", lambda e: e.matmul(pt3[:, 0:128], lhsT=Kt, rhs=Bt, start=True, stop=True), reads=[rKt, rBt], writes=[rp3])
        p.op("dve", lambda e: e.tensor_tensor(out=AkbT[h][:], in0=pt3[:, 0:128], in1=msu[:], op=ALU.mult), reads=[rp3, r_small], writes=[rAkbT[h]])
        pt4, rp4 = bank()
        p.op("pe", lambda e: e.transpose(pt4[:, 0:64], Bt, ident[hs, hs]), reads=[rBt, r_small], writes=[rp4])
        p.op("act", lambda e: e.copy(out=RCt[h][:, 0:64], in_=pt4[:, 0:64]), reads=[rp4], writes=[rRCt[h]])
        pt5, rp5 = bank()
        p.op("pe", lambda e: e.matmul(pt5[:, 0:64], lhsT=AkbT[h][:], rhs=Vt, start=True, stop=True), reads=[rAkbT[h], r_VT], writes=[rp5])
        p.op("act", lambda e: e.copy(out=RCt[h][:, 64:128], in_=pt5[:, 0:64]), reads=[rp5], writes=[rRCt[h]])
        yield
        cur = 0
        for k in range(1, 6):
            nxt = 1 - cur
            pa, rpa = bank()
            p.op("pe", lambda e, pa=pa, cur=cur: e.matmul(pa[:, 0:128], lhsT=Pb[h][cur][:], rhs=Pa[h][cur][:], start=True, stop=True),
                 reads=[rPb[h][cur], rPa[h][cur]], writes=[rpa])
            if k < 5:
                pb, rpb = bank()
                p.op("pe", lambda e, pb=pb, cur=cur: e.matmul(pb[:, 0:128], lhsT=Pa[h][cur][:], rhs=Pb[h][cur][:], start=True, stop=True),
                     reads=[rPb[h][cur], rPa[h][cur]], writes=[rpb])
            p.op("act", lambda e, pa=pa, nxt=nxt: e.copy(out=Pa[h][nxt][:], in_=pa[:, 0:128]), reads=[rpa], writes=[rPa[h][nxt]])
            if k < 5:
                p.op("dve", lambda e, pb=pb, nxt=nxt: e.tensor_copy(out=Pb[h][nxt][:], in_=pb[:, 0:128]), reads=[rpb], writes=[rPb[h][nxt]])
            px, rpx = bank()
            p.op("pe", lambda e, px=px, cur=cur, nxt=nxt: e.matmul(px[:, 0:128], lhsT=Pa[h][nxt][:], rhs=X[h][cur][:], start=True, stop=True),
                 reads=[rPa[h][nxt], rX[h][cur]], writes=[rpx])
            p.op("dve", lambda e, px=px, cur=cur, nxt=nxt: e.tensor_tensor(out=X[h][nxt][:], in0=px[:, 0:128], in1=X[h][cur][:], op=ALU.add),
                 reads=[rpx, rX[h][cur]], writes=[rX[h][nxt]])
            cur = nxt
            yield
        Xf, rXf = X[h][cur], rX[h][cur]
        pt6, rp6 = bank()
        p.op("pe", lambda e: e.matmul(pt6[:, 0:128], lhsT=Xf[:], rhs=RCt[h][:], start=True, stop=True), reads=[rXf, rRCt[h]], writes=[rp6])
        p.op("act", lambda e: e.copy(out=TBZ[h][:], in_=pt6[:, 0:128]), reads=[rp6], writes=[rTBZ[h]])
        egb = egc[hs, 2 * pr:2 * pr + 2].unsqueeze(2).to_broadcast([64, 2, RC_])
        p.op("dve", lambda e: e.tensor_tensor(out=ACf[h][hs, :].rearrange("p (n c) -> p n c", c=RC_), in0=At.rearrange("p (n c) -> p n c", c=RC_),
                                              in1=egb, op=ALU.mult), reads=[rAt, r_egc], writes=[rACf[h]])
        p.op("pool", lambda e: e.tensor_tensor(out=KCf[h][hs, :].rearrange("p (n c) -> p n c", c=RC_), in0=Kt.rearrange("p (n c) -> p n c", c=RC_),
                                               in1=egb, op=ALU.mult), reads=[rKt, r_egc], writes=[rKCf[h]])
        pt7, rp7 = bank()
        p.op("pe", lambda e: e.transpose(pt7[:, 0:64], ACf[h][hs, :], ident[hs, hs]), reads=[rACf[h], r_small], writes=[rp7])
        p.op("act", lambda e: e.copy(out=ACt[h][:], in_=pt7[:, 0:64]), reads=[rp7], writes=[rACt[h]])
        pt8, rp8 = bank()
        p.op("pe", lambda e: e.transpose(pt8[:, 0:64], KCf[h][hs, :], ident[hs, hs]), reads=[rKCf[h], r_small], writes=[rp8])
        p.op("dve", lambda e: e.tensor_copy(out=KCt[h][:], in_=pt8[:, 0:64]), reads=[rp8], writes=[rKCt[h]])
        yield
        pt9, rp9 = bank()
        p.op("pe", lambda e: e.matmul(pt9[:, 0:128], lhsT=At, rhs=Rt, start=True, stop=True), reads=[rAt, rRt], writes=[rp9])
        p.op("dve", lambda e: e.tensor_tensor(out=AarT[h][:], in0=pt9[:, 0:128], in1=miu[:], op=ALU.mult), reads=[rp9, r_small], writes=[rAarT[h]])
        pt10, rp10 = bank()
        p.op("pe", lambda e: e.matmul(pt10[:, 0:128], lhsT=Kt, rhs=Rt, start=True, stop=True), reads=[rKt, rRt], writes=[rp10])
        p.op("dve", lambda e: e.tensor_tensor(out=AkrT[h][:], in0=pt10[:, 0:128], in1=miu[:], op=ALU.mult), reads=[rp10, r_small], writes=[rAkrT[h]])
        pt11, rp11 = bank()
        p.op("pe", lambda e: e.matmul(pt11[0:64, 0:128], lhsT=TBZ[h][:, 0:64], rhs=AarT[h][:], start=True, stop=False),
             reads=[rTBZ[h], rAarT[h]], writes=[rp11])
        p.op("pe", lambda e: e.matmul(pt11[0:64, 0:128], lhsT=ident[hs, hs], rhs=Rt, start=False, stop=True),
             reads=[r_small, rRt], writes=[rp11])
        p.op("act", lambda e: e.copy(out=PT[h][:], in_=pt11[0:64, 0:128]), reads=[rp11], writes=[rPT[h]])
        yield
        for j in range(2):
            c = 2 * pr + j
            rows = slice(64 * j, 64 * j + 64)
            pm, rpm = bank()
            p.op("pe", lambda e, pm=pm, rows=rows: e.matmul(pm[0:64, 0:64], lhsT=TBZ[h][rows, 0:64], rhs=ACt[h][rows, :], start=True, stop=True),
                 reads=[rTBZ[h], rACt[h]], writes=[rpm])
            p.op("pe", lambda e, pm=pm, rows=rows: e.matmul(pm[0:64, 64:128], lhsT=ACt[h][rows, :], rhs=TBZ[h][rows, 64:128], start=True, stop=False),
                 reads=[rTBZ[h], rACt[h]], writes=[rpm])
            p.op("pe", lambda e, pm=pm, rows=rows: e.matmul(pm[0:64, 64:128], lhsT=KCt[h][rows, :], rhs=VT[rows, pr, hs], start=False, stop=True),
                 reads=[rKCt[h], r_VT], writes=[rpm])
            p.op("dve", lambda e, pm=pm, j=j, c=c: e.scalar_tensor_tensor(out=MT[h][j][:], in0=ident[0:64, 0:64], scalar=egl[:, h, c:c + 1],
                                                                           in1=pm[0:64, 0:64], op0=ALU.mult, op1=ALU.add),
                 reads=[rpm, r_small, r_egl], writes=[rMT[h][j]])
            p.op("act", lambda e, pm=pm, j=j: e.copy(out=NN[h][j][:], in_=pm[0:64, 64:128]), reads=[rpm], writes=[rNN[h][j]])
            ph, rph = bank()
            p.op("pe", lambda e, ph=ph, j=j, c=c: e.matmul(ph[0:64, 0:64], lhsT=MT[h][j][:], rhs=hall[:, h, c % 4, :], start=True, stop=True),
                 reads=[rMT[h][j], r_hall[h][c % 4]], writes=[rph])
            p.op("dve", lambda e, ph=ph, j=j, c=c: e.tensor_tensor(out=hall[:, h, (c + 1) % 4, :], in0=ph[0:64, 0:64], in1=NN[h][j][:], op=ALU.add),
                 reads=[rph, rNN[h][j]], writes=[r_hall[h][(c + 1) % 4]])
            yield
        py, rpy = bank()
        p.op("pe", lambda e: e.matmul(py[0:64, 0:128], lhsT=TBZ[h][:, 64:128], rhs=AarT[h][:], start=True, stop=False),
             reads=[rTBZ[h], rAarT[h]], writes=[rpy])
        p.op("pe", lambda e: e.matmul(py[0:64, 0:128], lhsT=Vt, rhs=AkrT[h][:], start=False, stop=False),
             reads=[r_VT, rAkrT[h]], writes=[rpy])
        for j in range(2):
            c = 2 * pr + j
            p.op("pe", lambda e, j=j, c=c: e.matmul(py[0:64, 64 * j:64 * j + 64], lhsT=hall[:, h, c % 4, :], rhs=PT[h][:, 64 * j:64 * j + 64],
                                                    start=False, stop=(j == 1)),
                 reads=[r_hall[h][c % 4], rPT[h]], writes=[rpy])
        p.op("act", lambda e: e.copy(out=B[YB[h]][0:64, tk], in_=py[0:64, 0:128]), reads=[rpy], writes=[rB[YB[h]]])
        yield

    for pr in range(NPAIR):
        gens = [unit(0, pr), unit(1, pr)]
        alive = [True, True]
        while any(alive):
            for gi, g in enumerate(gens):
                if alive[gi]:
                    try:
                        next(g)
                    except StopIteration:
                        alive[gi] = False
    Y = YB[0]
    p.dma("sp", B[Y][64:128, 1:W], B[YB[1]][0:64, 1:W], reads=[rB[YB[1]]], writes=[rB[Y]])
    SQ, OUT = T1, A_
    p.op("act", lambda e: e.activation(out=v1(SQ), in_=v1(Y), func=AF.Square), reads=[rB[Y], rB[SQ]], writes=[rB[SQ]])
    mb = [p.sbuf(f"mb{i}", [128, 512], F32) for i in range(2)]
    vb = [p.sbuf(f"vb{i}", [128, 512], F32) for i in range(2)]
    r_mb, r_vb = p.regions(2, "mb"), p.regions(2, "vb")
    for tb in range(8):
        i = tb % 2
        sl = slice(1 + tb * 512, 1 + (tb + 1) * 512)
        pm, rpm = bank()
        p.op("pe", lambda e, pm=pm, sl=sl: e.matmul(pm[:, :], lhsT=bo[:], rhs=B[Y][:, sl], start=True, stop=True), reads=[r_small, rB[Y]], writes=[rpm])
        pq, rpq = bank()
        p.op("pe", lambda e, pq=pq, sl=sl: e.matmul(pq[:, :], lhsT=bo[:], rhs=B[SQ][:, sl], start=True, stop=True), reads=[r_small, rB[SQ]], writes=[rpq])
        p.op("act", lambda e, pm=pm, i=i: e.activation(out=mb[i][:], in_=pm[:, :], func=AF.Copy, scale=1.0 / 64.0), reads=[rpm], writes=[r_mb[i]])
        p.op("dve", lambda e, i=i, sl=sl: e.tensor_tensor(out=B[OUT][:, sl], in0=B[Y][:, sl], in1=mb[i][:], op=ALU.subtract),
             reads=[rB[Y], r_mb[i], rB[OUT]], writes=[rB[OUT]])
        p.op("act", lambda e, i=i: e.activation(out=mb[i][:], in_=mb[i][:], func=AF.Square), reads=[r_mb[i]], writes=[r_mb[i]])
        p.op("dve", lambda e, pq=pq, i=i: e.scalar_tensor_tensor(out=vb[i][:], in0=pq[:, :], scalar=1.0 / 64.0, in1=mb[i][:], op0=ALU.mult, op1=ALU.subtract),
             reads=[rpq, r_mb[i]], writes=[r_vb[i]])
        p.op("act", lambda e, i=i: e.activation(out=vb[i][:], in_=vb[i][:], func=AF.Sqrt, bias=pv[:, 11:12], scale=1.0), reads=[r_vb[i], r_small], writes=[r_vb[i]])
        p.op("dve", lambda e, i=i: e.reciprocal(out=vb[i][:], in_=vb[i][:]), reads=[r_vb[i]], writes=[r_vb[i]])
        p.op("dve", lambda e, i=i, sl=sl: e.tensor_tensor(out=B[OUT][:, sl], in0=B[OUT][:, sl], in1=vb[i][:], op=ALU.mult),
             reads=[rB[OUT], r_vb[i]], writes=[rB[OUT]])
        p.op("dve", lambda e, sl=sl: e.tensor_scalar(out=B[OUT][:, sl], in0=B[OUT][:, sl], scalar1=pv[:, 8:9], scalar2=pv[:, 9:10], op0=ALU.mult, op1=ALU.add),
             reads=[rB[OUT], r_small], writes=[rB[OUT]])
        p.op("pool", lambda e, sl=sl: e.tensor_tensor(out=B[OUT][:, sl], in0=B[OUT][:, sl], in1=B[V_][:, sl], op=ALU.add),
             reads=[rB[OUT], rB[V_]], writes=[rB[OUT]])
        p.op("pool", lambda e, sl=sl: e.tensor_tensor(out=B[OUT][:, sl], in0=B[OUT][:, sl], in1=B[GT][:, sl], op=ALU.mult),
             reads=[rB[OUT], rB[GT]], writes=[rB[OUT]])
    p.dma("sp", o_d, B[OUT][:, 1:W], reads=[rB[OUT]], is_output=True)
    p.emit()
    return nc


KC = 16
ALPHA = 4 ** 0.25
LN_EPS = 1e-5


def make_stream(p, nwb=2, sk=8):
    stg = [p.sbuf(f"stg{i}", [128, sk * 512], F32) for i in range(2)]
    r_stg = p.regions(2, "stg")
    wb = [p.sbuf(f"wb{i}", [128, 16, 512], BF16) for i in range(nwb)]
    r_wb = p.regions(nwb, "wb")
    st = {"si": 0, "wi": 0, "ci": 0}

    def stage():
        si = st["si"] % 2
        st["si"] += 1
        return stg[si], r_stg[si]

    def ceng():
        st["ci"] += 1
        return "dve" if st["ci"] % 3 == 0 else "act"

    def load(w_ap, nk, cw=512, into=None):
        if into is None:
            i = st["wi"] % nwb
            st["wi"] += 1
            dst, rdst = wb[i], r_wb[i]
        else:
            dst, rdst = into
        for k0 in range(0, nk, sk):
            kk = min(sk, nk - k0)
            sg, rsg = stage()
            sv = sg[:, 0:kk * 512].rearrange("p (k n) -> p k n", n=512)
            p.dma("sp", sv[:, :, 0:cw], w_ap[k0 * 128:(k0 + kk) * 128, :].rearrange("(k p) n -> p k n", p=128), writes=[rsg])
            ce = ceng()
            if ce == "act":
                p.op("act", lambda e, dst=dst, sv=sv, k0=k0, kk=kk, cw=cw: e.copy(out=dst[:, k0:k0 + kk, 0:cw], in_=sv[:, :, 0:cw]),
                     reads=[rsg], writes=[rdst])
            else:
                p.op(ce, lambda e, dst=dst, sv=sv, k0=k0, kk=kk, cw=cw: e.tensor_copy(out=dst[:, k0:k0 + kk, 0:cw], in_=sv[:, :, 0:cw]),
                     reads=[rsg], writes=[rdst])
        return dst, rdst

    def load_act(a_ap, dst, rdst, k0, nk, ntok=1024):
        sg, rsg = stage()
        sv = sg[:, 0:nk * ntok].rearrange("p (k t) -> p k t", t=ntok)
        p.dma("sp", sv, a_ap.rearrange("(k p) t -> p k t", p=128), writes=[rsg])
        p.op("dve", lambda e: e.tensor_copy(out=dst[:, k0:k0 + nk, :], in_=sv), reads=[rsg], writes=[rdst])

    return load, load_act, stage, ceng


def build_c1(ntok=1024):
    nc = bass.Bass("TRN2", target_bir_lowering=False)
    D = 2048
    dt_ = lambda n, s: nc.dram_tensor(n, s, F32, kind="ExternalInput").ap()
    xT_d, sc_d, sh_d = dt_("xT", [D, ntok]), dt_("sc", [128, KC]), dt_("sh", [128, KC])
    wg_d, wbr_d = dt_("wg", [D, 4 * D]), dt_("wbr", [1792, D])
    ya_d, yc_d, yd_d = dt_("yaT", [512, ntok]), dt_("ycT", [512, ntok]), dt_("ydT", [512, ntok])
    ybn_d, ybd_d = dt_("ybn", [3, 256, ntok]), dt_("ybd", [3, 256, ntok])
    o_d = nc.dram_tensor("o", [D, ntok], F32, kind="ExternalOutput").ap()
    p = Prog(nc)
    load, load_act, stage, ceng = make_stream(p)
    sc = p.sbuf("sc_s", [128, KC], F32)
    sh = p.sbuf("sh_s", [128, KC], F32)
    hT = p.sbuf("hT", [128, KC, ntok], BF16)
    r_sc = p.region("sc")
    r_h = p.regions(KC, "h")
    p.dma("sp", sc[:], sc_d, writes=[r_sc])
    p.dma("sp", sh[:], sh_d, writes=[r_sc])
    p.op("dve", lambda e: e.tensor_scalar_add(out=sc[:], in0=sc[:], scalar1=1.0), reads=[r_sc], writes=[r_sc])
    for g in range(KC // 4):
        sg, rsg = stage()
        sv = sg[:].rearrange("p (k t) -> p k t", t=ntok)
        p.dma("sp", sv, xT_d[g * 512:(g + 1) * 512, :].rearrange("(k p) t -> p k t", p=128), writes=[rsg])
        for j in range(4):
            kc = g * 4 + j
            p.op("act", lambda e, sv=sv, j=j, kc=kc: e.activation(out=hT[:, kc, :], in_=sv[:, j, :], func=AF.Identity,
                                                                  bias=sh[:, kc:kc + 1], scale=sc[:, kc:kc + 1]),
                 reads=[rsg, r_sc], writes=[r_h[kc]])
    yT = p.sbuf("yT", [128, 14, ntok], BF16)
    r_y = p.regions(4, "y")
    load_act(ya_d, yT, r_y[0], 0, 4, ntok)
    load_act(yc_d, yT, r_y[2], 6, 4, ntok)
    load_act(yd_d, yT, r_y[3], 10, 4, ntok)
    for j in range(2):
        sn, rsn = stage()
        sd, rsd = stage()
        nv = sn[:, 0:3 * ntok].rearrange("p (g t) -> p g t", t=ntok)
        dv = sd[:, 0:3 * ntok].rearrange("p (g t) -> p g t", t=ntok)
        p.dma("sp", nv, ybn_d[:, j * 128:(j + 1) * 128, :].rearrange("g p t -> p g t"), writes=[rsn])
        p.dma("sp", dv, ybd_d[:, j * 128:(j + 1) * 128, :].rearrange("g p t -> p g t"), writes=[rsd])
        p.op("dve", lambda e, nv=nv: e.tensor_tensor(out=nv[:, 0, :], in0=nv[:, 0, :], in1=nv[:, 1, :], op=ALU.add), reads=[rsn], writes=[rsn])
        p.op("dve", lambda e, nv=nv: e.tensor_tensor(out=nv[:, 0, :], in0=nv[:, 0, :], in1=nv[:, 2, :], op=ALU.add), reads=[rsn], writes=[rsn])
        p.op("pool", lambda e, dv=dv: e.tensor_tensor(out=dv[:, 0, :], in0=dv[:, 0, :], in1=dv[:, 1, :], op=ALU.add), reads=[rsd], writes=[rsd])
        p.op("pool", lambda e, dv=dv: e.tensor_tensor(out=dv[:, 0, :], in0=dv[:, 0, :], in1=dv[:, 2, :], op=ALU.add), reads=[rsd], writes=[rsd])
        p.op("dve", lambda e, dv=dv: e.reciprocal(out=dv[:, 0, :], in_=dv[:, 0, :]), reads=[rsd], writes=[rsd])
        p.op("dve", lambda e, nv=nv, dv=dv, j=j: e.tensor_tensor(out=yT[:, 4 + j, :], in0=nv[:, 0, :], in1=dv[:, 0, :], op=ALU.mult),
             reads=[rsn, rsd], writes=[r_y[1]])
    brk = ((0, 4), (4, 6), (6, 10), (10, 14))
    wbrb = p.sbuf("wbrb", [128, 16, 512], BF16)
    r_wbrb = p.region("wbrb")
    acc = p.sbuf("acc", [128, 2, 4, 2, 512], F32)
    r_acc = [[[p.region(f"acc{a}{b}{c}") for c in range(2)] for b in range(4)] for a in range(2)]
    sig = [p.sbuf(f"sig{i}", [128, 512], F32) for i in range(3)]
    r_sig = p.regions(3, "sig")
    pg = [p.psum(f"pg{i}", [128, 512]) for i in range(3)]
    pb = [p.psum(f"pb{i}", [128, 512]) for i in range(3)]
    r_pg, r_pb = p.regions(3, "pg"), p.regions(3, "pb")
    cnt = 0
    ntb = ntok // 512
    for dg in range(4):
        par = dg % 2
        load(wbr_d[:, dg * 512:(dg + 1) * 512], 14, into=(wbrb, r_wbrb))
        for br in range(4):
            wt, rwt = load(wg_d[:, br * D + dg * 512: br * D + (dg + 1) * 512], 16)
            k0, k1 = brk[br]
            for dci in range(4):
                cs = slice(dci * 128, (dci + 1) * 128)
                for tb in range(ntb):
                    ts_ = slice(tb * 512, (tb + 1) * 512)
                    i = cnt % 3
                    cnt += 1
                    for kc in range(KC):
                        p.op("pe", lambda e, i=i, wt=wt, kc=kc, cs=cs, ts_=ts_: e.matmul(pg[i][:, :], lhsT=wt[:, kc, cs], rhs=hT[:, kc, ts_],
                                                                                        start=(kc == 0), stop=(kc == KC - 1)),
                             reads=[rwt, r_h[kc]], writes=[r_pg[i]])
                    for kc in range(k0, k1):
                        p.op("pe", lambda e, i=i, kc=kc, cs=cs, ts_=ts_, k0=k0, k1=k1: e.matmul(pb[i][:, :], lhsT=wbrb[:, kc, cs], rhs=yT[:, kc, ts_],
                                                                                                start=(kc == k0), stop=(kc == k1 - 1)),
                             reads=[r_wbrb, r_y[br]], writes=[r_pb[i]])
                    p.op("act", lambda e, i=i: e.activation(out=sig[i][:], in_=pg[i][:, :], func=AF.Sigmoid), reads=[r_pg[i]], writes=[r_sig[i]])
                    a_ap = acc[:, par, dci, tb, :]
                    ra = r_acc[par][dci][tb]
                    if br == 0:
                        p.op("dve", lambda e, i=i, a_ap=a_ap: e.tensor_tensor(out=a_ap, in0=pb[i][:, :], in1=sig[i][:], op=ALU.mult),
                             reads=[r_pb[i], r_sig[i]], writes=[ra])
                    else:
                        p.op("dve", lambda e, i=i: e.tensor_tensor(out=sig[i][:], in0=pb[i][:, :], in1=sig[i][:], op=ALU.mult),
                             reads=[r_pb[i], r_sig[i]], writes=[r_sig[i]])
                        p.op("pool", lambda e, i=i, a_ap=a_ap: e.tensor_tensor(out=a_ap, in0=a_ap, in1=sig[i][:], op=ALU.add),
                             reads=[ra, r_sig[i]], writes=[ra])
        p.dma("sp", o_d[dg * 512:(dg + 1) * 512, :].rearrange("(c p) (b t) -> p c b t", p=128, t=512), acc[:, par],
              reads=[r for b_ in r_acc[par] for r in b_], is_output=True)
    p.emit()
    return nc


def emit_ln(p, zT, r_z, ntok, gcol, bcol, onesD, r_const, eps_ap, bank, name, tmps=None):
    if tmps is None:
        sq = [p.sbuf(f"{name}sq{i}", [128, 512], F32) for i in range(2)]
        r_sq = p.regions(2, name + "sq")
        mean = p.sbuf(name + "mean", [128, 512], F32)
        rstd = p.sbuf(name + "rstd", [128, 512], F32)
        r_mean, r_rstd = p.region(name + "mean"), p.region(name + "rstd")
    else:
        (sq0, rs0), (sq1, rs1), (mean, r_mean), (rstd, r_rstd) = tmps
        sq, r_sq = [sq0, sq1], [rs0, rs1]
    def one_tb(tb):
        ts_ = slice(tb * 512, (tb + 1) * 512)
        pm, rpm = bank()
        pq, rpq = bank()
        for dc in range(KC):
            i = dc % 2
            p.op("pe", lambda e, dc=dc: e.matmul(pm[:, :], lhsT=onesD[:], rhs=zT[:, dc, ts_], start=(dc == 0), stop=(dc == KC - 1)),
                 reads=[r_const, r_z[dc]], writes=[rpm])
            p.op("act", lambda e, dc=dc, i=i: e.activation(out=sq[i][:], in_=zT[:, dc, ts_], func=AF.Square), reads=[r_z[dc]], writes=[r_sq[i]])
            p.op("pe", lambda e, dc=dc, i=i: e.matmul(pq[:, :], lhsT=onesD[:], rhs=sq[i][:], start=(dc == 0), stop=(dc == KC - 1)),
                 reads=[r_const, r_sq[i]], writes=[rpq])
        p.op("act", lambda e: e.copy(out=mean[:], in_=pm[:, :]), reads=[rpm], writes=[r_mean])
        p.op("act", lambda e: e.activation(out=rstd[:], in_=pm[:, :], func=AF.Square), reads=[rpm], writes=[r_rstd])
        p.op("dve", lambda e: e.tensor_tensor(out=rstd[:], in0=pq[:, :], in1=rstd[:], op=ALU.subtract), reads=[rpq, r_rstd], writes=[r_rstd])
        p.op("act", lambda e: e.activation(out=rstd[:], in_=rstd[:], func=AF.Sqrt, bias=eps_ap, scale=1.0), reads=[r_rstd, r_const], writes=[r_rstd])
        p.op("dve", lambda e: e.reciprocal(out=rstd[:], in_=rstd[:]), reads=[r_rstd], writes=[r_rstd])
        for dc in range(KC):
            p.op("dve", lambda e, dc=dc: e.tensor_tensor(out=zT[:, dc, ts_], in0=zT[:, dc, ts_], in1=mean[:], op=ALU.subtract),
                 reads=[r_z[dc], r_mean], writes=[r_z[dc]])
            p.op("pool", lambda e, dc=dc: e.tensor_tensor(out=zT[:, dc, ts_], in0=zT[:, dc, ts_], in1=rstd[:], op=ALU.mult),
                 reads=[r_z[dc], r_rstd], writes=[r_z[dc]])
            p.op("act", lambda e, dc=dc: e.activation(out=zT[:, dc, ts_], in_=zT[:, dc, ts_], func=AF.Identity, bias=bcol(dc), scale=gcol(dc)),
                 reads=[r_z[dc], r_const], writes=[r_z[dc]])

    for tb in range(ntok // 512):
        one_tb(tb)


def build_c2(ntok=1024):
    nc = bass.Bass("TRN2", target_bir_lowering=False)
    D = 2048
    dt_ = lambda n, s: nc.dram_tensor(n, s, F32, kind="ExternalInput").ap()
    m_d, xT_d, wo_d = dt_("mT", [D, ntok]), dt_("xT", [D, ntok]), dt_("wout", [D, D])
    pv_d = dt_("pv", [128, 8, KC])
    rw_d, rb_d = dt_("rw", [D, 32]), dt_("rb", [32, 1])
    id_d, on_d = dt_("ident", [128, 128]), dt_("onesD", [128, 128])
    x1_d = nc.dram_tensor("x1T", [D, ntok], F32, kind="ExternalOutput").ap()
    wt_d = nc.dram_tensor("wtT", [32, ntok], F32, kind="ExternalOutput").ap()
    p = Prog(nc)
    load, load_act, stage, ceng = make_stream(p)
    pv = p.sbuf("pv_s", [128, 8, KC], F32)
    rw = p.sbuf("rw_s", [128, KC, 32], F32)
    rb = p.sbuf("rb_s", [32, 1], F32)
    ident = p.sbuf("ident_s", [128, 128], F32)
    onesD = p.sbuf("onesD_s", [128, 128], F32)
    eps = p.sbuf("eps_s", [128, 1], F32)
    r_const = p.region("const")
    p.dma("sp", pv[:], pv_d, writes=[r_const])
    p.dma("sp", rw[:], rw_d.rearrange("(k p) e -> p k e", p=128), writes=[r_const])
    p.dma("sp", rb[:], rb_d, writes=[r_const])
    p.dma("sp", ident[:], id_d, writes=[r_const])
    p.dma("sp", onesD[:], on_d, writes=[r_const])
    p.op("pool", lambda e: e.memset(eps[:], LN_EPS), writes=[r_const])
    p.op("dve", lambda e: e.tensor_scalar_add(out=pv[:, 3, :], in0=pv[:, 3, :], scalar1=1.0), reads=[r_const], writes=[r_const])
    ps = [p.psum(f"ps{i}", [128, 512]) for i in range(8)]
    r_ps = p.regions(8, "ps")
    pc = [0]

    def bank():
        i = pc[0] % 8
        pc[0] += 1
        return ps[i], r_ps[i]

    mTb = p.sbuf("mTb", [128, KC, ntok], BF16)
    r_m = p.regions(4, "m")
    for g in range(4):
        load_act(m_d[g * 512:(g + 1) * 512, :], mTb, r_m[g], g * 4, 4, ntok)
    zT = p.sbuf("zT", [128, KC, ntok], F32)
    r_z = p.regions(KC, "z")
    xa = [p.sbuf(f"xa{i}", [128, ntok], F32) for i in range(2)]
    r_xa = p.regions(2, "xa")
    ntb = ntok // 512
    for dg in range(4):
        wt, rwt = load(wo_d[:, dg * 512:(dg + 1) * 512], 16)
        for dci in range(4):
            dc = dg * 4 + dci
            i = dc % 2
            p.dma("sp", xa[i][:], xT_d[dc * 128:(dc + 1) * 128, :], writes=[r_xa[i]])
            p.op("act", lambda e, i=i: e.mul(out=xa[i][:], in_=xa[i][:], mul=ALPHA), reads=[r_xa[i]], writes=[r_xa[i]])
            for tb in range(ntb):
                ts_ = slice(tb * 512, (tb + 1) * 512)
                pt, rp = bank()
                for kc in range(KC):
                    p.op("pe", lambda e, pt=pt, wt=wt, kc=kc, dci=dci, ts_=ts_: e.matmul(pt[:, :], lhsT=wt[:, kc, dci * 128:(dci + 1) * 128],
                                                                                        rhs=mTb[:, kc, ts_], start=(kc == 0), stop=(kc == KC - 1)),
                         reads=[rwt, r_m[kc // 4]], writes=[rp])
                p.op("dve", lambda e, pt=pt, dc=dc, i=i, ts_=ts_: e.scalar_tensor_tensor(out=zT[:, dc, ts_], in0=pt[:, :], scalar=pv[:, 0, dc:dc + 1],
                                                                                        in1=xa[i][:, ts_], op0=ALU.mult, op1=ALU.add),
                     reads=[rp, r_const, r_xa[i]], writes=[r_z[dc]])
    emit_ln(p, zT, r_z, ntok, lambda dc: pv[:, 1, dc:dc + 1], lambda dc: pv[:, 2, dc:dc + 1], onesD, r_const, eps[:], bank, "ln1")
    p.dma("sp", x1_d.rearrange("(k p) t -> p k t", p=128), zT[:], reads=r_z, is_output=True)
    h2 = [p.sbuf(f"h2{i}", [128, ntok], F32) for i in range(2)]
    r_h2 = p.regions(2, "h2")
    pl = [bank() for _ in range(ntb)]
    for dc in range(KC):
        i = dc % 2
        p.op("act", lambda e, dc=dc, i=i: e.activation(out=h2[i][:], in_=zT[:, dc, :], func=AF.Identity, bias=pv[:, 4, dc:dc + 1], scale=pv[:, 3, dc:dc + 1]),
             reads=[r_z[dc], r_const], writes=[r_h2[i]])
        for tb in range(ntb):
            p.op("pe", lambda e, dc=dc, i=i, tb=tb: e.matmul(pl[tb][0][0:32, :], lhsT=rw[:, dc, :], rhs=h2[i][:, tb * 512:(tb + 1) * 512],
                                                             start=(dc == 0), stop=(dc == KC - 1)),
                 reads=[r_const, r_h2[i]], writes=[pl[tb][1]])
    LT = p.sbuf("LT", [32, ntok], F32)
    r_LT = p.region("LT")
    for tb in range(ntb):
        p.op("act", lambda e, tb=tb: e.activation(out=LT[:, tb * 512:(tb + 1) * 512], in_=pl[tb][0][0:32, :], func=AF.Identity, bias=rb[:], scale=1.0),
             reads=[pl[tb][1], r_const], writes=[r_LT])
    wtT = p.sbuf("wtT_s", [32, ntok], F32)
    r_wtT = p.regions(ntok // 128, "wtT")
    L = [p.sbuf(f"L{i}", [128, 32], F32) for i in range(2)]
    E = [p.sbuf(f"E{i}", [128, 32], F32) for i in range(2)]
    M8 = [p.sbuf(f"M8{i}", [128, 8], F32) for i in range(2)]
    S1 = [p.sbuf(f"S1{i}", [128, 2], F32) for i in range(2)]
    r_L, r_E, r_M8, r_S1 = p.regions(2, "L"), p.regions(2, "E"), p.regions(2, "M8"), p.regions(2, "S1")
    for tt in range(ntok // 128):
        i = tt % 2
        pt, rp = bank()
        p.op("pe", lambda e, pt=pt, tt=tt: e.transpose(pt[:, 0:32], LT[:, tt * 128:(tt + 1) * 128], ident[0:32, 0:32]), reads=[r_LT, r_const], writes=[rp])
        p.op("act", lambda e, pt=pt, i=i: e.copy(out=L[i][:], in_=pt[:, 0:32]), reads=[rp], writes=[r_L[i]])
        p.op("dve", lambda e, i=i: e.max(out=M8[i][:], in_=L[i][:]), reads=[r_L[i]], writes=[r_M8[i]])
        p.op("dve", lambda e, i=i: e.tensor_scalar(out=S1[i][:, 0:1], in0=M8[i][:, 0:1], scalar1=-1.0, scalar2=None, op0=ALU.mult),
             reads=[r_M8[i]], writes=[r_S1[i]])
        p.op("act", lambda e, i=i: e.activation(out=E[i][:], in_=L[i][:], func=AF.Exp, bias=S1[i][:, 0:1], scale=1.0),
             reads=[r_L[i], r_S1[i]], writes=[r_E[i]])
        p.op("dve", lambda e, i=i: e.tensor_scalar(out=L[i][:], in0=L[i][:], scalar1=M8[i][:, 3:4], scalar2=None, op0=ALU.is_ge),
             reads=[r_L[i], r_M8[i], r_E[i]], writes=[r_L[i]])
        p.op("dve", lambda e, i=i: e.tensor_tensor(out=E[i][:], in0=E[i][:], in1=L[i][:], op=ALU.mult), reads=[r_E[i], r_L[i]], writes=[r_E[i]])
        p.op("dve", lambda e, i=i: e.reduce_sum(out=S1[i][:, 1:2], in_=E[i][:], axis=AX.X), reads=[r_E[i], r_S1[i]], writes=[r_S1[i]])
        p.op("dve", lambda e, i=i: e.reciprocal(out=S1[i][:, 1:2], in_=S1[i][:, 1:2]), reads=[r_S1[i]], writes=[r_S1[i]])
        p.op("dve", lambda e, i=i: e.tensor_scalar(out=E[i][:], in0=E[i][:], scalar1=S1[i][:, 1:2], scalar2=None, op0=ALU.mult),
             reads=[r_E[i], r_S1[i]], writes=[r_E[i]])
        pt2, rp2 = bank()
        p.op("pe", lambda e, pt2=pt2, i=i: e.transpose(pt2[0:32, 0:128], E[i][:], ident[:]), reads=[r_E[i], r_const], writes=[rp2])
        p.op("act", lambda e, pt2=pt2, tt=tt: e.copy(out=wtT[:, tt * 128:(tt + 1) * 128], in_=pt2[0:32, 0:128]), reads=[rp2], writes=[r_wtT[tt]])
    p.dma("sp", wt_d, wtT[:], reads=r_wtT, is_output=True)
    p.emit()
    return nc


def build_c3(ntok=1024, nexp=32):
    nc = bass.Bass("TRN2", target_bir_lowering=False)
    D, F = 2048, 1024
    dt_ = lambda n, s: nc.dram_tensor(n, s, F32, kind="ExternalInput").ap()
    x1_d, wt_d = dt_("x1T", [D, ntok]), dt_("wtT", [32, ntok])
    pv_d = dt_("pv", [128, 8, KC])
    wgu_d, bgu_d = dt_("wgu", [nexp, D, 2 * F]), dt_("bgu", [128, nexp, 16])
    wd_d, bd_d = dt_("wd", [nexp, F, D]), dt_("bd", [32, D])
    on_d = dt_("onesD", [128, 128])
    o_d = nc.dram_tensor("o", [D, ntok], F32, kind="ExternalOutput").ap()
    p = Prog(nc)
    load, load_act, stage, ceng = make_stream(p, nwb=3, sk=4)
    pv = p.sbuf("pv_s", [128, 8, KC], F32)
    onesD = p.sbuf("onesD_s", [128, 128], F32)
    cst = p.sbuf("cst", [128, 4], F32)
    bgu = p.sbuf("bgu_s", [128, nexp, 16], F32)
    bd = p.sbuf("bd_s", [32, D], F32)
    wts = p.sbuf("wts", [32, ntok], F32)
    r_const = p.region("const")
    p.dma("sp", pv[:], pv_d, writes=[r_const])
    p.dma("sp", onesD[:], on_d, writes=[r_const])
    p.dma("sp", bgu[:], bgu_d, writes=[r_const])
    p.dma("sp", bd[:], bd_d, writes=[r_const])
    p.dma("sp", wts[:], wt_d, writes=[r_const])
    for j, val in enumerate((LN_EPS, 7.0, -7.0, 1.0)):
        p.op("pool", lambda e, j=j, val=val: e.memset(cst[:, j:j + 1], val), writes=[r_const])
    p.op("dve", lambda e: e.tensor_scalar_add(out=pv[:, 3, :], in0=pv[:, 3, :], scalar1=1.0), reads=[r_const], writes=[r_const])
    ps = [p.psum(f"ps{i}", [128, 512]) for i in range(8)]
    r_ps = p.regions(8, "ps")
    pc = [0]

    def bank():
        i = pc[0] % 8
        pc[0] += 1
        return ps[i], r_ps[i]

    ntb = ntok // 512
    h2 = p.sbuf("h2", [128, KC, ntok], BF16)
    r_h = p.regions(KC, "h")
    for dc in range(KC):
        sg, rsg = stage()
        p.dma("sp", sg[:, 0:ntok], x1_d[dc * 128:(dc + 1) * 128, :], writes=[rsg])
        p.op("act", lambda e, dc=dc, sg=sg: e.activation(out=h2[:, dc, :], in_=sg[:, 0:ntok], func=AF.Identity, bias=pv[:, 4, dc:dc + 1], scale=pv[:, 3, dc:dc + 1]),
             reads=[rsg, r_const], writes=[r_h[dc]])
    accT = p.sbuf("accT", [128, KC, ntok], F32)
    r_acc = [[p.region(f"acc{dc}_{tb}") for tb in range(ntb)] for dc in range(KC)]
    actT = p.sbuf("actT", [128, 8, ntok], BF16)
    r_act = [[p.region(f"act{fc}_{tb}") for tb in range(ntb)] for fc in range(8)]
    wtb = [p.sbuf("wtb0", [128, ntok], F32)] * 2
    r_wtb = [p.region("wtb")] * 2
    tg = [p.sbuf(f"tg{i}", [128, 512], F32) for i in range(2)]
    tsg = [p.sbuf(f"tsg{i}", [128, 512], F32) for i in range(2)]
    tl = [p.sbuf(f"tl{i}", [128, 512], F32) for i in range(2)]
    r_tg, r_tsg, r_tl = p.regions(2, "tg"), p.regions(2, "tsg"), p.regions(2, "tl")
    ec = 0
    for ex in range(nexp):
        xi = ex % 2
        p.dma("sp", wtb[xi][:], wt_d[ex:ex + 1, :].partition_broadcast(128), writes=[r_wtb[xi]])
        for half in range(2):
            wg_t, rwg = load(wgu_d[ex][:, half * 512:(half + 1) * 512], 16)
            wl_t, rwl = load(wgu_d[ex][:, F + half * 512:F + (half + 1) * 512], 16)
            for fi in range(4):
                fc = half * 4 + fi
                cs = slice(fi * 128, (fi + 1) * 128)
                for tb in range(ntb):
                    ts_ = slice(tb * 512, (tb + 1) * 512)
                    i = ec % 2
                    ec += 1
                    pgb, rpg = bank()
                    plb, rpl = bank()
                    for kc in range(KC):
                        p.op("pe", lambda e, pgb=pgb, wg_t=wg_t, kc=kc, cs=cs, ts_=ts_: e.matmul(pgb[:, :], lhsT=wg_t[:, kc, cs], rhs=h2[:, kc, ts_],
                                                                                                  start=(kc == 0), stop=(kc == KC - 1)),
                             reads=[rwg, r_h[kc]], writes=[rpg])
                    for kc in range(KC):
                        p.op("pe", lambda e, plb=plb, wl_t=wl_t, kc=kc, cs=cs, ts_=ts_: e.matmul(plb[:, :], lhsT=wl_t[:, kc, cs], rhs=h2[:, kc, ts_],
                                                                                                  start=(kc == 0), stop=(kc == KC - 1)),
                             reads=[rwl, r_h[kc]], writes=[rpl])
                    p.op("act", lambda e, plb=plb, i=i, ex=ex, fc=fc: e.activation(out=tl[i][:], in_=plb[:, :], func=AF.Identity,
                                                                                   bias=bgu[:, ex, 8 + fc:9 + fc], scale=1.0),
                         reads=[rpl, r_const], writes=[r_tl[i]])
                    p.op("dve", lambda e, pgb=pgb, i=i, ex=ex, fc=fc: e.tensor_scalar(out=tg[i][:], in0=pgb[:, :], scalar1=bgu[:, ex, fc:fc + 1], scalar2=cst[:, 1:2],
                                                                                       op0=ALU.add, op1=ALU.min),
                         reads=[rpg, r_const], writes=[r_tg[i]])
                    p.op("act", lambda e, i=i: e.activation(out=tsg[i][:], in_=tg[i][:], func=AF.Sigmoid, scale=1.702), reads=[r_tg[i]], writes=[r_tsg[i]])
                    p.op("dve", lambda e, i=i: e.tensor_scalar(out=tl[i][:], in0=tl[i][:], scalar1=cst[:, 2:3], scalar2=cst[:, 1:2], op0=ALU.max, op1=ALU.min),
                         reads=[r_tl[i], r_const], writes=[r_tl[i]])
                    p.op("dve", lambda e, i=i, xi=xi, ts_=ts_: e.scalar_tensor_tensor(out=tl[i][:], in0=tl[i][:], scalar=1.0, in1=wtb[xi][:, ts_],
                                                                                       op0=ALU.add, op1=ALU.mult),
                         reads=[r_tl[i], r_wtb[xi]], writes=[r_tl[i]])
                    p.op("pool", lambda e, i=i: e.tensor_tensor(out=tg[i][:], in0=tg[i][:], in1=tsg[i][:], op=ALU.mult), reads=[r_tg[i], r_tsg[i]], writes=[r_tg[i]])
                    p.op("dve", lambda e, i=i, fc=fc, ts_=ts_: e.tensor_tensor(out=actT[:, fc, ts_], in0=tg[i][:], in1=tl[i][:], op=ALU.mult),
                         reads=[r_tg[i], r_tl[i]], writes=[r_act[fc][tb]])
        for dg in range(4):
            wd_t, rwd = load(wd_d[ex][:, dg * 512:(dg + 1) * 512], 8)
            for dci in range(4):
                dc = dg * 4 + dci
                cs = slice(dci * 128, (dci + 1) * 128)
                for tb in range(ntb):
                    ts_ = slice(tb * 512, (tb + 1) * 512)
                    pdb, rpd = bank()
                    for fc in range(8):
                        p.op("pe", lambda e, pdb=pdb, wd_t=wd_t, fc=fc, cs=cs, ts_=ts_, ex=ex: e.matmul(pdb[:, :], lhsT=wd_t[:, fc, cs], rhs=actT[:, fc, ts_],
                                                                                                         start=(fc == 0), stop=(fc == 7 and ex > 0)),
                             reads=[rwd, r_act[fc][tb]], writes=[rpd])
                    a_ap = accT[:, dc, ts_]
                    if ex == 0:
                        p.op("pe", lambda e, pdb=pdb, dc=dc, ts_=ts_: e.matmul(pdb[:, :], lhsT=bd[:, dc * 128:(dc + 1) * 128], rhs=wts[:, ts_], start=False, stop=True),
                             reads=[r_const], writes=[rpd])
                        p.op("act", lambda e, pdb=pdb, a_ap=a_ap: e.copy(out=a_ap, in_=pdb[:, :]), reads=[rpd], writes=[r_acc[dc][tb]])
                    else:
                        p.op("dve", lambda e, pdb=pdb, a_ap=a_ap: e.tensor_tensor(out=a_ap, in0=pdb[:, :], in1=a_ap, op=ALU.add),
                             reads=[rpd, r_acc[dc][tb]], writes=[r_acc[dc][tb]])
    r_z = p.regions(KC, "z")
    for dc in range(KC):
        sg, rsg = stage()
        p.dma("sp", sg[:, 0:ntok], x1_d[dc * 128:(dc + 1) * 128, :], writes=[rsg])
        p.op("act", lambda e, sg=sg: e.mul(out=sg[:, 0:ntok], in_=sg[:, 0:ntok], mul=ALPHA), reads=[rsg], writes=[rsg])
        p.op("dve", lambda e, dc=dc, sg=sg: e.scalar_tensor_tensor(out=accT[:, dc, :], in0=accT[:, dc, :], scalar=pv[:, 0, dc:dc + 1], in1=sg[:, 0:ntok],
                                                                    op0=ALU.mult, op1=ALU.add),
             reads=[rsg, r_const] + r_acc[dc], writes=[r_z[dc]])
    emit_ln(p, accT, r_z, ntok, lambda dc: pv[:, 1, dc:dc + 1], lambda dc: pv[:, 2, dc:dc + 1], onesD, r_const, cst[:, 0:1], bank, "ln2",
            tmps=[(tg[0], r_tg[0]), (tg[1], r_tg[1]), (tsg[0], r_tsg[0]), (tsg[1], r_tsg[1])])
    p.dma("sp", o_d.rearrange("(k p) t -> p k t", p=128), accT[:], reads=r_z, is_output=True)
    p.emit()
    return nc

import numpy as np
from concourse.bass_utils import run_bass_kernel_spmd

NCORES = 8
_cache = {}


def run_prog(key, builder, in_maps):
    if key not in _cache:
        _cache[key] = builder()
    res = run_bass_kernel_spmd(_cache[key], in_maps, core_ids=list(range(NCORES)))
    return res.results


def fm16(v):
    return np.ascontiguousarray(v.reshape(16, 128).T)


def host_pool(pv, pool_w, pool_scale):
    B, S, _ = pv.shape
    H = HALO
    pvT = np.zeros((B, 4, 128, S + H), np.float32)
    pvT[:, :, :, H:] = pv.reshape(B, S, 4, 128).transpose(0, 2, 3, 1)
    t = np.arange(S)
    inv = np.stack([1.0 / np.minimum(t + 1, w) for w in (2, 4, 8, 16)]).astype(np.float32)
    psc = np.ascontiguousarray(pool_scale.reshape(4, 128).T)
    in_maps = []
    for core in range(NCORES):
        b, q = core // 4, core % 4
        in_maps.append({"v": np.ascontiguousarray(pvT[b, :, :, q * 1024:(q + 1) * 1024 + H]),
                        "inv": np.ascontiguousarray(np.broadcast_to(inv[:, None, q * 1024:(q + 1) * 1024], (4, 128, 1024))),
                        "pw": np.ascontiguousarray(pool_w), "psc": psc})
    res = run_prog("pool", lambda: build_pool(1024), in_maps)
    ya = np.zeros((B, S, 512), np.float32)
    for core in range(NCORES):
        b, q = core // 4, core % 4
        ya[b, q * 1024:(q + 1) * 1024] = res[core]["o"].transpose(2, 0, 1).reshape(1024, 512)
    return ya


ATT_GROUPS = ((128, 1), (512, 4), (2048, 16))
ALIBI_SLOPES = tuple(2.0 ** (-8.0 * (h + 1) / 12) for h in range(12))


def att_bias_tables(hs):
    kk = np.arange(128)[:, None]
    qi = np.arange(128)[None, :]
    tabs = []
    for g, (window, dil) in enumerate(ATT_GROUPS):
        slope = ALIBI_SLOPES[g * 4 + hs]
        span = window // dil
        halves = []
        for kb in range(2):
            dist = qi + 128 - (kk + kb * 128)
            valid = (dist >= 0) & (dist <= span)
            halves.append(np.where(valid, -slope * dist * dil, -30000.0))
        tabs.append(np.concatenate(halves, axis=1))
    return np.stack(tabs).astype(np.float32)


def host_att(aq, ak, av):
    B, S, _ = aq.shape
    perms = []
    for window, dil in ATT_GROUPS:
        L = S // dil
        perms.append((np.arange(S).reshape(L, dil).T).reshape(-1))
    in_maps = []
    for core in range(NCORES):
        b, hs = core // 4, core % 4
        qT = np.zeros((3, 64, S), np.float32); kT = np.zeros((3, 64, S), np.float32); v = np.zeros((3, S, 64), np.float32)
        for g in range(3):
            c0 = g * 256 + hs * 64
            qT[g] = aq[b, perms[g], c0:c0 + 64].T
            kT[g] = ak[b, perms[g], c0:c0 + 64].T
            v[g] = av[b, perms[g], c0:c0 + 64]
        in_maps.append({"qT": qT, "kT": kT, "v": v, "bias": att_bias_tables(hs)})
    res = run_prog("att", build_att, in_maps)
    num = np.zeros((B, 3, S, 256), np.float32)
    den = np.zeros((B, 3, S, 4), np.float32)
    for core in range(NCORES):
        b, hs = core // 4, core % 4
        o = res[core]["o"].reshape(3, S, 65)
        for g in range(3):
            num[b, g, perms[g], hs * 64:(hs + 1) * 64] = o[g, :, 0:64]
            den[b, g, perms[g], hs] = o[g, :, 64]
    return num, den


def host_gla(gq, gk, gv, gg, galo, w_alpha, b_alpha, norm_g):
    B, S, _ = gq.shape
    mask = (np.arange(128)[:, None] <= np.arange(128)[None, :]).astype(np.float32)
    ident = np.eye(64, dtype=np.float32)
    in_maps = []
    for core in range(NCORES):
        b, h = core // 4, core % 4
        in_maps.append({
            "qT": np.ascontiguousarray(gq[b, :, h * 64:(h + 1) * 64].T),
            "kT": np.ascontiguousarray(gk[b, :, h * 64:(h + 1) * 64].T),
            "v": np.ascontiguousarray(gv[b, :, h * 128:(h + 1) * 128]),
            "g": np.ascontiguousarray(gg[b, :, h * 128:(h + 1) * 128]),
            "aloT": np.ascontiguousarray(galo[b].T),
            "wa": np.ascontiguousarray(w_alpha[:, h * 64:(h + 1) * 64]),
            "ba": np.ascontiguousarray(b_alpha[h * 64:(h + 1) * 64, None]),
            "ng": np.ascontiguousarray(np.broadcast_to(norm_g[None, h * 128:(h + 1) * 128], (128, 128))),
            "mask": mask, "ident": ident})
    res = run_prog("gla", build_gla, in_maps)
    yc = np.zeros((B, S, 512), np.float32)
    for core in range(NCORES):
        b, h = core // 4, core % 4
        yc[b, :, h * 128:(h + 1) * 128] = res[core]["o"]
    return yc


def host_rwkv(rp, mu, w0, w2, a0, a2, g2, k_k, k_a, r_k, ln_g, ln_b):
    B, S, _ = rp.shape
    W = S + 1
    blk = np.arange(128) // 64
    same = blk[:, None] == blk[None, :]
    ii = np.arange(128)
    bo = same.astype(np.float32)
    msl = (same & (ii[None, :] < ii[:, None])).astype(np.float32)
    msu = np.ascontiguousarray(msl.T)
    miu = (same & (ii[:, None] <= ii[None, :])).astype(np.float32)
    ident = np.eye(128, dtype=np.float32)
    rkf = r_k.reshape(512)

    def padT(a):
        o = np.zeros((a.shape[1], W), np.float32)
        o[:, 1:] = a.T
        return o

    in_maps = []
    for core in range(NCORES):
        b, hq = core // 4, core % 4
        cs = slice(128 * hq, 128 * hq + 128)
        x = rp[b]
        pv = np.zeros((128, 16), np.float32)
        for j, vec in enumerate((mu[0:512][cs], mu[512:1024][cs], mu[1024:1536][cs], w0[cs], a0[cs], k_k[cs], k_a[cs], rkf[cs], ln_g[cs], ln_b[cs])):
            pv[:, j] = vec
        vtok = np.zeros((W, 128), np.float32)
        vtok[1:] = x[:, 1024:1536][:, cs]
        in_maps.append({
            "r": padT(x[:, 0:512][:, cs]), "k": padT(x[:, 512:1024][:, cs]), "v": padT(x[:, 1024:1536][:, cs]),
            "lo1": padT(x[:, 1536:1600]), "lo2": padT(x[:, 1600:1696]), "vtok": vtok, "pv": pv,
            "mulo1": np.ascontiguousarray(mu[1536:1600, None]), "mulo2": np.ascontiguousarray(mu[1600:1696, None]),
            "muvt": np.ascontiguousarray(np.broadcast_to(mu[1024:1536][cs][None, :], (128, 128))),
            "w2a2": np.ascontiguousarray(np.concatenate([w2[:, cs], a2[:, cs]], 0)), "g2": np.ascontiguousarray(g2[:, cs]),
            "bo": bo, "msl": msl, "msu": msu, "miu": miu, "ident": ident})
    res = run_prog("rwkv", build_rwkv, in_maps)
    ydT = np.zeros((B, 512, S), np.float32)
    for core in range(NCORES):
        b, hq = core // 4, core % 4
        ydT[b, 128 * hq:128 * hq + 128] = res[core]["o"]
    return ydT


def tok_slices():
    return [(core // 4, slice((core % 4) * 1024, (core % 4 + 1) * 1024)) for core in range(NCORES)]


def host_c1(x, mod_l, w_in_l, wbr, ya, num, den, yc, ydT):
    wg = np.ascontiguousarray(w_in_l[:, 6064:])
    in_maps = []
    for b, ts in tok_slices():
        in_maps.append({
            "xT": np.ascontiguousarray(x[b, ts, :].T), "sc": fm16(mod_l[b, 2048:4096]), "sh": fm16(mod_l[b, 0:2048]),
            "wg": wg, "wbr": wbr,
            "yaT": np.ascontiguousarray(ya[b, ts, :].T), "ycT": np.ascontiguousarray(yc[b, ts, :].T),
            "ydT": np.ascontiguousarray(ydT[b, :, ts]),
            "ybn": np.ascontiguousarray(num[b, :, ts, :].transpose(0, 2, 1)),
            "ybd": np.ascontiguousarray(np.repeat(den[b, :, ts, :], 64, axis=2).transpose(0, 2, 1))})
    res = run_prog("c1", build_c1, in_maps)
    return [r["o"] for r in res]


def _pv(vecs):
    pv = np.zeros((128, 8, 16), np.float32)
    for j, v in enumerate(vecs):
        pv[:, j, :] = fm16(v)
    return pv


def host_c2(mT, x, mod_l, wout, ln_g, ln_b, router_w, router_b):
    ident = np.eye(128, dtype=np.float32)
    onesD = np.full((128, 128), 1.0 / 2048, np.float32)
    in_maps = []
    for core, (b, ts) in enumerate(tok_slices()):
        in_maps.append({
            "mT": mT[core], "xT": np.ascontiguousarray(x[b, ts, :].T), "wout": wout,
            "pv": _pv([mod_l[b, 4096:6144], ln_g, ln_b, mod_l[b, 8192:10240], mod_l[b, 6144:8192]]),
            "rw": router_w, "rb": np.ascontiguousarray(router_b[:, None]), "ident": ident, "onesD": onesD})
    res = run_prog("c2", build_c2, in_maps)
    return [r["x1T"] for r in res], [r["wtT"] for r in res]


def host_c3(x1T, wtT, mod_l, wgu, bgu, wd, bd, ln_g, ln_b):
    onesD = np.full((128, 128), 1.0 / 2048, np.float32)
    bgu_l = np.ascontiguousarray(bgu.reshape(32, 16, 128).transpose(2, 0, 1))
    in_maps = []
    for core, (b, ts) in enumerate(tok_slices()):
        in_maps.append({
            "x1T": x1T[core], "wtT": wtT[core],
            "pv": _pv([mod_l[b, 10240:12288], ln_g, ln_b, mod_l[b, 8192:10240], mod_l[b, 6144:8192]]),
            "wgu": wgu, "bgu": bgu_l, "wd": wd, "bd": bd, "onesD": onesD})
    res = run_prog("c3", build_c3, in_maps)
    return [r["o"] for r in res]


def host_p0(c, ada_w, ada_b):
    cT = np.ascontiguousarray(c.T.reshape(16, 128, 2).transpose(1, 0, 2))
    in_maps = []
    for core in range(NCORES):
        l, q = core // 4, core % 4
        in_maps.append({"cT": cT, "w": np.ascontiguousarray(ada_w[l][:, q * 3072:(q + 1) * 3072]),
                        "b": np.ascontiguousarray(ada_b[l][None, q * 3072:(q + 1) * 3072])})
    res = run_prog("p0", lambda: build_p0(3072), in_maps)
    mod = np.zeros((2, 2, 12288), np.float32)
    for core in range(NCORES):
        l, q = core // 4, core % 4
        mod[l][:, q * 3072:(q + 1) * 3072] = res[core]["o"]
    return mod


def host_a(x, mod_l, w_in_l):
    NC_ = 6064
    wA = np.ascontiguousarray(w_in_l[:, :NC_])
    in_maps = []
    for b, ts in tok_slices():
        in_maps.append({"xT": np.ascontiguousarray(x[b, ts, :].T), "sc": fm16(mod_l[b, 2048:4096]), "sh": fm16(mod_l[b, 0:2048]), "w": wA})
    res = run_prog("a", lambda: build_a(NC_, 1024), in_maps)
    P = np.zeros((x.shape[0], x.shape[1], NC_), np.float32)
    for core, (b, ts) in enumerate(tok_slices()):
        P[b, ts] = res[core]["o"].T
    return P


def kernel(**inputs):
    g = {k: np.asarray(v) for k, v in inputs.items()}
    x = g["x"].astype(np.float32, copy=False)
    B, S, D_ = x.shape
    mod = host_p0(g["c"], g["ada_w"], g["ada_b"])
    for l in range(2):
        L = lambda n: g[n][l]
        P = host_a(x, mod[l], L("w_in"))
        ya = host_pool(P[..., 0:512], L("pool_w"), L("pool_scale"))
        num, den = host_att(P[..., 512:1280], P[..., 1280:2048], P[..., 2048:2816])
        o = 2816
        yc = host_gla(P[..., o:o + 256], P[..., o + 256:o + 512], P[..., o + 512:o + 1024], P[..., o + 1024:o + 1536],
                      P[..., o + 1536:o + 1552], L("gla_w_alpha"), L("gla_b_alpha"), L("gla_norm_g"))
        ydT = host_rwkv(P[..., 4368:6064], L("rwkv_mu"), L("rwkv_w0"), L("rwkv_w2"), L("rwkv_a0"), L("rwkv_a2"), L("rwkv_g2"),
                        L("rwkv_k_k"), L("rwkv_k_a"), L("rwkv_r_k"), L("rwkv_ln_g"), L("rwkv_ln_b"))
        wbr = np.ascontiguousarray(np.concatenate([L("w_branch_a"), L("w_branch_b"), L("w_branch_c"), L("w_branch_d")], 0))
        mT = host_c1(x, mod[l], L("w_in"), wbr, ya, num, den, yc, ydT)
        x1T, wtT = host_c2(mT, x, mod[l], L("w_out"), L("ln1_g"), L("ln1_b"), L("router_w"), L("router_b"))
        x2T = host_c3(x1T, wtT, mod[l], L("w_gate_up"), L("b_gate_up"), L("w_down"), L("b_down"), L("ln2_g"), L("ln2_b"))
        xn = np.zeros((B, S, D_), np.float32)
        for core, (b, ts) in enumerate(tok_slices()):
            xn[b, ts] = x2T[core].T
        x = xn
    return x
```
